# Optimizing a Trainium2 kernel written in Bass

```python
import math
import jax, jax.numpy as jnp
from jax import lax
import numpy as np

D_MODEL = 1024
BATCH = 8
SEQ = 4096
DEPTH = 1

POOL_WIDTH = D_MODEL // 2
POOL_WINDOWS = (2, 4, 8, 16)
POOL_GROUPS = len(POOL_WINDOWS)
POOL_GROUP_DIM = POOL_WIDTH // POOL_GROUPS
N_HEADS = 8
N_KV_GROUPS = 2
HEADS_PER_GROUP = N_HEADS // N_KV_GROUPS
HEAD_DIM = 64
Q_WIDTH = N_HEADS * HEAD_DIM
KV_WIDTH = N_KV_GROUPS * HEAD_DIM
CMP_BLOCK = 32
CMP_STRIDE = 16
CMP_HIDDEN = 128
SEL_BLOCK = 64
SEL_TOPK = 16
WINDOW = 512
Q_CHUNK = 64
FORCE_SCORE = 1000.0
N_BUCKETS = 32
MAX_DISTANCE = 128
PEER_HEADS = 8
PEER_KEYS = 128
PEER_EXPERTS = PEER_KEYS * PEER_KEYS
PEER_TOPK = 16
PEER_QDIM = 256
PEER_CHUNK = 128
IN_SIZES = (POOL_WIDTH, Q_WIDTH, KV_WIDTH, KV_WIDTH, KV_WIDTH, KV_WIDTH, KV_WIDTH, KV_WIDTH, N_HEADS * 3, 2 * D_MODEL)
IN_COLS = sum(IN_SIZES)
EPS = 1e-6
NEG_INF = -1e30

kernel_name = 'hybrid_pool_nsa_peer_block'


def rms_norm(x, g):
    xf = x.astype(jnp.float32)
    y = xf * lax.rsqrt(jnp.mean(xf * xf, axis=-1, keepdims=True) + EPS)
    return (y * g.astype(jnp.float32)).astype(x.dtype)


def masked_softmax(logits, valid):
    lf = jnp.where(valid, logits.astype(jnp.float32), NEG_INF)
    p = jax.nn.softmax(lf, axis=-1)
    return jnp.where(valid, p, 0.0)


def t5_bucket(rel):
    n = jnp.maximum(rel, 0)
    max_exact = N_BUCKETS // 2
    nf = jnp.maximum(n, 1).astype(jnp.float32)
    large = max_exact + (jnp.log(nf / max_exact) / math.log(MAX_DISTANCE / max_exact) * (N_BUCKETS - max_exact)).astype(jnp.int32)
    large = jnp.minimum(large, N_BUCKETS - 1)
    return jnp.where(n < max_exact, n, large)


def rel_bias_dense(table, rel):
    b = table[t5_bucket(rel)]
    return jnp.transpose(b, (2, 0, 1)).reshape(N_KV_GROUPS, HEADS_PER_GROUP, *rel.shape)


def sel_mapping(n_cmp, n_sel):
    m = np.zeros((n_cmp, n_sel), np.float32)
    pos = np.arange(n_cmp)[:, None] * CMP_STRIDE + np.arange(CMP_BLOCK)[None, :]
    np.add.at(m, (np.repeat(np.arange(n_cmp), CMP_BLOCK), (pos // SEL_BLOCK).ravel()), 1.0 / CMP_BLOCK)
    return jnp.asarray(m)


def pool_mixer(u, w_pool, pool_scale):
    b_, s_, c_ = u.shape
    uf = u.astype(jnp.float32)
    csp = jnp.concatenate([jnp.zeros((b_, 1, c_), jnp.float32), jnp.cumsum(uf, axis=1)], axis=1)
    t = jnp.arange(s_)
    outs = []
    for gi, w in enumerate(POOL_WINDOWS):
        sl = slice(gi * POOL_GROUP_DIM, (gi + 1) * POOL_GROUP_DIM)
        cg = csp[:, :, sl]
        upper = cg[:, 1:]
        lower = jnp.pad(cg[:, :s_ + 1 - w], ((0, 0), (w - 1, 0), (0, 0)))
        cnt = jnp.minimum(t + 1, w).astype(jnp.float32)[:, None]
        outs.append((upper - lower) / cnt - uf[:, :, sl])
    pooled = jnp.stack(outs, axis=2).astype(u.dtype)
    y = jnp.einsum('bsgc,gcd->bsgd', pooled, w_pool).reshape(b_, s_, POOL_WIDTH)
    return y * pool_scale


def compress_blocks(t, pe, w1, w2):
    b_, g_, s_, dh = t.shape
    ratio = CMP_BLOCK // CMP_STRIDE
    n_str = s_ // CMP_STRIDE
    n_cmp = n_str - ratio + 1
    pieces = t.reshape(b_, g_, n_str, CMP_STRIDE, dh)
    blk = jnp.concatenate([pieces[:, :, r:r + n_cmp] for r in range(ratio)], axis=3)
    blk = (blk + pe).reshape(b_, g_, n_cmp, CMP_BLOCK * dh)
    return jax.nn.gelu(blk @ w1) @ w2


def nsa_attention(q, kc, vc, ks, vs, kw, vw, gates, rel_bias):
    b_, g_, j_, s_, dh = q.shape
    scale = dh ** -0.5
    n_cmp = kc.shape[2]
    n_sel = s_ // SEL_BLOCK
    k_top = min(SEL_TOPK, n_sel)
    cmp_end = jnp.arange(n_cmp) * CMP_STRIDE + CMP_BLOCK - 1
    smap = sel_mapping(n_cmp, n_sel)
    ksb = ks.reshape(b_, g_, n_sel, SEL_BLOCK, dh)
    vsb = vs.reshape(b_, g_, n_sel, SEL_BLOCK, dh)
    kw_pad = jnp.pad(kw, ((0, 0), (0, 0), (WINDOW, 0), (0, 0)))
    vw_pad = jnp.pad(vw, ((0, 0), (0, 0), (WINDOW, 0), (0, 0)))
    tbl_g = rel_bias.reshape(N_BUCKETS, g_, j_)
    bi = jnp.arange(b_)[:, None, None, None]
    gi = jnp.arange(g_)[None, :, None, None]
    blk_id = jnp.arange(n_sel)

    def chunk(ci):
        s0 = ci * Q_CHUNK
        qc = lax.dynamic_slice_in_dim(q, s0, Q_CHUNK, axis=3)
        gc = lax.dynamic_slice_in_dim(gates, s0, Q_CHUNK, axis=3)
        tpos = s0 + jnp.arange(Q_CHUNK)
        rel_c = tpos[:, None] - cmp_end[None, :]
        lg_c = jnp.einsum('bgjqd,bgnd->bgjqn', qc, kc) * scale + rel_bias_dense(rel_bias, rel_c)
        p_c = masked_softmax(lg_c, rel_c >= 0)
        o_cmp = jnp.einsum('bgjqn,bgnd->bgjqd', p_c.astype(vc.dtype), vc)
        imp = jnp.einsum('bgjqn,ns->bgqs', p_c, smap)
        cur = tpos // SEL_BLOCK
        forced = (blk_id[None, :] == 0) | (blk_id[None, :] == cur[:, None]) | (blk_id[None, :] == cur[:, None] - 1)
        visible = blk_id[None, :] * SEL_BLOCK <= tpos[:, None]
        score = jnp.where(visible, imp + jnp.where(forced, FORCE_SCORE, 0.0), -1.0)
        _, idx = lax.top_k(score, k_top)
        k_g = ksb[bi, gi, idx].reshape(b_, g_, Q_CHUNK, k_top * SEL_BLOCK, dh)
        v_g = vsb[bi, gi, idx].reshape(b_, g_, Q_CHUNK, k_top * SEL_BLOCK, dh)
        kpos = (idx[..., None] * SEL_BLOCK + jnp.arange(SEL_BLOCK)).reshape(b_, g_, Q_CHUNK, k_top * SEL_BLOCK)
        rel_s = tpos[None, None, :, None] - kpos
        bias_s = jnp.moveaxis(tbl_g[t5_bucket(rel_s), gi], -1, 2)
        lg_s = jnp.einsum('bgjqd,bgqkd->bgjqk', qc, k_g) * scale + bias_s
        p_s = masked_softmax(lg_s, (rel_s >= 0)[:, :, None])
        o_sel = jnp.einsum('bgjqk,bgqkd->bgjqd', p_s.astype(v_g.dtype), v_g)
        kwc = lax.dynamic_slice_in_dim(kw_pad, s0, WINDOW + Q_CHUNK, axis=2)
        vwc = lax.dynamic_slice_in_dim(vw_pad, s0, WINDOW + Q_CHUNK, axis=2)
        kpos_w = s0 - WINDOW + jnp.arange(WINDOW + Q_CHUNK)
        rel_w = tpos[:, None] - kpos_w[None, :]
        valid_w = (rel_w >= 0) & (rel_w < WINDOW) & (kpos_w >= 0)[None, :]
        lg_w = jnp.einsum('bgjqd,bgkd->bgjqk', qc, kwc) * scale + rel_bias_dense(rel_bias, rel_w)
        p_w = masked_softmax(lg_w, valid_w)
        o_win = jnp.einsum('bgjqk,bgkd->bgjqd', p_w.astype(vwc.dtype), vwc)
        out = gc[..., 0:1] * o_cmp + gc[..., 1:2] * o_sel + gc[..., 2:3] * o_win
        return out.astype(q.dtype)

    outs = lax.map(chunk, jnp.arange(s_ // Q_CHUNK))
    return jnp.transpose(outs, (1, 0, 4, 2, 3, 5)).reshape(b_, s_, g_ * j_ * dh)


def peer_ffn(h, w_q, sub_keys, u, v):
    b_, s_, d_ = h.shape
    tokens = h.reshape(-1, PEER_CHUNK, d_)

    def chunk(xc):
        qv = (xc @ w_q).reshape(PEER_CHUNK, PEER_HEADS, 2, PEER_QDIM // 2)
        sc = jnp.einsum('chpd,hpkd->chpk', qv, sub_keys).astype(jnp.float32)
        s_half, i_half = lax.top_k(sc, PEER_TOPK)
        cand = (s_half[:, :, 0, :, None] + s_half[:, :, 1, None, :]).reshape(PEER_CHUNK, PEER_HEADS, PEER_TOPK * PEER_TOPK)
        cid = (i_half[:, :, 0, :, None] * PEER_KEYS + i_half[:, :, 1, None, :]).reshape(PEER_CHUNK, PEER_HEADS, PEER_TOPK * PEER_TOPK)
        top_s, top_j = lax.top_k(cand, PEER_TOPK)
        eid = jnp.take_along_axis(cid, top_j, axis=-1)
        g = jax.nn.softmax(top_s, axis=-1).astype(xc.dtype)
        ue = u[eid]
        ve = v[eid]
        a = jax.nn.gelu(jnp.einsum('cd,chkd->chk', xc, ue))
        return jnp.einsum('chk,chkd->cd', g * a, ve)

    return lax.map(chunk, tokens).reshape(b_, s_, d_)


def setup_inputs(seed: int = 0) -> dict:
    key = jax.random.key(seed)
    ks = jax.random.split(key, 25)
    f32 = jnp.float32
    L = DEPTH
    D = D_MODEL

    def nrm(k, shape, s):
        return jax.random.normal(k, shape, f32) * s

    return {
        'x': nrm(ks[0], (BATCH, SEQ, D), 1.0),
        'c': nrm(ks[1], (BATCH, D), 1.0),
        'rel_bias': nrm(ks[2], (N_BUCKETS, N_HEADS), 0.5),
        'ada_w': nrm(ks[3], (L, D, 6 * D), 0.5 * D ** -0.5),
        'ada_b': nrm(ks[4], (L, 6 * D), 0.01),
        'norm1_g': 1.0 + nrm(ks[5], (L, D), 0.05),
        'norm2_g': 1.0 + nrm(ks[6], (L, D), 0.05),
        'w_in': nrm(ks[7], (L, D, IN_COLS), D ** -0.5),
        'pool_w': nrm(ks[8], (L, POOL_GROUPS, POOL_GROUP_DIM, POOL_GROUP_DIM), POOL_GROUP_DIM ** -0.5),
        'pool_scale': 1.0 + nrm(ks[9], (L, POOL_WIDTH), 0.05),
        'cmp_pe_k': nrm(ks[10], (L, CMP_BLOCK, HEAD_DIM), 0.5),
        'cmp_w1_k': nrm(ks[11], (L, CMP_BLOCK * HEAD_DIM, CMP_HIDDEN), (CMP_BLOCK * HEAD_DIM) ** -0.5),
        'cmp_w2_k': nrm(ks[12], (L, CMP_HIDDEN, HEAD_DIM), CMP_HIDDEN ** -0.5),
        'cmp_pe_v': nrm(ks[13], (L, CMP_BLOCK, HEAD_DIM), 0.5),
        'cmp_w1_v': nrm(ks[14], (L, CMP_BLOCK * HEAD_DIM, CMP_HIDDEN), (CMP_BLOCK * HEAD_DIM) ** -0.5),
        'cmp_w2_v': nrm(ks[15], (L, CMP_HIDDEN, HEAD_DIM), CMP_HIDDEN ** -0.5),
        'q_norm_g': 1.0 + nrm(ks[16], (L, HEAD_DIM), 0.05),
        'k_norm_g': 1.0 + nrm(ks[17], (L, 3, HEAD_DIM), 0.05),
        'w_branch_pool': nrm(ks[18], (L, POOL_WIDTH, D), POOL_WIDTH ** -0.5),
        'w_branch_attn': nrm(ks[19], (L, Q_WIDTH, D), Q_WIDTH ** -0.5),
        'w_out': nrm(ks[20], (L, D, D), D ** -0.5),
        'peer_w_q': nrm(ks[21], (L, D, PEER_HEADS * PEER_QDIM), D ** -0.5),
        'peer_sub_keys': nrm(ks[22], (L, PEER_HEADS, 2, PEER_KEYS, PEER_QDIM // 2), (PEER_QDIM // 2) ** -0.5),
        'peer_u': nrm(ks[23], (L, PEER_EXPERTS, D), D ** -0.5),
        'peer_v': nrm(ks[24], (L, PEER_EXPERTS, D), PEER_HEADS ** -0.5),
    }


def reference(x, c, rel_bias, ada_w, ada_b, norm1_g, norm2_g, w_in, pool_w, pool_scale, cmp_pe_k, cmp_w1_k, cmp_w2_k, cmp_pe_v, cmp_w1_v, cmp_w2_v, q_norm_g, k_norm_g, w_branch_pool, w_branch_attn, w_out, peer_w_q, peer_sub_keys, peer_u, peer_v):
    b_, s_, d_ = x.shape
    split_at = np.cumsum(IN_SIZES)[:-1].tolist()

    def to_groups(t):
        return jnp.transpose(t.reshape(b_, s_, N_KV_GROUPS, HEAD_DIM), (0, 2, 1, 3))

    for l in range(DEPTH):
        ada = (jax.nn.silu(c) @ ada_w[l] + ada_b[l]).reshape(b_, 6, 1, d_)
        shift1, scale1, gate1 = ada[:, 0], ada[:, 1], ada[:, 2]
        shift2, scale2, gate2 = ada[:, 3], ada[:, 4], ada[:, 5]
        h = rms_norm(x, norm1_g[l]) * (1.0 + scale1) + shift1
        z = h @ w_in[l]
        z_pool, z_q, z_kc, z_vc, z_ks, z_vs, z_kw, z_vw, z_gate, z_merge = jnp.split(z, split_at, axis=-1)
        y_pool = pool_mixer(z_pool, pool_w[l], pool_scale[l])
        q = rms_norm(z_q.reshape(b_, s_, N_KV_GROUPS, HEADS_PER_GROUP, HEAD_DIM), q_norm_g[l])
        q = jnp.transpose(q, (0, 2, 3, 1, 4))
        kc = rms_norm(compress_blocks(to_groups(z_kc), cmp_pe_k[l], cmp_w1_k[l], cmp_w2_k[l]), k_norm_g[l, 0])
        vc = compress_blocks(to_groups(z_vc), cmp_pe_v[l], cmp_w1_v[l], cmp_w2_v[l])
        ksl = rms_norm(to_groups(z_ks), k_norm_g[l, 1])
        vsl = to_groups(z_vs)
        kwn = rms_norm(to_groups(z_kw), k_norm_g[l, 2])
        vwn = to_groups(z_vw)
        gates = jnp.transpose(jax.nn.sigmoid(z_gate.reshape(b_, s_, N_KV_GROUPS, HEADS_PER_GROUP, 3)), (0, 2, 3, 1, 4))
        y_attn = nsa_attention(q, kc, vc, ksl, vsl, kwn, vwn, gates, rel_bias)
        g_merge = jax.nn.sigmoid(z_merge).reshape(b_, s_, 2, d_)
        mixed = g_merge[:, :, 0] * (y_pool @ w_branch_pool[l]) + g_merge[:, :, 1] * (y_attn @ w_branch_attn[l])
        x = x + gate1 * (mixed @ w_out[l])
        h2 = rms_norm(x, norm2_g[l]) * (1.0 + scale2) + shift2
        x = x + gate2 * peer_ffn(h2, peer_w_q[l], peer_sub_keys[l], peer_u[l], peer_v[l])
    return x
```

```python
import math
from contextlib import ExitStack
import numpy as np
import ml_dtypes
import concourse.bass as bass
import concourse.mybir as mybir
from concourse.bass_utils import run_bass_kernel_spmd
from concourse.bass_types import AP

F32 = mybir.dt.float32
BF16 = mybir.dt.bfloat16
U32 = mybir.dt.uint32
AF = mybir.ActivationFunctionType
ALU = mybir.AluOpType
AX = mybir.AxisListType
NPBF = ml_dtypes.bfloat16

SEQ = 4096
DM = 1024
TG = 256
NGROUPS = SEQ // TG
EPS = 1e-6
NEG = -30000.0
RW = 768
RC = 6200
RC_OFF = 2100
NA = 3840
NB = 280


class Tk:
    __slots__ = ("name", "w", "r", "dsem", "dcnt")

    def __init__(self, name):
        self.name = name
        self.w = {}
        self.r = {}
        self.dsem = None
        self.dcnt = 0


class Sync:
    SEM_MAX = 20000

    def __init__(self, nc, stack):
        self.nc = nc
        self.stack = stack
        self.eng = {}
        self.nsem = 0
        self.pending = {}
        self.track_all = True
        for name, e in [("pe", nc.tensor), ("act", nc.scalar), ("dve", nc.vector),
                        ("pool", nc.gpsimd), ("sp", nc.sync)]:
            self.eng[name] = dict(e=e, sem=self._newsem(name), cnt=0, waited={}, name=name, n=0)

    def _newsem(self, name):
        self.nsem += 1
        return self.stack.enter_context(self.nc.semaphore("s%d_%s" % (self.nsem, name)))

    def _wait(self, E, s, v):
        k = id(s)
        if E["waited"].get(k, 0) >= v:
            return
        E["e"].wait_ge(s, v)
        E["waited"][k] = v

    def _deps(self, E, reads, writes, nowaw=False, skip_own=False):
        deps = {}

        def add(tok):
            if tok is None:
                return
            s, v = tok
            k = id(s)
            if k not in deps or deps[k][1] < v:
                deps[k] = (s, v)
        for t in reads:
            for tok in t.w.values():
                add(tok)
        for t in writes:
            if not nowaw:
                for tok in t.w.values():
                    add(tok)
            for tok in t.r.values():
                add(tok)
        for k, (s, v) in deps.items():
            if skip_own and s is E["sem"]:
                continue
            self._wait(E, s, v)

    def _mark(self, tok, reads, writes, nowaw=False):
        k = id(tok[0])
        for t in reads:
            t.r[k] = tok
        for t in writes:
            if nowaw:
                t.w[k] = tok
            else:
                t.w = {k: tok}
                t.r = {}

    def op(self, en, fn, reads=(), writes=()):
        E = self.eng[en]
        self._deps(E, reads, writes, skip_own=(en == "pe"))
        if E["cnt"] >= self.SEM_MAX:
            E["sem"] = self._newsem(en)
            E["cnt"] = 0
        ins = fn(E["e"])
        E["cnt"] += 1
        E["n"] += 1
        ins.then_inc(E["sem"], 1)
        E["last"] = (E["sem"], E["cnt"])
        self._mark((E["sem"], E["cnt"]), reads, writes)
        return ins

    def dma(self, qn, out, in_, reads=(), writes=(), nowaw=False, track=False, semtk=None, **kw):
        Q = self.eng[qn]
        self._deps(Q, reads, writes, nowaw=nowaw)
        t0 = semtk if semtk is not None else writes[0]
        if t0.dsem is None or t0.dcnt >= self.SEM_MAX:
            t0.dsem = self._newsem("d_" + t0.name)
            t0.dcnt = 0
        ins = Q["e"].dma_start(out=out, in_=in_, **kw)
        t0.dcnt += 16
        Q["n"] += 1
        ins.then_inc(t0.dsem, 16)
        if track or self.track_all:
            self.pending[id(t0.dsem)] = (t0.dsem, t0.dcnt)
        self._mark((t0.dsem, t0.dcnt), reads, writes, nowaw=nowaw)
        return ins

    def wait_all(self, en, tks):
        self._deps(self.eng[en], tks, tks)

    def barrier(self):
        names = ["pe", "act", "dve", "pool"]
        for a in names + ["sp"]:
            for b in names:
                if a != b and self.eng[b].get("last") is not None:
                    self._wait(self.eng[a], *self.eng[b]["last"])
            for (sm, v) in self.pending.values():
                self._wait(self.eng[a], sm, v)
        self.pending = {}


def _bucket(n):
    n = int(n)
    if n < 16:
        return n
    nf = np.float32(n)
    v = np.log(nf / np.float32(16)) / np.float32(math.log(128 / 16)) * np.float32(16)
    return min(31, 16 + int(np.float32(v)))


def host_consts():
    c = {}
    c["ident_f"] = np.eye(128, dtype=np.float32)
    c["ident_b"] = np.eye(128, dtype=np.float32).astype(NPBF)
    c["J_b"] = np.eye(128, dtype=np.float32)[::-1].copy().astype(NPBF)
    bd = np.zeros((128, 128), np.float32)
    bd[:64, :64] = 1
    bd[64:, 64:] = 1
    c["BD_b"] = bd.astype(NPBF)
    c["iotaB"] = np.tile(np.arange(128, dtype=np.float32), (128, 1)).astype(NPBF)
    c["iota16"] = np.tile(np.arange(16, dtype=np.float32), (128, 1))
    c["ones_row"] = np.ones((1, 128), np.float32)
    ohw = np.zeros((33, RW), np.float32)
    for i in range(RW):
        r = i - 128
        if r < 0 or r >= 512:
            ohw[32, i] = 1
        else:
            ohw[_bucket(r), i] = 1
    c["ohw"] = ohw
    ohc = np.zeros((33, RC), np.float32)
    for i in range(RC):
        r = i - RC_OFF
        if r < 0:
            ohc[32, i] = 1
        else:
            ohc[_bucket(r) if r < 128 else 31, i] = 1
    c["ohc"] = ohc
    smap = np.zeros((256, 64), np.float32)
    for n in range(255):
        for p in range(32):
            smap[n, (16 * n + p) // 64] += 1.0 / 32
    c["smapc"] = smap.reshape(2, 128, 64).transpose(1, 0, 2).copy().astype(NPBF)
    E = np.zeros((128, 4096), np.float32)
    E[np.arange(4096) // 64, np.arange(4096)] = 1
    c["Eall"] = E.astype(NPBF)
    t = np.arange(4096)[:, None]
    s = np.arange(64)[None, :]
    cur = t // 64
    forced = (s == 0) | (s == cur) | (s == cur - 1)
    vis = (s * 64) <= t
    C = np.where(vis, np.where(forced, 1000.0, 0.0), -1.0).astype(np.float32)
    c["Cst"] = C.reshape(32, 128, 64).transpose(1, 0, 2).copy()
    inv = np.zeros((128, 4, 16), np.float32)
    for ci, w in enumerate((2, 4, 8, 16)):
        for tt in range(16):
            inv[:, ci, tt] = 1.0 / min(tt + 1, w)
    c["invc16"] = inv
    return c


CONST_SPECS = {
    "ident_f": ([128, 128], F32), "ident_b": ([128, 128], BF16), "J_b": ([128, 128], BF16),
    "BD_b": ([128, 128], BF16), "iotaB": ([128, 128], BF16), "iota16": ([128, 16], F32),
    "ones_row": ([1, 128], F32), "ohw": ([33, RW], F32), "ohc": ([33, RC], F32),
    "smapc": ([128, 2, 64], BF16), "Eall": ([128, 4096], BF16), "Cst": ([128, 32, 64], F32),
    "invc16": ([128, 4, 16], F32),
}

INPUT_SPECS = {
    "x": [SEQ, DM], "cT": [128, 8], "rel_bias": [32, 8], "ada_w": [DM, 6 * DM], "adabT": [128, 48],
    "n1g": [128, 8], "n2g": [128, 8], "w_in": [DM, 3864], "pool_w": [4, 128, 128], "pscale": [128, 4],
    "pek": [128, 16], "w1k": [2048, 128], "w2k": [128, 64], "pev": [128, 16], "w1v": [2048, 128],
    "w2v": [128, 64], "gq": [128, 1], "gk": [128, 3], "wbp": [512, DM], "wba": [512, DM],
    "wout": [DM, DM], "wq": [DM, 2048], "subk": [8, 2, 128, 128], "pu": [16384, DM], "pv": [16384, DM],
}


def colT(v, k):
    return np.ascontiguousarray(np.asarray(v, np.float32).reshape(k, 128).T)


def make_in_map(inputs, b, consts):
    m = {}
    m["x"] = np.ascontiguousarray(inputs["x"][b])
    m["cT"] = colT(inputs["c"][b], 8)
    m["rel_bias"] = np.ascontiguousarray(inputs["rel_bias"])
    m["ada_w"] = np.ascontiguousarray(inputs["ada_w"][0])
    m["adabT"] = colT(inputs["ada_b"][0], 48)
    m["n1g"] = colT(inputs["norm1_g"][0], 8)
    m["n2g"] = colT(inputs["norm2_g"][0], 8)
    m["w_in"] = np.ascontiguousarray(inputs["w_in"][0])
    m["pool_w"] = np.ascontiguousarray(inputs["pool_w"][0])
    m["pscale"] = colT(inputs["pool_scale"][0], 4)
    m["pek"] = colT(inputs["cmp_pe_k"][0].reshape(-1), 16)
    m["w1k"] = np.ascontiguousarray(inputs["cmp_w1_k"][0])
    m["w2k"] = np.ascontiguousarray(inputs["cmp_w2_k"][0])
    m["pev"] = colT(inputs["cmp_pe_v"][0].reshape(-1), 16)
    m["w1v"] = np.ascontiguousarray(inputs["cmp_w1_v"][0])
    m["w2v"] = np.ascontiguousarray(inputs["cmp_w2_v"][0])
    gq = np.asarray(inputs["q_norm_g"][0], np.float32)
    m["gq"] = np.ascontiguousarray(np.concatenate([gq, gq]).reshape(128, 1))
    gk = np.asarray(inputs["k_norm_g"][0], np.float32)
    m["gk"] = np.ascontiguousarray(np.concatenate([gk, gk], axis=1).T)
    m["wbp"] = np.ascontiguousarray(inputs["w_branch_pool"][0])
    m["wba"] = np.ascontiguousarray(inputs["w_branch_attn"][0])
    m["wout"] = np.ascontiguousarray(inputs["w_out"][0])
    m["wq"] = np.ascontiguousarray(inputs["peer_w_q"][0])
    m["subk"] = np.ascontiguousarray(inputs["peer_sub_keys"][0])
    m["pu"] = np.ascontiguousarray(inputs["peer_u"][0])
    m["pv"] = np.ascontiguousarray(inputs["peer_v"][0])
    for k in m:
        m[k] = np.asarray(m[k], np.float32)
    m.update(consts)
    return m


class _Stop(Exception):
    pass


def build(NG=NGROUPS, debug=False, prepass=True, stop=None):
    nc = bass.Bass("TRN2", target_bir_lowering=False)
    din = {}
    for k, shp in INPUT_SPECS.items():
        din[k] = nc.dram_tensor(k, list(shp), F32, kind="ExternalInput")
    for k, (shp, dt) in CONST_SPECS.items():
        din[k] = nc.dram_tensor(k, list(shp), dt, kind="ExternalInput")
    out_d = nc.dram_tensor("out", [SEQ, DM], F32, kind="ExternalOutput")
    winA_s = nc.dram_tensor("winA_s", [128, 8, NA], BF16, kind="Internal")
    winB_s = nc.dram_tensor("winB_s", [128, 8, NB], BF16, kind="Internal")
    wbp_s = nc.dram_tensor("wbp_s", [128, 4, DM], BF16, kind="Internal")
    wba_s = nc.dram_tensor("wba_s", [128, 4, DM], BF16, kind="Internal")
    wout_s = nc.dram_tensor("wout_s", [128, 8, DM], BF16, kind="Internal")
    wq_s = nc.dram_tensor("wq_s", [128, 8, 2048], BF16, kind="Internal")
    uT_s = nc.dram_tensor("uT_s", [64, 128, 2, 8, 128], BF16, kind="Internal")
    v_s = nc.dram_tensor("v_s", [64, 128, 2, DM], BF16, kind="Internal")
    fW_s = nc.dram_tensor("fW_s", [8, RW], BF16, kind="Internal")
    fC_s = nc.dram_tensor("fC_s", [8, RC], BF16, kind="Internal")
    dbg = {}

    with ExitStack() as st:
        S = Sync(nc, st)

        def ck(name):
            if stop == name:
                raise _Stop()

        def sb(name, shape, dt):
            return st.enter_context(nc.sbuf_tensor("s_" + name, list(shape), dt))

        def PE(fn, r, w):
            return S.op("pe", fn, r, w)

        def ACT(fn, r, w):
            return S.op("act", fn, r, w)

        def DVE(fn, r, w):
            return S.op("dve", fn, r, w)

        def POOL(fn, r, w):
            return S.op("pool", fn, r, w)

        def DMA(out, in_, r, w, nowaw=False, q="sp", track=False, semtk=None):
            return S.dma(q, out, in_, r, w, nowaw=nowaw, track=track, semtk=semtk)

        flip = [0]

        def EV(fn, r, w):
            flip[0] ^= 1
            if flip[0]:
                return S.op("act", lambda e: fn(e, True), r, w)
            return S.op("dve", lambda e: fn(e, False), r, w)

        def copy_ev(out, in_, r, w):
            return EV(lambda e, a: (e.copy(out=out, in_=in_) if a else e.tensor_copy(out=out, in_=in_)), r, w)

        pb = [st.enter_context(nc.psum_tensor("pb%d" % i, [128, 512], F32)) for i in range(8)]
        Tpb = [Tk("pb%d" % i) for i in range(8)]
        rot = {"all": [list(range(8)), 0], "acc": [[0, 1, 2], 0], "sc": [[3, 4, 5, 6, 7], 0],
               "hi": [[4, 5, 6, 7], 0]}

        def bank(pool="all"):
            lst, i = rot[pool]
            rot[pool][1] = (i + 1) % len(lst)
            b = lst[i]
            return pb[b], Tpb[b]

        def bfv(P):
            return P[:].bitcast(BF16)

        Tc = Tk("consts")

        def cload(name, shape, dt, src):
            t = sb(name, shape, dt)
            DMA(t[:], src, [], [Tc], nowaw=True)
            return t

        ident_f = cload("ident_f", [128, 128], F32, din["ident_f"].ap())
        ident_b = cload("ident_b", [128, 128], BF16, din["ident_b"].ap())
        Jb = cload("J_b", [128, 128], BF16, din["J_b"].ap())
        BD = cload("BD_b", [128, 128], BF16, din["BD_b"].ap())
        iotaB = cload("iotaB", [128, 128], BF16, din["iotaB"].ap())
        iota16 = cload("iota16", [128, 16], F32, din["iota16"].ap())
        Eall = cload("Eall", [128, 4096], BF16, din["Eall"].ap())
        invc16 = cload("invc16", [128, 4, 16], F32, din["invc16"].ap())
        n1g = cload("n1g", [128, 8], F32, din["n1g"].ap())
        n2g = cload("n2g", [128, 8], F32, din["n2g"].ap())
        pscale = cload("pscale", [128, 4], F32, din["pscale"].ap())
        gq = cload("gq", [128, 1], F32, din["gq"].ap())
        gk = cload("gk", [128, 3], F32, din["gk"].ap())

        ksT = sb("ksT", [128, SEQ], BF16)
        kwT = sb("kwT", [128, SEQ], BF16)
        Tks, Tkw = Tk("ksT"), Tk("kwT")
        vsA = sb("vsA", [128, 32, 2, 65], BF16)
        vwA = sb("vwA", [128, 32, 2, 65], BF16)
        Tvs, Tvw = Tk("vsA"), Tk("vwA")
        kcT = sb("kcT", [128, 256], BF16)
        vcT = sb("vcT", [128, 256], BF16)
        vcA = sb("vcA", [128, 2, 2, 129], BF16)
        Tkc, Tvc, TvcA = Tk("kcT"), Tk("vcT"), Tk("vcA")
        TZ = sb("TZ", [128, 3, 8, 128], BF16)
        TTZ = Tk("TZ")
        skT = sb("skT", [128, 16, 128], BF16)
        Tsk = Tk("skT")
        w1 = {"k": sb("w1k", [128, 16, 128], BF16), "v": sb("w1v", [128, 16, 128], BF16)}
        w2 = {"k": sb("w2k", [128, 2, 128], BF16), "v": sb("w2v", [128, 2, 128], BF16)}
        cb = {"k": sb("cbk", [128, 1], F32), "v": sb("cbv", [128, 1], F32)}
        Tw1 = Tk("w1")
        winB = sb("winB", [128, 8, NB], BF16)
        TwinB = Tk("winB")
        wpool = sb("wpool", [128, 4, 128], BF16)
        Twpool = Tk("wpool")
        adaT = sb("adaT", [128, 96], F32)
        TadaT = Tk("adaT")
        gmod = [sb("gmod1", [128, 8], F32), sb("gmod2", [128, 8], F32)]
        Tgmod = Tk("gmod")
        b31c = sb("b31c", [128, 8], F32)
        b31m = sb("b31m", [128, 8], F32)
        n256 = sb("n256", [128, 1], F32)
        zcol = sb("zcol", [128, 1], F32)
        Tb31 = Tk("b31")
        xt = [sb("xt0", [128, DM], F32), sb("xt1", [128, DM], F32)]
        Txt = [Tk("xt0"), Tk("xt1")]
        xin = [sb("xin0", [128, DM], F32), sb("xin1", [128, DM], F32)]
        Txin = [Tk("xin0"), Tk("xin1")]
        h2T = sb("h2T", [128, 8, TG], BF16)
        Th2 = Tk("h2T")
        ITt = sb("ITt", [128, TG], F32)
        JTt = sb("JTt", [128, TG], F32)
        GTt = sb("GTt", [128, TG], F32)
        Tijg = Tk("ijg")
        NLR = 4
        Lb = [sb("L%d" % i, [128, 128], BF16) for i in range(NLR)]
        Rb = [sb("R%d" % i, [128, 128], BF16) for i in range(NLR)]
        TL = [Tk("L%d" % i) for i in range(NLR)]
        TR = [Tk("R%d" % i) for i in range(NLR)]
        NGA = 3
        gab = [sb("ga%d" % i, [128, TG], BF16) for i in range(NGA)]
        wab = [sb("wa%d" % i, [128, TG], BF16) for i in range(NGA)]
        Tga = [Tk("ga%d" % i) for i in range(NGA)]
        Twa = [Tk("wa%d" % i) for i in range(NGA)]
        NWS = 3
        ws = [sb("ws%d" % i, [128, 8, 256], BF16) for i in range(NWS)]
        Tws = [Tk("ws%d" % i) for i in range(NWS)]
        wsi = [0]
        wbb = [sb("wbb%d" % i, [128, 4, 512], BF16) for i in range(2)]
        Twbb = [Tk("wbb%d" % i) for i in range(2)]
        NUV = 3
        uvu = [sb("uvu%d" % i, [128, 2, 8, 128], BF16) for i in range(NUV)]
        uvv = [sb("uvv%d" % i, [128, 2, DM], BF16) for i in range(NUV)]
        Tuv = [Tk("uv%d" % i) for i in range(NUV)]
        Tus = [Tk("us%d" % i) for i in range(5)]
        TTC = [Tk("TC0"), Tk("TC1")]
        Cstt = [sb("Cst%d" % i, [128, 2, 64], F32) for i in range(2)]
        TCst = [Tk("Cst%d" % i) for i in range(2)]
        arena = sb("arena", [128, 32768], BF16)
        Tout = Tk("out")
        Tscr = {k: Tk(k) for k in ("winA", "winB", "wbp", "wba", "wout", "wq", "uT", "v", "fW", "fC")}

        def adacol(which, k):
            i = (which * 8 + k) * 2
            return adaT[:, i:i + 1]

        def strided(tile, col, step, n):
            base = tile[:, col:col + 1]
            return AP(base.tensor, base.offset, [list(base.ap[0]), [step, n]])

        aoff = [0]

        def carve_reset(base=0):
            aoff[0] = base

        def carve(free_shape, dt):
            n = int(np.prod(free_shape))
            nb = n * (2 if dt == BF16 else 4)
            nb4 = (nb + 3) // 4 * 4
            a = aoff[0]
            aoff[0] += nb4
            assert aoff[0] <= 65536, ("arena overflow", aoff[0])
            v = arena[:, a // 2:(a + nb4) // 2]
            if dt != BF16:
                v = v.bitcast(dt)
            v = v[:, 0:n]
            if len(free_shape) == 2:
                v = v.rearrange("p (a b) -> p a b", a=free_shape[0])
            elif len(free_shape) == 3:
                v = v.rearrange("p (a b c) -> p a b c", a=free_shape[0], b=free_shape[1])
            return v

        try:
          if True:
              carve_reset()
              gbc = [carve([DM], F32), carve([DM], F32)]
              Tgbc = Tk("gbc")
              base1 = aoff[0]
              ones_row = carve([128], F32)
              DMA(ones_row[0:1, :], din["ones_row"].ap(), [], [Tc], nowaw=True)
              cT = carve([8], F32)
              DMA(cT, din["cT"].ap(), [], [Tc], nowaw=True)
              adabT = carve([48], F32)
              DMA(adabT, din["adabT"].ap(), [], [Tc], nowaw=True)
              csil = carve([8, 2], F32)
              Tcs = Tk("csil")
              for dd in range(2):
                  ACT(lambda e: e.activation(out=csil[:, :, dd], in_=cT, func=AF.Silu), [Tc], [Tcs])
              adawt = [carve([8, 512], F32) for i in range(2)]
              Tadaw = [Tk("adaw%d" % i) for i in range(2)]
              adaw_v = din["ada_w"].ap().rearrange("(k p) n -> p k n", p=128)
              PA, TPA = bank()
              for n in range(12):
                  sl = n % 2
                  DMA(adawt[sl], adaw_v[:, :, n * 512:(n + 1) * 512], [], [Tadaw[sl]])
                  for fc in range(4):
                      i = n * 4 + fc
                      for k in range(8):
                          PE(lambda e: e.matmul(PA[:, 2 * i:2 * i + 2], lhsT=adawt[sl][:, k, fc * 128:(fc + 1) * 128],
                                                rhs=csil[:, k, :], start=(k == 0), stop=(k == 7)), [Tcs, Tadaw[sl]], [TPA])
              av = adaT[:, :].rearrange("p (i t) -> p i t", t=2)
              pv_ = PA[:, 0:96].rearrange("p (i t) -> p i t", t=2)
              for dd in range(2):
                  DVE(lambda e: e.tensor_tensor(out=av[:, :, dd], in0=pv_[:, :, dd], in1=adabT, op=ALU.add),
                      [TPA, Tc], [TadaT])
              tmp8 = carve([8], F32)
              Ttmp8 = Tk("tmp8")
              for li, (which, gn) in enumerate(((1, n1g), (4, n2g))):
                  sv = av[:, which * 8:(which + 1) * 8, 0]
                  DVE(lambda e: e.tensor_scalar(out=tmp8, in0=sv, scalar1=1.0, scalar2=None, op0=ALU.add),
                      [TadaT], [Ttmp8])
                  DVE(lambda e: e.tensor_tensor(out=gmod[li][:], in0=tmp8, in1=gn[:], op=ALU.mult),
                      [Ttmp8, Tc], [Tgmod])
              onesq = carve([128], F32)
              Tonesq = Tk("onesq")
              DVE(lambda e: e.memset(onesq, 1.0), [], [Tonesq])
              dg = [carve([128], F32), carve([128], F32)]
              Tdg = [Tk("dg0"), Tk("dg1")]
              for gi, which in enumerate((2, 5)):
                  for k in range(8):
                      d_ = dg[k % 2]
                      DVE(lambda e: e.tensor_scalar(out=d_, in0=ident_f[:], scalar1=adacol(which, k), scalar2=None,
                                                    op0=ALU.mult), [Tc, TadaT], [Tdg[k % 2]])
                      P, TP = bank()
                      PE(lambda e: e.matmul(P[:, 0:128], lhsT=onesq, rhs=d_, start=True, stop=True),
                         [Tonesq, Tdg[k % 2]], [TP])
                      copy_ev(gbc[gi][:, k * 128:(k + 1) * 128], P[:, 0:128], [TP], [Tgbc])
              S.barrier()

              ck("ada")
              carve_reset(base1)
              ones_row = carve([128], F32)
              Tor = Tk("ones_row")
              DMA(ones_row[0:1, :], din["ones_row"].ap(), [], [Tor])
              tab = carve([8], F32)
              Ttab = Tk("tab")
              DVE(lambda e: e.memset(tab[0:64, :], NEG), [], [Ttab])
              DMA(tab[0:32, :], din["rel_bias"].ap(), [], [Ttab])
              r31 = carve([8], F32)
              DMA(r31[0:1, :], din["rel_bias"].ap()[31:32, :], [], [Tor], nowaw=True)
              ohw = carve([RW], F32)
              DMA(ohw[0:33, :], din["ohw"].ap(), [], [Tor], nowaw=True)
              ohc = carve([RC], F32)
              DMA(ohc[0:33, :], din["ohc"].ap(), [], [Tor], nowaw=True)
              fwsb = carve([RW], BF16)
              fcsb = carve([RC], BF16)
              Tfw, Tfc = Tk("fwsb"), Tk("fcsb")
              for (oh, fsb, Tf, R) in ((ohw, fwsb, Tfw, RW), (ohc, fcsb, Tfc, RC)):
                  for c0 in range(0, R, 512):
                      c1 = min(R, c0 + 512)
                      P, TP = bank()
                      PE(lambda e: e.matmul(P[0:8, 0:c1 - c0], lhsT=tab[0:33, 0:8], rhs=oh[0:33, c0:c1],
                                            start=True, stop=True), [Ttab, Tor], [TP])
                      copy_ev(fsb[0:8, c0:c1], P[0:8, 0:c1 - c0], [TP], [Tf])
              DMA(fW_s.ap(), fwsb[0:8, :], [Tfw], [Tscr["fW"]])
              DMA(fC_s.ap(), fcsb[0:8, :], [Tfc], [Tscr["fC"]])
              for i, off in enumerate((1, 129, 513)):
                  src = AP(fW_s, off, [[1, 128], [RW, 8], [1, 128]])
                  DMA(TZ[:, i, :, :], src, [Tscr["fW"]], [TTZ], nowaw=True)
              P, TP = bank()
              PE(lambda e: e.matmul(P[:, 0:8], lhsT=ones_row[0:1, 0:128], rhs=r31[0:1, 0:8], start=True, stop=True),
                 [Tor], [TP])
              ACT(lambda e: e.copy(out=b31c[:], in_=P[:, 0:8]), [TP], [Tb31])
              DVE(lambda e: e.tensor_scalar(out=b31m[:], in0=P[:, 0:8], scalar1=-256.0, scalar2=None, op0=ALU.add),
                  [TP], [Tb31])
              DVE(lambda e: e.memset(n256[:], -256.0), [], [Tb31])
              DVE(lambda e: e.memset(zcol[:], 0.0), [], [Tb31])

              ck("t5")
              DVE(lambda e: e.memset(vsA[:], 1.0), [], [Tvs])
              DVE(lambda e: e.memset(vwA[:], 1.0), [], [Tvw])
              DVE(lambda e: e.memset(kcT[:], 0.0), [], [Tkc])
              DVE(lambda e: e.memset(vcT[:], 0.0), [], [Tvc])
              DVE(lambda e: e.memset(vcA[:], 1.0), [], [TvcA])
              for g in range(2):
                  DMA(vcA[:, :, g, 65:129], din["smapc"].ap(), [], [TvcA])
              S.barrier()

              ck("misc")
              carve_reset(base1)
              stg = carve([16, 128], F32)
              Tstg = Tk("stg")
              pe_c = {"k": carve([16], F32), "v": carve([16], F32)}
              pe_b = {"k": carve([16], BF16), "v": carve([16], BF16)}
              stg2 = carve([64], F32)
              Tstg2 = Tk("stg2")
              for kv in ("k", "v"):
                  DMA(stg, din["w1" + kv].ap().rearrange("(c p) h -> p c h", p=128), [], [Tstg])
                  DVE(lambda e: e.tensor_copy(out=w1[kv][:], in_=stg), [Tstg], [Tw1])
                  DMA(stg2, din["w2" + kv].ap(), [], [Tstg2])
                  DVE(lambda e: e.memset(w2[kv][:], 0.0), [], [Tw1])
                  for g in range(2):
                      DVE(lambda e: e.tensor_copy(out=w2[kv][:, g, 64 * g:64 * g + 64], in_=stg2), [Tstg2], [Tw1])
                  DMA(pe_c[kv], din["pe" + kv].ap(), [], [Tstg2])
                  DVE(lambda e: e.tensor_copy(out=pe_b[kv], in_=pe_c[kv]), [Tstg2], [Tw1])
                  P, TP = bank()
                  for c in range(16):
                      PE(lambda e: e.matmul(P[:, 0:1], lhsT=w1[kv][:, c, :], rhs=pe_b[kv][:, c:c + 1],
                                            start=(c == 0), stop=(c == 15)), [Tw1], [TP])
                  ACT(lambda e: e.copy(out=cb[kv][:], in_=P[:, 0:1]), [TP], [Tw1])
              DMA(stg[:, 0:4, :], din["pool_w"].ap().rearrange("g c d -> c g d"), [], [Tstg])
              DVE(lambda e: e.tensor_copy(out=wpool[:], in_=stg[:, 0:4, :]), [Tstg], [Twpool])
              DMA(stg, din["subk"].ap().rearrange("h p k d -> k (h p) d"), [], [Tstg])
              for cbk in range(4):
                  P, TP = bank()
                  for i in range(4):
                      c = cbk * 4 + i
                      PE(lambda e: e.transpose(out=P[:, i * 128:(i + 1) * 128], in_=stg[:, c, :], identity=ident_f[:]),
                         [Tstg, Tc], [TP])
                  copy_ev(skT[:, cbk * 4:(cbk + 1) * 4, :], P[:, 0:512].rearrange("p (c k) -> p c k", c=4), [TP], [Tsk])
              S.barrier()

              ck("small")
              carve_reset(base1)
              cvf = [carve([8, 512], F32) for i in range(2)]
              cvb = [carve([8, 512], BF16) for i in range(2)]
              Tcvf = [Tk("cvf%d" % i) for i in range(2)]
              Tcvb = [Tk("cvb%d" % i) for i in range(2)]
              cvi = [0]

              def conv(src_v, KC, N, runs, scale_bc=None):
                  for p0 in range(0, N, 512):
                      p1 = min(N, p0 + 512)
                      w = p1 - p0
                      sl = cvi[0] % 2
                      cvi[0] += 1
                      DMA(cvf[sl][:, 0:KC, 0:w], src_v[:, :, p0:p1], [], [Tcvf[sl]])
                      if scale_bc is None:
                          copy_ev(cvb[sl][:, 0:KC, 0:w], cvf[sl][:, 0:KC, 0:w], [Tcvf[sl]], [Tcvb[sl]])
                      else:
                          for k in range(KC):
                              DVE(lambda e: e.tensor_tensor(out=cvb[sl][:, k, 0:w], in0=cvf[sl][:, k, 0:w],
                                                            in1=scale_bc[:, p0:p1], op=ALU.mult),
                                  [Tcvf[sl], Tgbc], [Tcvb[sl]])
                      for (s0, n, dst, d0, Td) in runs:
                          a = max(s0, p0)
                          b = min(s0 + n, p1)
                          if a < b:
                              DMA(dst.ap()[:, :, d0 + a - s0: d0 + b - s0], cvb[sl][:, 0:KC, a - p0:b - p0],
                                  [Tcvb[sl]], [Td], nowaw=True, semtk=Tcvb[sl])

              runs = [(0, 512, winA_s, 0, Tscr["winA"])]
              for h in range(8):
                  pos = 2 * h if h < 4 else 2 * (h - 4) + 1
                  runs.append((512 + 64 * h, 64, winA_s, 512 + 64 * pos, Tscr["winA"]))
              for g in range(2):
                  for dup in range(2):
                      runs.append((1024 + 64 * g, 64, winA_s, 1280 + 128 * g + 64 * dup, Tscr["winA"]))
                      runs.append((1152 + 64 * g, 64, winA_s, 1536 + 128 * g + 64 * dup, Tscr["winA"]))
              runs.append((1280, 128, winA_s, 1024, Tscr["winA"]))
              runs.append((1536, 128, winA_s, 1152, Tscr["winA"]))
              runs.append((1816, 2048, winA_s, 1792, Tscr["winA"]))
              runs.append((1408, 128, winB_s, 0, Tscr["winB"]))
              runs.append((1664, 128, winB_s, 128, Tscr["winB"]))
              runs.append((1792, 24, winB_s, 256, Tscr["winB"]))
              conv(din["w_in"].ap().rearrange("(k p) n -> p k n", p=128), 8, 3864, runs)
              conv(din["wbp"].ap().rearrange("(k p) n -> p k n", p=128), 4, DM, [(0, DM, wbp_s, 0, Tscr["wbp"])])
              conv(din["wba"].ap().rearrange("(k p) n -> p k n", p=128), 4, DM, [(0, DM, wba_s, 0, Tscr["wba"])])
              conv(din["wout"].ap().rearrange("(k p) n -> p k n", p=128), 8, DM, [(0, DM, wout_s, 0, Tscr["wout"])],
                   scale_bc=gbc[0])
              conv(din["wq"].ap().rearrange("(k p) n -> p k n", p=128), 8, 2048, [(0, 2048, wq_s, 0, Tscr["wq"])])
              DMA(winB[:], winB_s.ap(), [Tscr["winB"]], [TwinB])
              S.barrier()

              ck("conv")
              carve_reset(base1)
              if prepass:
                  NPB = 3
                  ublk = [carve([2, DM], F32) for i in range(NPB)]
                  vblk = [carve([2, DM], F32) for i in range(NPB)]
                  uo = [uvu[i][:] for i in range(NPB)]
                  vo = [uvv[i][:] for i in range(NPB)]
                  Tub = [Tk("ublk%d" % i) for i in range(NPB)]
                  Tvb = [Tk("vblk%d" % i) for i in range(NPB)]
                  Tuo = [Tk("uo%d" % i) for i in range(NPB)]
                  Tvo = [Tk("vo%d" % i) for i in range(NPB)]
                  u_v = din["pu"].ap().rearrange("(i j) d -> i j d", j=128)
                  v_v = din["pv"].ap().rearrange("(i j) d -> i j d", j=128)
                  def pre_load(jp):
                      sl = jp % NPB
                      DMA(ublk[sl], u_v[:, 2 * jp:2 * jp + 2, :], [], [Tub[sl]])
                      DMA(vblk[sl], v_v[:, 2 * jp:2 * jp + 2, :], [], [Tvb[sl]])

                  pre_load(0)
                  pre_load(1)
                  for jp in range(64):
                      sl = jp % NPB
                      if jp + 2 < 64:
                          pre_load(jp + 2)
                      for jj in range(2):
                          for hb in range(2):
                              P, TP = bank()
                              for i in range(4):
                                  k = hb * 4 + i
                                  PE(lambda e: e.transpose(out=P[:, i * 128:(i + 1) * 128],
                                                           in_=ublk[sl][:, jj, k * 128:(k + 1) * 128],
                                                           identity=ident_f[:]), [Tub[sl], Tc], [TP])
                              copy_ev(uo[sl][:, jj, hb * 4:(hb + 1) * 4, :],
                                      P[:, 0:512].rearrange("p (k i) -> p k i", k=4), [TP], [Tuo[sl]])
                          POOL(lambda e: e.tensor_tensor(out=vo[sl][:, jj, :], in0=vblk[sl][:, jj, :], in1=gbc[1],
                                                         op=ALU.mult), [Tvb[sl], Tgbc], [Tvo[sl]])
                      DMA(uT_s.ap()[jp], uo[sl], [Tuo[sl]], [Tscr["uT"]], nowaw=True, semtk=Tuo[sl])
                      DMA(v_s.ap()[jp], vo[sl], [Tvo[sl]], [Tscr["v"]], nowaw=True, semtk=Tvo[sl])
              S.barrier()
          ck("pre")
          S.track_all = False
          def dump(name, ap_, tk, shape, dt=F32):
              if not debug:
                  return
              if not isinstance(ap_, AP):
                  ap_ = ap_[:]
              d = nc.dram_tensor("dbg_" + name, list(shape), dt, kind="ExternalOutput")
              dbg[name] = d
              DMA(d.ap(), ap_, [tk], [Tout], nowaw=True, track=True)

          def v8_(t_):
              a_ = t_[:]
              if len(a_.shape) == 4:
                  a_ = a_.rearrange("p a b c -> p (a b c)")
              else:
                  a_ = a_.rearrange("p a b -> p (a b)")
              return a_.rearrange("p (k n) -> p k n", k=8)

          wsl = [ws[i][:] for i in range(NWS)] + [v8_(uvu[i]) for i in range(3)] + [v8_(uvv[i]) for i in range(3)]
          Twsl = list(Tws) + [Tk("wsa%d" % i) for i in range(6)]
          NWSL = len(wsl)

          def ws_next():
              i = wsi[0] % NWSL
              wsi[0] += 1
              return wsl[i], Twsl[i]

          def load_x(G_):
              for tt_ in range(2):
                  DMA(xin[tt_][:], din["x"].ap()[G_ * TG + tt_ * 128:G_ * TG + (tt_ + 1) * 128, :], [], [Txin[tt_]])

          load_x(0)

          def norm_both(li, dstT, Tdst, xn2, Txn2, ss2, Tss2):
              shift_which = 0 if li == 0 else 3
              srcs = [((xin[tt], Txin[tt]) if li == 0 else (xt[tt], Txt[tt])) for tt in range(2)]
              for tt in range(2):
                  xs, Txs = srcs[tt]
                  ACT(lambda e: e.activation(out=xn2[tt][:, :], in_=xs[:], func=AF.Square, accum_out=ss2[tt][:, 0:1]),
                      [Txs], [Txn2[tt], Tss2[tt]])
              for tt in range(2):
                  DVE(lambda e: e.tensor_scalar(out=ss2[tt][:, 1:2], in0=ss2[tt][:, 0:1], scalar1=1.0 / DM, scalar2=EPS,
                                                op0=ALU.mult, op1=ALU.add), [Tss2[tt]], [Tss2[tt]])
              for tt in range(2):
                  ACT(lambda e: e.activation(out=ss2[tt][:, 2:3], in_=ss2[tt][:, 1:2], func=AF.Sqrt), [Tss2[tt]], [Tss2[tt]])
              for tt in range(2):
                  DVE(lambda e: e.reciprocal(out=ss2[tt][:, 3:4], in_=ss2[tt][:, 2:3]), [Tss2[tt]], [Tss2[tt]])
              for tt in range(2):
                  xs, Txs = srcs[tt]
                  DVE(lambda e: e.tensor_scalar(out=xn2[tt][:, :], in0=xs[:], scalar1=ss2[tt][:, 3:4], scalar2=None,
                                                op0=ALU.mult), [Txs, Tss2[tt]], [Txn2[tt]])
              banks_ = [bank() for _ in range(2)]
              for tt in range(2):
                  P, TP = banks_[tt]
                  Pb = bfv(P)
                  for k in range(8):
                      PE(lambda e: e.transpose(out=Pb[:, k * 128:(k + 1) * 128], in_=xn2[tt][:, k * 128:(k + 1) * 128],
                                               identity=ident_b[:]), [Txn2[tt], Tc], [TP])
              for tt in range(2):
                  P, TP = banks_[tt]
                  Pb = bfv(P)
                  for k in range(8):
                      o = dstT[:, k, tt * 128:(tt + 1) * 128]
                      i_ = Pb[:, k * 128:(k + 1) * 128]
                      gm = gmod[li][:, k:k + 1]
                      sh = adacol(shift_which, k)
                      EV(lambda e, a: (e.activation(out=o, in_=i_, func=AF.Identity, scale=gm, bias=sh) if a else
                                       e.tensor_scalar(out=o, in0=i_, scalar1=gm, scalar2=sh, op0=ALU.mult, op1=ALU.add)),
                         [TP, Tgmod, TadaT], [Tdst])

          def load_ws(scr, Tsc, c0):
              wt, Twt = ws_next()
              DMA(wt, scr.ap()[:, :, c0:c0 + 256], [Tsc], [Twt])
              return wt, Twt

          PFW = 3
          for G in range(NG):
              g0 = G * TG
              carve_reset()
              hT = carve([8, TG], BF16); ThT = Tk("hT")
              qT = carve([8, TG], BF16); TqT = Tk("qT")
              zp = carve([4, 272], F32); Tzp = Tk("zp")
              ptmp = [carve([272], F32), carve([272], F32)]; Tptmp = [Tk("pt0"), Tk("pt1")]
              pooled = carve([4, TG], BF16); Tpooled = Tk("pooled")
              ypT = carve([4, TG], BF16); TypT = Tk("ypT")
              yaT = carve([4, TG], BF16); TyaT = Tk("yaT")
              mixT = carve([8, TG], BF16); TmixT = Tk("mixT")
              Xk = [carve([273], BF16), carve([273], BF16)]
              Xv = [carve([273], BF16), carve([273], BF16)]
              TX = Tk("X")
              gtt = [carve([24], F32), carve([24], F32)]; Tgt = [Tk("gt0"), Tk("gt1")]
              OC = carve([8, 129], F32); TOC = Tk("OC")
              Os = carve([8, 65], F32); TOs = Tk("Os")
              Ow = carve([8, 65], F32); TOw = Tk("Ow")
              NPT = 4
              PTb = [carve([512], BF16) for _ in range(NPT)]; TPT = [Tk("PT%d" % i) for i in range(NPT)]
              pti = [0]
              TCt = [[carve([8, 128], BF16) for _ in range(2)] for _ in range(2)]
              xn2 = [carve([DM], BF16), carve([DM], BF16)]; Txn2 = [Tk("xn0"), Tk("xn1")]
              ss2 = [carve([4], F32), carve([4], F32)]; Tss2 = [Tk("ss0"), Tk("ss1")]
              sq = carve([TG], BF16); Tsq = Tk("sq")
              r1 = carve([TG], F32); Tr1 = Tk("r1")
              sqs = [sq, carve([TG], BF16)]; Tsqs = [Tsq, Tk("sq1")]
              r1s = [r1, carve([TG], F32)]; Tr1s = [Tr1, Tk("r11")]
              hid = {"k": [carve([16], BF16), carve([16], BF16)], "v": [carve([16], BF16), carve([16], BF16)]}
              Thid = Tk("hid")
              sc64 = [carve([64], F32), carve([64], F32)]; score2 = [carve([64], F32), carve([64], F32)]
              selb = [carve([128], BF16), carve([128], BF16)]
              m8 = [carve([16], F32), carve([16], F32)]; Tsel = [Tk("sel0"), Tk("sel1")]
              selT = [carve([128], BF16), carve([128], BF16)]; TselT = [Tk("selT0"), Tk("selT1")]
              cf = carve([6, 8], F32); Tcf = Tk("cf")
              yacc = carve([8, 64], F32); ytmp = carve([8, 64], F32); Tyacc = Tk("yacc")
              ya = carve([512], BF16); Tya = Tk("ya")
              mgt = [carve([TG], BF16) for _ in range(2)]; Tmgt = [Tk("mg0"), Tk("mg1")]
              t1 = carve([TG], F32); Tt1 = Tk("t1")
              tmp16 = carve([16], F32)

              if G == 0:
                  pass
              if G == 0:
                  zph = sb("zph", [128, 4, 16], F32); Tzph = Tk("zph")
                  Xh = {("k", 0): sb("Xhk0", [128, 17], BF16), ("k", 1): sb("Xhk1", [128, 17], BF16),
                        ("v", 0): sb("Xhv0", [128, 17], BF16), ("v", 1): sb("Xhv1", [128, 17], BF16)}
                  TXh = Tk("Xh")
                  DVE(lambda e: e.memset(zph[:], 0.0), [], [Tzph])
                  for kk in Xh:
                      DVE(lambda e: e.memset(Xh[kk][:], 0.0), [], [TXh])
              DVE(lambda e: e.tensor_copy(out=zp[:, :, 0:16], in_=zph[:]), [Tzph], [Tzp])
              for kv, XX in (("k", Xk), ("v", Xv)):
                  for g in range(2):
                      POOL(lambda e: e.tensor_copy(out=XX[g][:, 0:17], in_=Xh[(kv, g)][:]), [TXh], [TX])
                      POOL(lambda e: e.memset(XX[g][:, 272:273], 0.0), [], [TX])

              DVE(lambda e: e.memset(qT[64:128, 0:4, :], 0.0), [], [TqT])
              DVE(lambda e: e.memset(qT[0:64, 4:8, :], 0.0), [], [TqT])
              for g in range(2):
                  DVE(lambda e: e.memset(selb[g][:, 64:128], 0.0), [], [Tsel[g]])
              slc = G % 2
              DMA(Cstt[slc][:], din["Cst"].ap()[:, 2 * G:2 * G + 2, :], [], [TCst[slc]])
              for tt_ in range(2):
                  ti_ = 2 * G + tt_
                  for m_ in ([0] if ti_ < 16 else [0, 1]):
                      src = AP(fC_s, 128 * (ti_ - 16 * m_) + 37, [[16, 128], [RC, 8], [1, 128]])
                      DMA(TCt[ti_ % 2][m_], src, [Tscr["fC"]], [TTC[ti_ % 2]], nowaw=(m_ > 0), track=True)
              wq_ = [load_ws(winA_s, Tscr["winA"], p_ * 256) for p_ in range(PFW)]
              DMA(wbb[0][:], wbp_s.ap()[:, :, 0:512], [Tscr["wbp"]], [Twbb[0]])
              DMA(wbb[1][:], wba_s.ap()[:, :, 0:512], [Tscr["wba"]], [Twbb[1]])
              norm_both(0, hT, ThT, xn2, Txn2, ss2, Tss2)
              if G == 0:
                  dump("hT", hT, ThT, [128, 8, TG], BF16)

              ck("A")
              hn_i = [0]

              def headnorm(P, TP, ncols, gcol, mult, epst, dst, Tdst):
                  ii = hn_i[0] % 2
                  hn_i[0] += 1
                  sq_, Tsq_, r1_, Tr1_ = sqs[ii], Tsqs[ii], r1s[ii], Tr1s[ii]
                  ACT(lambda e: e.activation(out=sq_[:, 0:ncols], in_=P[:, 0:ncols], func=AF.Square), [TP], [Tsq_])
                  P2, TP2 = bank()
                  PE(lambda e: e.matmul(P2[:, 0:ncols], lhsT=BD[:], rhs=sq_[:, 0:ncols], start=True, stop=True),
                     [Tsq_, Tc], [TP2])
                  DVE(lambda e: e.tensor_scalar(out=r1_[:, 0:ncols], in0=P2[:, 0:ncols], scalar1=mult, scalar2=epst,
                                                op0=ALU.mult, op1=ALU.add), [TP2], [Tr1_])
                  ACT(lambda e: e.activation(out=r1_[:, 0:ncols], in_=r1_[:, 0:ncols], func=AF.Sqrt), [Tr1_], [Tr1_])
                  DVE(lambda e: e.reciprocal(out=r1_[:, 0:ncols], in_=r1_[:, 0:ncols]), [Tr1_], [Tr1_])
                  if isinstance(dst, tuple):
                      for hh, d_ in enumerate(dst):
                          prr = slice(64 * hh, 64 * hh + 64)
                          DVE(lambda e: e.scalar_tensor_tensor(out=d_, in0=P[prr, 0:ncols], scalar=gcol[prr, :],
                                                               in1=r1_[prr, 0:ncols], op0=ALU.mult, op1=ALU.mult),
                              [TP, Tr1_, Tc], [Tdst])
                  else:
                      DVE(lambda e: e.scalar_tensor_tensor(out=dst, in0=P[:, 0:ncols], scalar=gcol, in1=r1_[:, 0:ncols],
                                                           op0=ALU.mult, op1=ALU.mult), [TP, Tr1_, Tc], [Tdst])

              for pc in range(7):
                  wt, Twt = wq_.pop(0)
                  if pc + PFW < 7:
                      wq_.append(load_ws(winA_s, Tscr["winA"], (pc + PFW) * 256))
                  for i in range(2):
                      ch = pc * 2 + i
                      P, TP = bank()
                      for k in range(8):
                          PE(lambda e: e.matmul(P[:, 0:TG], lhsT=wt[:, k, i * 128:(i + 1) * 128], rhs=hT[:, k, :],
                                                start=(k == 0), stop=(k == 7)), [Twt, ThT], [TP])
                      if ch < 4:
                          ACT(lambda e: e.copy(out=zp[:, ch, 16:272], in_=P[:, 0:TG]), [TP], [Tzp])
                      elif ch < 8:
                          headnorm(P, TP, TG, gq[:, 0:1], 1.0, 64 * EPS, (qT[0:64, ch - 4, :], qT[64:128, ch, :]), TqT)
                      elif ch == 8:
                          headnorm(P, TP, TG, gk[:, 1:2], 1.0 / 64, EPS, ksT[:, g0:g0 + TG], Tks)
                      elif ch == 9:
                          headnorm(P, TP, TG, gk[:, 2:3], 1.0 / 64, EPS, kwT[:, g0:g0 + TG], Tkw)
                      else:
                          XX = Xk if ch < 12 else Xv
                          g = (ch - 10) % 2
                          ACT(lambda e: e.copy(out=XX[g][0:64, 17:273], in_=P[0:64, 0:TG]), [TP], [TX])
                          DVE(lambda e: e.tensor_copy(out=XX[g][64:128, 16:272], in_=P[64:128, 0:TG]), [TP], [TX])
              for tt in range(2):
                  ti = 2 * G + tt
                  P, TP = bank()
                  for k in range(8):
                      PE(lambda e: e.matmul(P[:, 0:NB], lhsT=hT[:, k, tt * 128:(tt + 1) * 128], rhs=winB[:, k, :],
                                            start=(k == 0), stop=(k == 7)), [ThT, TwinB], [TP])
                  ACT(lambda e: e.copy(out=vsA[:, ti, :, 0:64], in_=P[:, 0:128].rearrange("p (g d) -> p g d", g=2)),
                      [TP], [Tvs])
                  DVE(lambda e: e.tensor_copy(out=vwA[:, ti, :, 0:64],
                                              in_=P[:, 128:256].rearrange("p (g d) -> p g d", g=2)), [TP], [Tvw])
                  ACT(lambda e: e.activation(out=gtt[tt][:, :], in_=P[:, 256:280], func=AF.Sigmoid), [TP], [Tgt[tt]])
              if G == 0:
                  dump("qT", qT, TqT, [128, 8, TG], BF16)
                  dump("ksT", ksT[:, 0:TG], Tks, [128, TG], BF16)

              ck("B")
              n_lo = 0 if G == 0 else 16 * G - 1
              n_hi = 16 * G + 15
              nn = n_hi - n_lo
              col0 = 17 + 16 * n_lo - g0
              for kv, XX in (("k", Xk), ("v", Xv)):
                  for g in range(2):
                      P, TP = bank()
                      for c in range(16):
                          PE(lambda e: e.matmul(P[:, 0:nn], lhsT=w1[kv][:, c, :],
                                                rhs=strided(XX[g], col0 + 2 * c, 16, nn),
                                                start=(c == 0), stop=(c == 15)), [Tw1, TX], [TP])
                      ACT(lambda e: e.activation(out=hid[kv][g][:, 0:nn], in_=P[:, 0:nn], func=AF.Gelu_apprx_tanh,
                                                 bias=cb[kv][:, 0:1]), [TP, Tw1], [Thid])
              P, TP = bank()
              for g in range(2):
                  PE(lambda e: e.matmul(P[:, 0:nn], lhsT=w2["k"][:, g, :], rhs=hid["k"][g][:, 0:nn],
                                        start=(g == 0), stop=(g == 1)), [Tw1, Thid], [TP])
              headnorm(P, TP, nn, gk[:, 0:1], 1.0 / 64, EPS, kcT[:, n_lo:n_hi], Tkc)
              P, TP = bank()
              for g in range(2):
                  PE(lambda e: e.matmul(P[:, 0:nn], lhsT=w2["v"][:, g, :], rhs=hid["v"][g][:, 0:nn],
                                        start=(g == 0), stop=(g == 1)), [Tw1, Thid], [TP])
              ACT(lambda e: e.copy(out=vcT[:, n_lo:n_hi], in_=P[:, 0:nn]), [TP], [Tvc])
              P, TP = bank()
              Pb = bfv(P)
              for m in range(2):
                  PE(lambda e: e.transpose(out=Pb[:, m * 128:(m + 1) * 128], in_=vcT[:, m * 128:(m + 1) * 128],
                                           identity=ident_b[:]), [Tvc, Tc], [TP])
              for m in range(2):
                  DVE(lambda e: e.tensor_copy(out=vcA[:, m, :, 0:64],
                                              in_=Pb[:, m * 128:(m + 1) * 128].rearrange("p (g d) -> p g d", g=2)),
                      [TP], [TvcA])
              DVE(lambda e: e.tensor_copy(out=zph[:], in_=zp[:, :, 256:272]), [Tzp], [Tzph])
              for kv, XX in (("k", Xk), ("v", Xv)):
                  for g in range(2):
                      POOL(lambda e: e.tensor_copy(out=Xh[(kv, g)][:], in_=XX[g][:, 256:273]), [TX], [TXh])
              if G == 0:
                  dump("kcT", kcT, Tkc, [128, 256], BF16)
                  dump("vcT", vcT, Tvc, [128, 256], BF16)

              ck("C")
              for tt in range(2):
                  ti = 2 * G + tt
                  qsl = slice(tt * 128, (tt + 1) * 128)
                  ms = [0] if ti < 16 else [0, 1]
                  tcs = ti % 2
                  accs = {}

                  def make_tasks(kind, g, j):
                      h = 4 * g + j
                      if kind == "cmp":
                          return [dict(kind=kind, g=g, j=j, h=h, bt=list(ms), isnear=True, first=True, last=True)]
                      if kind == "sel":
                          kts = list(range(ti + 1))
                          near = [kt for kt in kts if ti - kt <= 1]
                          far = [kt for kt in kts if ti - kt >= 2]
                      else:
                          kts = list(range(max(0, ti - 4), ti + 1))
                          near = [kt for kt in kts if ti - kt in (0, 1, 4)]
                          far = [kt for kt in kts if ti - kt in (2, 3)]
                      batches = [(far[i:i + 4], False) for i in range(0, len(far), 4)] + [(near, True)]
                      out_ = []
                      for bi_, (bt, isnear) in enumerate(batches):
                          out_.append(dict(kind=kind, g=g, j=j, h=h, bt=bt, isnear=isnear, first=(bi_ == 0),
                                           last=(bi_ == len(batches) - 1)))
                      return out_

                  def emit_scores(tk_):
                      kind, g, h, bt, isnear = tk_["kind"], tk_["g"], tk_["h"], tk_["bt"], tk_["isnear"]
                      P, TP = bank("sc")
                      if kind == "cmp":
                          for m in bt:
                              PE(lambda e: e.matmul(P[:, m * 128:(m + 1) * 128], lhsT=kcT[:, m * 128:(m + 1) * 128],
                                                    rhs=qT[:, h, qsl], start=True, stop=False), [Tkc, TqT], [TP])
                              PE(lambda e: e.matmul(P[:, m * 128:(m + 1) * 128], lhsT=Jb[:], rhs=TCt[tcs][m][:, h, :],
                                                    start=False, stop=True), [Tc, TTC[tcs]], [TP])
                          bias = zcol[:, 0:1]
                      else:
                          if kind == "sel":
                              KT, TK_ = ksT, Tks
                              fbias, nbias = b31m[:, h:h + 1], n256[:, 0:1]
                          else:
                              KT, TK_ = kwT, Tkw
                              fbias, nbias = b31c[:, h:h + 1], zcol[:, 0:1]
                          for i, kt in enumerate(bt):
                              rg = slice(i * 128, (i + 1) * 128)
                              ksl = slice(kt * 128, (kt + 1) * 128)
                              only = (kind == "win") and not isnear
                              PE(lambda e: e.matmul(P[:, rg], lhsT=KT[:, ksl], rhs=qT[:, h, qsl], start=True, stop=only),
                                 [TK_, TqT], [TP])
                              if kind == "sel":
                                  PE(lambda e: e.matmul(P[:, rg], lhsT=Eall[:, ksl], rhs=selT[g][:, :],
                                                        start=False, stop=(not isnear)), [Tc, TselT[g]], [TP])
                              if isnear:
                                  zi = {0: 0, 1: 1, 4: 2}[ti - kt]
                                  PE(lambda e: e.matmul(P[:, rg], lhsT=Jb[:], rhs=TZ[:, zi, h, :], start=False, stop=True),
                                     [Tc, TTZ], [TP])
                          bias = nbias if isnear else fbias
                      nb_ = len(bt)
                      pi_ = pti[0] % NPT
                      pti[0] += 1
                      if kind == "cmp" or (kind == "win" and isnear):
                          ACT(lambda e: e.activation(out=PTb[pi_][:, 0:nb_ * 128], in_=P[:, 0:nb_ * 128], func=AF.Exp),
                              [TP], [TPT[pi_]])
                      else:
                          ACT(lambda e: e.activation(out=PTb[pi_][:, 0:nb_ * 128], in_=P[:, 0:nb_ * 128], func=AF.Exp,
                                                     bias=bias), [TP, Tb31], [TPT[pi_]])
                      tk_["pt"] = pi_

                  def emit_pv(tk_):
                      kind, g, h, bt = tk_["kind"], tk_["g"], tk_["h"], tk_["bt"]
                      key = (kind, h)
                      if tk_["first"]:
                          accs[key] = bank("acc")
                      P2, TP2 = accs[key]
                      pi_ = tk_["pt"]
                      for i, kt in enumerate(bt):
                          st_ = tk_["first"] and i == 0
                          sp_ = tk_["last"] and i == len(bt) - 1
                          if kind == "cmp":
                              PE(lambda e: e.matmul(P2[:, 0:129], lhsT=PTb[pi_][:, kt * 128:(kt + 1) * 128],
                                                    rhs=vcA[:, kt, g, :], start=st_, stop=sp_), [TPT[pi_], TvcA], [TP2])
                          else:
                              VA, TV = (vsA, Tvs) if kind == "sel" else (vwA, Tvw)
                              PE(lambda e: e.matmul(P2[:, 0:65], lhsT=PTb[pi_][:, i * 128:(i + 1) * 128],
                                                    rhs=VA[:, kt, g, :], start=st_, stop=sp_), [TPT[pi_], TV], [TP2])
                      if tk_["last"]:
                          if kind == "cmp":
                              ACT(lambda e: e.copy(out=OC[:, h, :], in_=P2[:, 0:129]), [TP2], [TOC])
                          elif kind == "sel":
                              ACT(lambda e: e.copy(out=Os[:, h, :], in_=P2[:, 0:65]), [TP2], [TOs])
                          else:
                              ACT(lambda e: e.copy(out=Ow[:, h, :], in_=P2[:, 0:65]), [TP2], [TOw])

                  LAGA = 3

                  def run_tasks(tasks):
                      for i_ in range(len(tasks) + LAGA):
                          if i_ < len(tasks):
                              emit_scores(tasks[i_])
                          if i_ >= LAGA:
                              emit_pv(tasks[i_ - LAGA])

                  run_tasks([t_ for g in range(2) for j in range(4) for t_ in make_tasks("cmp", g, j)])
                  ck("D1")
                  DVE(lambda e: e.tensor_scalar(out=cf[:, 0, :], in0=OC[:, :, 64], scalar1=1e-30, scalar2=None,
                                                op0=ALU.max), [TOC], [Tcf])
                  DVE(lambda e: e.reciprocal(out=cf[:, 1, :], in_=cf[:, 0, :]), [Tcf], [Tcf])
                  for g in range(2):
                      sc_, s2_, m8_, sb_ = sc64[g], score2[g], m8[g], selb[g]
                      DVE(lambda e: e.tensor_scalar(out=sc_[:, :], in0=OC[:, 4 * g, 65:129],
                                                    scalar1=cf[:, 1, 4 * g:4 * g + 1], scalar2=None, op0=ALU.mult),
                          [TOC, Tcf], [Tsel[g]])
                      for j in range(1, 4):
                          DVE(lambda e: e.scalar_tensor_tensor(out=sc_[:, :], in0=OC[:, 4 * g + j, 65:129],
                                                               scalar=cf[:, 1, 4 * g + j:4 * g + j + 1], in1=sc_[:, :],
                                                               op0=ALU.mult, op1=ALU.add), [TOC, Tcf, Tsel[g]], [Tsel[g]])
                      DVE(lambda e: e.tensor_tensor(out=sc_[:, :], in0=sc_[:, :], in1=Cstt[slc][:, tt, :], op=ALU.add),
                          [Tsel[g], TCst[slc]], [Tsel[g]])
                      DVE(lambda e: e.max(out=m8_[:, 0:8], in_=sc_[:, :]), [Tsel[g]], [Tsel[g]])
                      DVE(lambda e: e.match_replace(out=s2_[:, :], in_to_replace=m8_[:, 0:8], in_values=sc_[:, :],
                                                    imm_value=-1e30), [Tsel[g]], [Tsel[g]])
                      DVE(lambda e: e.max(out=m8_[:, 8:16], in_=s2_[:, :]), [Tsel[g]], [Tsel[g]])
                      DVE(lambda e: e.tensor_scalar(out=sb_[:, 0:64], in0=sc_[:, :], scalar1=m8_[:, 15:16], scalar2=256.0,
                                                    op0=ALU.is_ge, op1=ALU.mult), [Tsel[g]], [Tsel[g]])
                  if G == 0 and tt == 0:
                      dump("OC", OC, TOC, [128, 8, 129], F32)
                  ck("D2")
                  run_tasks([t_ for g in range(2) for j in range(4) for t_ in make_tasks("win", g, j)])
                  for g in range(2):
                      P, TP = bank("sc")
                      Pb = bfv(P)
                      PE(lambda e: e.transpose(out=Pb[:, 0:128], in_=selb[g][:, :], identity=ident_b[:]),
                         [Tsel[g], Tc], [TP])
                      ACT(lambda e: e.copy(out=selT[g][:, :], in_=Pb[:, 0:128]), [TP], [TselT[g]])
                  run_tasks([t_ for g in range(2) for j in range(4) for t_ in make_tasks("sel", g, j)])
                  ck("D3")
                  gtv = gtt[tt].rearrange("p (h b) -> p h b", b=3)
                  for bi, (O_, TO_) in enumerate(((OC, TOC), (Os, TOs), (Ow, TOw))):
                      DVE(lambda e: e.tensor_scalar(out=cf[:, 2, :], in0=O_[:, :, 64], scalar1=1e-30, scalar2=None,
                                                    op0=ALU.max), [TO_], [Tcf])
                      DVE(lambda e: e.reciprocal(out=cf[:, 2, :], in_=cf[:, 2, :]), [Tcf], [Tcf])
                      DVE(lambda e: e.tensor_tensor(out=cf[:, 3 + bi, :], in0=cf[:, 2, :], in1=gtv[:, :, bi], op=ALU.mult),
                          [Tcf, Tgt[tt]], [Tcf])
                  for bi, (O_, TO_) in enumerate(((OC, TOC), (Os, TOs), (Ow, TOw))):
                      cfb = cf[:, 3 + bi, :].unsqueeze(2).broadcast_to([128, 8, 64])
                      dst = yacc if bi == 0 else ytmp
                      DVE(lambda e: e.tensor_tensor(out=dst, in0=O_[:, :, 0:64], in1=cfb, op=ALU.mult),
                          [TO_, Tcf], [Tyacc])
                      if bi == 1:
                          DVE(lambda e: e.tensor_tensor(out=yacc, in0=yacc, in1=ytmp, op=ALU.add), [Tyacc], [Tyacc])
                      if bi == 2:
                          DVE(lambda e: e.tensor_tensor(out=ya.rearrange("p (h d) -> p h d", h=8), in0=yacc, in1=ytmp,
                                                        op=ALU.add), [Tyacc], [Tya])
                  if G == 0 and tt == 0:
                      dump("ya", ya, Tya, [128, 512], BF16)
                      dump("Os", Os, TOs, [128, 8, 65], F32)
                      dump("Ow", Ow, TOw, [128, 8, 65], F32)
                  P, TP = bank()
                  Pb = bfv(P)
                  for c in range(4):
                      PE(lambda e: e.transpose(out=Pb[:, c * 128:(c + 1) * 128], in_=ya[:, c * 128:(c + 1) * 128],
                                               identity=ident_b[:]), [Tya, Tc], [TP])
                  ACT(lambda e: e.copy(out=yaT[:, :, qsl], in_=Pb[:, 0:512].rearrange("p (c t) -> p c t", c=4)),
                      [TP], [TyaT])

              ck("D")
              for ci in range(4):
                  wdw = 2 ** (ci + 1)
                  a_ap, Ta = zp[:, ci, :], Tzp
                  for stp in range(ci + 1):
                      sh = 2 ** stp
                      lo = 2 ** (stp + 1) - 1
                      d_ap, Td = ptmp[stp % 2], Tptmp[stp % 2]
                      DVE(lambda e: e.tensor_tensor(out=d_ap[:, lo:272], in0=a_ap[:, lo:272], in1=a_ap[:, lo - sh:272 - sh],
                                                    op=ALU.add), [Ta], [Td])
                      a_ap, Ta = d_ap, Td
                  DVE(lambda e: e.scalar_tensor_tensor(out=pooled[:, ci, :], in0=a_ap[:, 16:272], scalar=1.0 / wdw,
                                                       in1=zp[:, ci, 16:272], op0=ALU.mult, op1=ALU.subtract),
                      [Ta, Tzp], [Tpooled])
                  if G == 0:
                      DVE(lambda e: e.tensor_tensor(out=tmp16[:, :], in0=a_ap[:, 16:32], in1=invc16[:, ci, :], op=ALU.mult),
                          [Ta, Tc], [Tt1])
                      DVE(lambda e: e.tensor_tensor(out=pooled[:, ci, 0:16], in0=tmp16[:, :], in1=zp[:, ci, 16:32],
                                                    op=ALU.subtract), [Tt1, Tzp], [Tpooled])
                  P, TP = bank()
                  PE(lambda e: e.matmul(P[:, 0:TG], lhsT=wpool[:, ci, :], rhs=pooled[:, ci, :], start=True, stop=True),
                     [Twpool, Tpooled], [TP])
                  ACT(lambda e: e.activation(out=ypT[:, ci, :], in_=P[:, 0:TG], func=AF.Copy, scale=pscale[:, ci:ci + 1]),
                      [TP, Tc], [TypT])
              if G == 0:
                  dump("ypT", ypT, TypT, [128, 4, TG], BF16)
                  dump("yaT", yaT, TyaT, [128, 4, TG], BF16)
              def load_mw(half_, pr2_):
                  return [load_ws(winA_s, Tscr["winA"], 1792 + gi_ * 1024 + half_ * 512 + pr2_ * 256) for gi_ in range(2)]

              mw_req = [(h_, p_) for h_ in range(2) for p_ in range(2)]
              mw_q = [load_mw(0, 0)]
              wo_all = None
              for half in range(2):
                  if half == 1:
                      DMA(wbb[0][:], wbp_s.ap()[:, :, half * 512:(half + 1) * 512], [Tscr["wbp"]], [Twbb[0]])
                      DMA(wbb[1][:], wba_s.ap()[:, :, half * 512:(half + 1) * 512], [Tscr["wba"]], [Twbb[1]])
                  for pr2 in range(2):
                      mw = mw_q.pop(0)
                      idx_ = half * 2 + pr2
                      if idx_ + 1 < 4:
                          mw_q.append(load_mw(*mw_req[idx_ + 1]))
                      else:
                          wo_all = [[load_ws(wout_s, Tscr["wout"], h_ * 512 + i_ * 256) for i_ in range(2)]
                                    for h_ in range(2)]
                      for i in range(2):
                          mc = half * 4 + pr2 * 2 + i
                          lc = pr2 * 2 + i
                          for gi in range(2):
                              P, TP = bank()
                              for k in range(8):
                                  PE(lambda e: e.matmul(P[:, 0:TG], lhsT=mw[gi][0][:, k, i * 128:(i + 1) * 128],
                                                        rhs=hT[:, k, :], start=(k == 0), stop=(k == 7)),
                                     [mw[gi][1], ThT], [TP])
                              ACT(lambda e: e.activation(out=mgt[gi][:, :], in_=P[:, 0:TG], func=AF.Sigmoid),
                                  [TP], [Tmgt[gi]])
                          Pa, TPa = bank()
                          for c in range(4):
                              PE(lambda e: e.matmul(Pa[:, 0:TG], lhsT=wbb[0][:, c, lc * 128:(lc + 1) * 128],
                                                    rhs=ypT[:, c, :], start=(c == 0), stop=(c == 3)),
                                 [Twbb[0], TypT], [TPa])
                          Pb_, TPb = bank()
                          for c in range(4):
                              PE(lambda e: e.matmul(Pb_[:, 0:TG], lhsT=wbb[1][:, c, lc * 128:(lc + 1) * 128],
                                                    rhs=yaT[:, c, :], start=(c == 0), stop=(c == 3)),
                                 [Twbb[1], TyaT], [TPb])
                          DVE(lambda e: e.tensor_tensor(out=t1[:, :], in0=Pa[:, 0:TG], in1=mgt[0][:, :], op=ALU.mult),
                              [TPa, Tmgt[0]], [Tt1])
                          DVE(lambda e: e.tensor_tensor(out=sq[:, :], in0=Pb_[:, 0:TG], in1=mgt[1][:, :], op=ALU.mult),
                              [TPb, Tmgt[1]], [Tsq])
                          DVE(lambda e: e.tensor_tensor(out=mixT[:, mc, :], in0=t1[:, :], in1=sq[:, :], op=ALU.add),
                              [Tt1, Tsq], [TmixT])
              if G == 0:
                  dump("mixT", mixT, TmixT, [128, 8, TG], BF16)
              ck("E1")
              for half in range(2):
                  wo = wo_all[half]
                  for tt in range(2):
                      P, TP = bank()
                      for i in range(2):
                          for k in range(8):
                              PE(lambda e: e.matmul(P[:, i * 256:(i + 1) * 256], lhsT=mixT[:, k, tt * 128:(tt + 1) * 128],
                                                    rhs=wo[i][0][:, k, :], start=(k == 0), stop=(k == 7)),
                                 [wo[i][1], TmixT], [TP])
                      DVE(lambda e: e.tensor_tensor(out=xt[tt][:, half * 512:(half + 1) * 512], in0=P[:, 0:512],
                                                    in1=xin[tt][:, half * 512:(half + 1) * 512], op=ALU.add),
                          [TP, Txin[tt]], [Txt[tt]])
              if G + 1 < NG:
                  load_x(G + 1)
              if G == 0:
                  dump("x1", xt[0][:], Txt[0], [128, DM], F32)
              ck("E")
              wq_ = [load_ws(wq_s, Tscr["wq"], p_ * 256) for p_ in range(PFW)]
              norm_both(1, h2T, Th2, xn2, Txn2, ss2, Tss2)
              if G == 0:
                  dump("h2T", h2T[:], Th2, [128, 8, TG], BF16)
              S.barrier()

              ck("F")
              carve_reset()
              pqT = carve([16, TG], BF16); Tpq = Tk("pqT")
              scs = carve([16, 128], F32); Tscs = Tk("scs")
              m1 = carve([16, 16], F32); i1 = carve([16, 16], U32); i1f = carve([16, 16], F32); Tm1 = Tk("m1")
              tmpr = carve([16, 128], F32)
              cand = carve([8, 16, 16], F32); Tcand = Tk("cand")
              ctmp = carve([8, 256], F32)
              eq = carve([128, 16], F32); Teq = Tk("eq")
              ts_ = carve([8, 16], F32); tj = carve([8, 16], U32); ta = carve([8, 16], U32); tb_ = carve([8, 16], U32)
              af_ = carve([128], F32); bf_ = carve([128], F32)
              If_ = carve([128], F32); Jf_ = carve([128], F32); ex = carve([8, 16], F32); gf = carve([8, 16], F32)
              s8 = carve([8], F32)
              Tts = Tk("ts")
              for pc in range(8):
                  wt, Twt = wq_.pop(0)
                  if pc + PFW < 8:
                      wq_.append(load_ws(wq_s, Tscr["wq"], (pc + PFW) * 256))
                  for i in range(2):
                      c = 2 * pc + i
                      P, TP = bank()
                      for k in range(8):
                          PE(lambda e: e.matmul(P[:, 0:TG], lhsT=wt[:, k, i * 128:(i + 1) * 128], rhs=h2T[:, k, :],
                                                start=(k == 0), stop=(k == 7)), [Twt, Th2], [TP])
                      ACT(lambda e: e.copy(out=pqT[:, c, :], in_=P[:, 0:TG]), [TP], [Tpq])
              for tt in range(2):
                  tsl = slice(tt * 128, (tt + 1) * 128)
                  for cbk in range(4):
                      P, TP = bank()
                      for i in range(4):
                          c = cbk * 4 + i
                          PE(lambda e: e.matmul(P[:, i * 128:(i + 1) * 128], lhsT=pqT[:, c, tsl], rhs=skT[:, c, :],
                                                start=True, stop=True), [Tpq, Tsk], [TP])
                      ACT(lambda e: e.copy(out=scs[:, cbk * 4:(cbk + 1) * 4, :],
                                           in_=P[:, 0:512].rearrange("p (c k) -> p c k", c=4)), [TP], [Tscs])
                  Ta_ = [Tk("m1a%d" % c) for c in range(16)]
                  Tb_ = [Tk("m1b%d" % c) for c in range(16)]
                  Tr_ = [Tk("tmpr%d" % c) for c in range(16)]
                  for c in range(16):
                      DVE(lambda e: e.max(out=m1[:, c, 0:8], in_=scs[:, c, :]), [Tscs], [Ta_[c]])
                  for c in range(16):
                      DVE(lambda e: e.match_replace(out=tmpr[:, c, :], in_to_replace=m1[:, c, 0:8], in_values=scs[:, c, :],
                                                    imm_value=-1e30), [Tscs, Ta_[c]], [Tr_[c]])
                  for c in range(16):
                      DVE(lambda e: e.max_index(out=i1[:, c, 0:8], in_max=m1[:, c, 0:8], in_values=scs[:, c, :]),
                          [Tscs, Ta_[c]], [Ta_[c]])
                  for c in range(16):
                      DVE(lambda e: e.max(out=m1[:, c, 8:16], in_=tmpr[:, c, :]), [Tr_[c]], [Tb_[c]])
                  for c in range(16):
                      DVE(lambda e: e.max_index(out=i1[:, c, 8:16], in_max=m1[:, c, 8:16], in_values=tmpr[:, c, :]),
                          [Tr_[c], Tb_[c]], [Tb_[c]])
                  TM = Ta_ + Tb_
                  DVE(lambda e: e.tensor_copy(out=i1f, in_=i1), TM, [Tm1])
                  m1v = m1.rearrange("p (h two) a -> p h two a", two=2)
                  i1v = i1f.rearrange("p (h two) a -> p h two a", two=2)
                  DVE(lambda e: e.tensor_tensor(out=cand, in0=m1v[:, :, 0, :].unsqueeze(3).broadcast_to([128, 8, 16, 16]),
                                                in1=m1v[:, :, 1, :].unsqueeze(2).broadcast_to([128, 8, 16, 16]),
                                                op=ALU.add), TM, [Tcand])
                  Tha = [Tk("tsa%d" % h) for h in range(8)]
                  Thb = [Tk("tsb%d" % h) for h in range(8)]
                  Thc = [Tk("ctmp%d" % h) for h in range(8)]
                  cvs = [cand[:, h, :, :].rearrange("p a b -> p (a b)") for h in range(8)]
                  for h in range(8):
                      DVE(lambda e: e.max(out=ts_[:, h, 0:8], in_=cvs[h]), [Tcand], [Tha[h]])
                  for h in range(8):
                      DVE(lambda e: e.match_replace(out=ctmp[:, h, :], in_to_replace=ts_[:, h, 0:8], in_values=cvs[h],
                                                    imm_value=-1e30), [Tcand, Tha[h]], [Thc[h]])
                  for h in range(8):
                      DVE(lambda e: e.max_index(out=tj[:, h, 0:8], in_max=ts_[:, h, 0:8], in_values=cvs[h]),
                          [Tcand, Tha[h]], [Tha[h]])
                  for h in range(8):
                      DVE(lambda e: e.max(out=ts_[:, h, 8:16], in_=ctmp[:, h, :]), [Thc[h]], [Thb[h]])
                  for h in range(8):
                      DVE(lambda e: e.max_index(out=tj[:, h, 8:16], in_max=ts_[:, h, 8:16], in_values=ctmp[:, h, :]),
                          [Thc[h], Thb[h]], [Thb[h]])
                  DVE(lambda e: e.tensor_single_scalar(out=ta, in_=tj, scalar=4, op=ALU.logical_shift_right),
                      Tha + Thb, [Tts])
                  DVE(lambda e: e.tensor_single_scalar(out=tb_, in_=tj, scalar=15, op=ALU.bitwise_and), [Tts], [Tts])
                  DVE(lambda e: e.tensor_copy(out=af_[:, :], in_=ta.rearrange("p h r -> p (h r)")), [Tts], [Tts])
                  DVE(lambda e: e.tensor_copy(out=bf_[:, :], in_=tb_.rearrange("p h r -> p (h r)")), [Tts], [Tts])
                  for (src_f, half_i, dstf) in ((af_, 0, If_), (bf_, 1, Jf_)):
                      DVE(lambda e: e.tensor_tensor(out=eq, in0=src_f[:, :].unsqueeze(2).broadcast_to([128, 128, 16]),
                                                    in1=iota16[:, :].unsqueeze(1).broadcast_to([128, 128, 16]),
                                                    op=ALU.is_equal), [Tts, Tc], [Teq])
                      eq4 = eq.rearrange("p (h r) a -> p h r a", h=8)
                      DVE(lambda e: e.tensor_tensor(out=eq4, in0=eq4,
                                                    in1=i1v[:, :, half_i, :].unsqueeze(2).broadcast_to([128, 8, 16, 16]),
                                                    op=ALU.mult), [Teq, Tm1], [Teq])
                      DVE(lambda e: e.tensor_reduce(out=dstf[:, :], in_=eq, axis=AX.X, op=ALU.add), [Teq], [Tts])
                  DVE(lambda e: e.tensor_tensor(out=ex, in0=ts_, in1=ts_[:, :, 0:1].broadcast_to([128, 8, 16]),
                                                op=ALU.subtract), [Tts], [Tts])
                  ACT(lambda e: e.activation(out=ex, in_=ex, func=AF.Exp), [Tts], [Tts])
                  DVE(lambda e: e.tensor_reduce(out=s8[:, :], in_=ex, axis=AX.X, op=ALU.add), [Tts], [Tts])
                  DVE(lambda e: e.reciprocal(out=s8[:, :], in_=s8[:, :]), [Tts], [Tts])
                  DVE(lambda e: e.tensor_tensor(out=gf, in0=ex, in1=s8[:, :].unsqueeze(2).broadcast_to([128, 8, 16]),
                                                op=ALU.mult), [Tts], [Tts])
                  P, TP = bank()
                  for i, srcT in enumerate((If_[:, :], Jf_[:, :], gf.rearrange("p h r -> p (h r)"))):
                      PE(lambda e: e.transpose(out=P[:, i * 128:(i + 1) * 128], in_=srcT, identity=ident_f[:]),
                         [Tts, Tc], [TP])
                  ACT(lambda e: e.copy(out=ITt[:, tsl], in_=P[:, 0:128]), [TP], [Tijg])
                  ACT(lambda e: e.copy(out=JTt[:, tsl], in_=P[:, 128:256]), [TP], [Tijg])
                  ACT(lambda e: e.copy(out=GTt[:, tsl], in_=P[:, 256:384]), [TP], [Tijg])
                  if G == 0 and tt == 0:
                      dump("If", If_, Tts, [128, 128], F32)
                      dump("skT", skT, Tsk, [128, 16, 128], BF16)
                      dump("pqT", pqT, Tpq, [128, 16, TG], BF16)
                      dump("scs", scs, Tscs, [128, 16, 128], F32)
                      dump("af", af_, Tts, [128, 128], F32)
                      dump("bf", bf_, Tts, [128, 128], F32)
                      dump("tj", tj, Tts, [128, 8, 16], U32)
                      dump("ts", ts_, Tts, [128, 8, 16], F32)
                      dump("m1", m1, Tm1, [128, 16, 16], F32)
                      dump("i1f", i1f, Tm1, [128, 16, 16], F32)
                      dump("Jf", Jf_, Tts, [128, 128], F32)
                      dump("gf", gf, Tts, [128, 8, 16], F32)
              S.barrier()

              ck("P2")
              carve_reset()
              W = carve([128, TG], BF16)
              TW = Tk("W")

              def flat_(t_):
                  a_ = t_[:]
                  return a_.rearrange("p a b -> p (a b)")

              NUS = 5
              uslots = [uvu[0][:], uvu[1][:], uvu[2][:],
                        flat_(ws[0]).rearrange("p (j k i) -> p j k i", j=2, k=8),
                        flat_(ws[2]).rearrange("p (j k i) -> p j k i", j=2, k=8)]
              vslots = [uvv[0][:], uvv[1][:], uvv[2][:],
                        flat_(ws[1]).rearrange("p (j d) -> p j d", j=2),
                        flat_(wbb[0]).rearrange("p (j d) -> p j d", j=2)]
              PFD = 3

              def load_uv(jp):
                  sl = jp % NUS
                  DMA(uslots[sl], uT_s.ap()[jp], [Tscr["uT"]], [Tus[sl]])
                  DMA(vslots[sl], v_s.ap()[jp], [Tscr["v"]], [Tus[sl]], nowaw=True)

              for jp_ in range(PFD):
                  load_uv(jp_)
              for t4 in range(TG // 4):
                  P, TP = bank("hi")
                  for i in range(4):
                      t = 4 * t4 + i
                      sl = t % NLR
                      DVE(lambda e: e.tensor_scalar(out=Lb[sl][:], in0=iotaB[:], scalar1=ITt[:, t:t + 1],
                                                    scalar2=GTt[:, t:t + 1], op0=ALU.is_equal, op1=ALU.mult),
                          [Tc, Tijg], [TL[sl]])
                      if t % 6 == 5:
                          ti_ = (t // 6) % NGA
                          tmp_ = gab[ti_][:, 0:128]
                          ACT(lambda e: e.activation(out=tmp_, in_=iotaB[:], func=AF.Square, bias=JTt[:, t:t + 1], scale=-1.0),
                              [Tc, Tijg], [Tga[ti_]])
                          ACT(lambda e: e.activation(out=Rb[sl][:], in_=tmp_, func=AF.Relu, bias=1.0, scale=-1.0),
                              [Tga[ti_]], [TR[sl]])
                      else:
                          DVE(lambda e: e.tensor_scalar(out=Rb[sl][:], in0=iotaB[:], scalar1=JTt[:, t:t + 1], scalar2=None,
                                                        op0=ALU.is_equal), [Tc, Tijg], [TR[sl]])
                      PE(lambda e: e.matmul(P[:, i * 128:(i + 1) * 128], lhsT=Lb[sl][:], rhs=Rb[sl][:], start=True, stop=True),
                         [TL[sl], TR[sl]], [TP])
                  ACT(lambda e: e.copy(out=W[:, :, 4 * t4:4 * t4 + 4], in_=P[:, 0:512].rearrange("p (t j) -> p j t", t=4)),
                      [TP], [TW])


              def stage_a(j):
                  jp, jj = j // 2, j % 2
                  sl = jp % NUS
                  if jj == 0 and jp + PFD < 64:
                      load_uv(jp + PFD)
                  s2 = j % NGA
                  P, TP = bank("hi")
                  for k in range(8):
                      PE(lambda e: e.matmul(P[:, 0:TG], lhsT=uslots[sl][:, jj, k, :], rhs=h2T[:, k, :],
                                            start=(k == 0), stop=(k == 7)), [Tus[sl], Th2], [TP])
                  ACT(lambda e: e.activation(out=gab[s2][:], in_=P[:, 0:TG], func=AF.Gelu_apprx_tanh), [TP], [Tga[s2]])
                  DVE(lambda e: e.tensor_tensor(out=wab[s2][:], in0=gab[s2][:], in1=W[:, j, :], op=ALU.mult),
                      [Tga[s2], TW], [Twa[s2]])

              def stage_b(j):
                  jp, jj = j // 2, j % 2
                  sl = jp % NUS
                  s2 = j % NGA
                  for tt in range(2):
                      for half in range(2):
                          bi = tt * 2 + half
                          PE(lambda e: e.matmul(pb[bi][:, 0:512], lhsT=wab[s2][:, tt * 128:(tt + 1) * 128],
                                                rhs=vslots[sl][:, jj, half * 512:(half + 1) * 512],
                                                start=(j == 0), stop=(j == 127)), [Twa[s2], Tus[sl]], [Tpb[bi]])

              LAG = 2
              for j in range(128 + LAG):
                  if j < 128:
                      stage_a(j)
                  if j >= LAG:
                      stage_b(j - LAG)
              for tt in range(2):
                  for half in range(2):
                      bi = tt * 2 + half
                      DVE(lambda e: e.tensor_tensor(out=xt[tt][:, half * 512:(half + 1) * 512], in0=pb[bi][:, 0:512],
                                                    in1=xt[tt][:, half * 512:(half + 1) * 512], op=ALU.add),
                          [Tpb[bi], Txt[tt]], [Txt[tt]])
                  DMA(out_d.ap()[g0 + tt * 128:g0 + (tt + 1) * 128, :], xt[tt][:], [Txt[tt]], [Tout], nowaw=True, semtk=Txt[tt])
              S.barrier()
        except _Stop:
            pass
        S.wait_all("sp", [Tout])
        S.wait_all("act", [Tout])
        counts = {k: v["n"] for k, v in S.eng.items()}
    return nc, dbg, counts


_CACHE = {}


def kernel(**inputs):
    if "nc" not in _CACHE:
        _CACHE["nc"] = build()[0]
        _CACHE["consts"] = host_consts()
    nc = _CACHE["nc"]
    consts = _CACHE["consts"]
    inputs = {k: np.asarray(v) for k, v in inputs.items()}
    in_maps = [make_in_map(inputs, b, consts) for b in range(8)]
    res = run_bass_kernel_spmd(nc, in_maps, core_ids=list(range(8)))
    out = np.stack([np.asarray(r["out"], np.float32) for r in res.results], axis=0)
    return out
```

```python
import math
from contextlib import ExitStack
import numpy as np
import ml_dtypes
import concourse.bass as bass
import concourse.mybir as mybir
from concourse.bass_utils import run_bass_kernel_spmd
from concourse.bass_types import AP

F32 = mybir.dt.float32
BF16 = mybir.dt.bfloat16
U32 = mybir.dt.uint32
AF = mybir.ActivationFunctionType
ALU = mybir.AluOpType
AX = mybir.AxisListType
NPBF = ml_dtypes.bfloat16

SEQ = 4096
DM = 1024
TG = 256
NGROUPS = SEQ // TG
EPS = 1e-6
NEG = -30000.0
RW = 768
RC = 6200
RC_OFF = 2100
NA = 3840
NB = 280


class Tk:
    __slots__ = ("name", "w", "r", "dsem", "dcnt")

    def __init__(self, name):
        self.name = name
        self.w = {}
        self.r = {}
        self.dsem = None
        self.dcnt = 0


class Sync:
    SEM_MAX = 20000

    def __init__(self, nc, stack):
        self.nc = nc
        self.stack = stack
        self.eng = {}
        self.nsem = 0
        self.pending = {}
        self.track_all = True
        for name, e in [("pe", nc.tensor), ("act", nc.scalar), ("dve", nc.vector),
                        ("pool", nc.gpsimd), ("sp", nc.sync)]:
            self.eng[name] = dict(e=e, sem=self._newsem(name), cnt=0, waited={}, name=name, n=0)

    def _newsem(self, name):
        self.nsem += 1
        return self.stack.enter_context(self.nc.semaphore("s%d_%s" % (self.nsem, name)))

    def _wait(self, E, s, v):
        k = id(s)
        if E["waited"].get(k, 0) >= v:
            return
        E["e"].wait_ge(s, v)
        E["waited"][k] = v

    def _deps(self, E, reads, writes, nowaw=False, skip_own=False):
        deps = {}

        def add(tok):
            if tok is None:
                return
            s, v = tok
            k = id(s)
            if k not in deps or deps[k][1] < v:
                deps[k] = (s, v)
        for t in reads:
            for tok in t.w.values():
                add(tok)
        for t in writes:
            if not nowaw:
                for tok in t.w.values():
                    add(tok)
            for tok in t.r.values():
                add(tok)
        for k, (s, v) in deps.items():
            if skip_own and s is E["sem"]:
                continue
            self._wait(E, s, v)

    def _mark(self, tok, reads, writes, nowaw=False):
        k = id(tok[0])
        for t in reads:
            t.r[k] = tok
        for t in writes:
            if nowaw:
                t.w[k] = tok
            else:
                t.w = {k: tok}
                t.r = {}

    def op(self, en, fn, reads=(), writes=()):
        E = self.eng[en]
        self._deps(E, reads, writes, skip_own=(en == "pe"))
        if E["cnt"] >= self.SEM_MAX:
            E["sem"] = self._newsem(en)
            E["cnt"] = 0
        ins = fn(E["e"])
        E["cnt"] += 1
        E["n"] += 1
        ins.then_inc(E["sem"], 1)
        E["last"] = (E["sem"], E["cnt"])
        self._mark((E["sem"], E["cnt"]), reads, writes)
        return ins

    def dma(self, qn, out, in_, reads=(), writes=(), nowaw=False, track=False, semtk=None, **kw):
        Q = self.eng[qn]
        self._deps(Q, reads, writes, nowaw=nowaw)
        t0 = semtk if semtk is not None else writes[0]
        if t0.dsem is None or t0.dcnt >= self.SEM_MAX:
            t0.dsem = self._newsem("d_" + t0.name)
            t0.dcnt = 0
        ins = Q["e"].dma_start(out=out, in_=in_, **kw)
        t0.dcnt += 16
        Q["n"] += 1
        ins.then_inc(t0.dsem, 16)
        if track or self.track_all:
            self.pending[id(t0.dsem)] = (t0.dsem, t0.dcnt)
        self._mark((t0.dsem, t0.dcnt), reads, writes, nowaw=nowaw)
        return ins

    def wait_all(self, en, tks):
        self._deps(self.eng[en], tks, tks)

    def barrier(self):
        names = ["pe", "act", "dve", "pool"]
        for a in names + ["sp"]:
            for b in names:
                if a != b and self.eng[b].get("last") is not None:
                    self._wait(self.eng[a], *self.eng[b]["last"])
            for (sm, v) in self.pending.values():
                self._wait(self.eng[a], sm, v)
        self.pending = {}


def _bucket(n):
    n = int(n)
    if n < 16:
        return n
    nf = np.float32(n)
    v = np.log(nf / np.float32(16)) / np.float32(math.log(128 / 16)) * np.float32(16)
    return min(31, 16 + int(np.float32(v)))


def host_consts():
    c = {}
    c["ident_f"] = np.eye(128, dtype=np.float32)
    c["ident_b"] = np.eye(128, dtype=np.float32).astype(NPBF)
    c["J_b"] = np.eye(128, dtype=np.float32)[::-1].copy().astype(NPBF)
    bd = np.zeros((128, 128), np.float32)
    bd[:64, :64] = 1
    bd[64:, 64:] = 1
    c["BD_b"] = bd.astype(NPBF)
    c["iotaB"] = np.tile(np.arange(128, dtype=np.float32), (128, 1)).astype(NPBF)
    c["iota16"] = np.tile(np.arange(16, dtype=np.float32), (128, 1))
    c["ones_row"] = np.ones((1, 128), np.float32)
    ohw = np.zeros((33, RW), np.float32)
    for i in range(RW):
        r = i - 128
        if r < 0 or r >= 512:
            ohw[32, i] = 1
        else:
            ohw[_bucket(r), i] = 1
    c["ohw"] = ohw
    ohc = np.zeros((33, RC), np.float32)
    for i in range(RC):
        r = i - RC_OFF
        if r < 0:
            ohc[32, i] = 1
        else:
            ohc[_bucket(r) if r < 128 else 31, i] = 1
    c["ohc"] = ohc
    smap = np.zeros((256, 64), np.float32)
    for n in range(255):
        for p in range(32):
            smap[n, (16 * n + p) // 64] += 1.0 / 32
    c["smapc"] = smap.reshape(2, 128, 64).transpose(1, 0, 2).copy().astype(NPBF)
    E = np.zeros((128, 4096), np.float32)
    E[np.arange(4096) // 64, np.arange(4096)] = 1
    c["Eall"] = E.astype(NPBF)
    t = np.arange(4096)[:, None]
    s = np.arange(64)[None, :]
    cur = t // 64
    forced = (s == 0) | (s == cur) | (s == cur - 1)
    vis = (s * 64) <= t
    C = np.where(vis, np.where(forced, 1000.0, 0.0), -1.0).astype(np.float32)
    c["Cst"] = C.reshape(32, 128, 64).transpose(1, 0, 2).copy()
    inv = np.zeros((128, 4, 16), np.float32)
    for ci, w in enumerate((2, 4, 8, 16)):
        for tt in range(16):
            inv[:, ci, tt] = 1.0 / min(tt + 1, w)
    c["invc16"] = inv
    return c


CONST_SPECS = {
    "ident_f": ([128, 128], F32), "ident_b": ([128, 128], BF16), "J_b": ([128, 128], BF16),
    "BD_b": ([128, 128], BF16), "iotaB": ([128, 128], BF16), "iota16": ([128, 16], F32),
    "ones_row": ([1, 128], F32), "ohw": ([33, RW], F32), "ohc": ([33, RC], F32),
    "smapc": ([128, 2, 64], BF16), "Eall": ([128, 4096], BF16), "Cst": ([128, 32, 64], F32),
    "invc16": ([128, 4, 16], F32),
}

INPUT_SPECS = {
    "x": [SEQ, DM], "cT": [128, 8], "rel_bias": [32, 8], "ada_w": [DM, 6 * DM], "adabT": [128, 48],
    "n1g": [128, 8], "n2g": [128, 8], "w_in": [DM, 3864], "pool_w": [4, 128, 128], "pscale": [128, 4],
    "pek": [128, 16], "w1k": [2048, 128], "w2k": [128, 64], "pev": [128, 16], "w1v": [2048, 128],
    "w2v": [128, 64], "gq": [128, 1], "gk": [128, 3], "wbp": [512, DM], "wba": [512, DM],
    "wout": [DM, DM], "wq": [DM, 2048], "subk": [8, 2, 128, 128], "pu": [16384, DM], "pv": [16384, DM],
}


def colT(v, k):
    return np.ascontiguousarray(np.asarray(v, np.float32).reshape(k, 128).T)


def make_in_map(inputs, b, consts):
    m = {}
    m["x"] = np.ascontiguousarray(inputs["x"][b])
    m["cT"] = colT(inputs["c"][b], 8)
    m["rel_bias"] = np.ascontiguousarray(inputs["rel_bias"])
    m["ada_w"] = np.ascontiguousarray(inputs["ada_w"][0])
    m["adabT"] = colT(inputs["ada_b"][0], 48)
    m["n1g"] = colT(inputs["norm1_g"][0], 8)
    m["n2g"] = colT(inputs["norm2_g"][0], 8)
    m["w_in"] = np.ascontiguousarray(inputs["w_in"][0])
    m["pool_w"] = np.ascontiguousarray(inputs["pool_w"][0])
    m["pscale"] = colT(inputs["pool_scale"][0], 4)
    m["pek"] = colT(inputs["cmp_pe_k"][0].reshape(-1), 16)
    m["w1k"] = np.ascontiguousarray(inputs["cmp_w1_k"][0])
    m["w2k"] = np.ascontiguousarray(inputs["cmp_w2_k"][0])
    m["pev"] = colT(inputs["cmp_pe_v"][0].reshape(-1), 16)
    m["w1v"] = np.ascontiguousarray(inputs["cmp_w1_v"][0])
    m["w2v"] = np.ascontiguousarray(inputs["cmp_w2_v"][0])
    gq = np.asarray(inputs["q_norm_g"][0], np.float32)
    m["gq"] = np.ascontiguousarray(np.concatenate([gq, gq]).reshape(128, 1))
    gk = np.asarray(inputs["k_norm_g"][0], np.float32)
    m["gk"] = np.ascontiguousarray(np.concatenate([gk, gk], axis=1).T)
    m["wbp"] = np.ascontiguousarray(inputs["w_branch_pool"][0])
    m["wba"] = np.ascontiguousarray(inputs["w_branch_attn"][0])
    m["wout"] = np.ascontiguousarray(inputs["w_out"][0])
    m["wq"] = np.ascontiguousarray(inputs["peer_w_q"][0])
    m["subk"] = np.ascontiguousarray(inputs["peer_sub_keys"][0])
    m["pu"] = np.ascontiguousarray(inputs["peer_u"][0])
    m["pv"] = np.ascontiguousarray(inputs["peer_v"][0])
    for k in m:
        m[k] = np.asarray(m[k], np.float32)
    m.update(consts)
    return m


class _Stop(Exception):
    pass


def build(NG=NGROUPS, debug=False, prepass=True, stop=None):
    nc = bass.Bass("TRN2", target_bir_lowering=False)
    din = {}
    for k, shp in INPUT_SPECS.items():
        din[k] = nc.dram_tensor(k, list(shp), F32, kind="ExternalInput")
    for k, (shp, dt) in CONST_SPECS.items():
        din[k] = nc.dram_tensor(k, list(shp), dt, kind="ExternalInput")
    out_d = nc.dram_tensor("out", [SEQ, DM], F32, kind="ExternalOutput")
    winA_s = nc.dram_tensor("winA_s", [128, 8, NA], BF16, kind="Internal")
    winB_s = nc.dram_tensor("winB_s", [128, 8, NB], BF16, kind="Internal")
    wbp_s = nc.dram_tensor("wbp_s", [128, 4, DM], BF16, kind="Internal")
    wba_s = nc.dram_tensor("wba_s", [128, 4, DM], BF16, kind="Internal")
    wout_s = nc.dram_tensor("wout_s", [128, 8, DM], BF16, kind="Internal")
    wq_s = nc.dram_tensor("wq_s", [128, 8, 2048], BF16, kind="Internal")
    uT_s = nc.dram_tensor("uT_s", [64, 128, 2, 8, 128], BF16, kind="Internal")
    v_s = nc.dram_tensor("v_s", [64, 128, 2, DM], BF16, kind="Internal")
    fW_s = nc.dram_tensor("fW_s", [8, RW], BF16, kind="Internal")
    fC_s = nc.dram_tensor("fC_s", [8, RC], BF16, kind="Internal")
    dbg = {}

    with ExitStack() as st:
        S = Sync(nc, st)

        def ck(name):
            if stop == name:
                raise _Stop()

        def sb(name, shape, dt):
            return st.enter_context(nc.sbuf_tensor("s_" + name, list(shape), dt))

        def PE(fn, r, w):
            return S.op("pe", fn, r, w)

        def ACT(fn, r, w):
            return S.op("act", fn, r, w)

        def DVE(fn, r, w):
            return S.op("dve", fn, r, w)

        def POOL(fn, r, w):
            return S.op("pool", fn, r, w)

        def DMA(out, in_, r, w, nowaw=False, q="sp", track=False, semtk=None):
            return S.dma(q, out, in_, r, w, nowaw=nowaw, track=track, semtk=semtk)

        flip = [0]

        def EV(fn, r, w):
            flip[0] ^= 1
            if flip[0]:
                return S.op("act", lambda e: fn(e, True), r, w)
            return S.op("dve", lambda e: fn(e, False), r, w)

        def copy_ev(out, in_, r, w):
            return EV(lambda e, a: (e.copy(out=out, in_=in_) if a else e.tensor_copy(out=out, in_=in_)), r, w)

        pb = [st.enter_context(nc.psum_tensor("pb%d" % i, [128, 512], F32)) for i in range(8)]
        Tpb = [Tk("pb%d" % i) for i in range(8)]
        rot = {"all": [list(range(8)), 0], "acc": [[0, 1, 2], 0], "sc": [[3, 4, 5, 6, 7], 0],
               "hi": [[4, 5, 6, 7], 0]}

        def bank(pool="all"):
            lst, i = rot[pool]
            rot[pool][1] = (i + 1) % len(lst)
            b = lst[i]
            return pb[b], Tpb[b]

        def bfv(P):
            return P[:].bitcast(BF16)

        Tc = Tk("consts")

        def cload(name, shape, dt, src):
            t = sb(name, shape, dt)
            DMA(t[:], src, [], [Tc], nowaw=True)
            return t

        ident_f = cload("ident_f", [128, 128], F32, din["ident_f"].ap())
        ident_b = cload("ident_b", [128, 128], BF16, din["ident_b"].ap())
        Jb = cload("J_b", [128, 128], BF16, din["J_b"].ap())
        BD = cload("BD_b", [128, 128], BF16, din["BD_b"].ap())
        iotaB = cload("iotaB", [128, 128], BF16, din["iotaB"].ap())
        iota16 = cload("iota16", [128, 16], F32, din["iota16"].ap())
        Eall = cload("Eall", [128, 4096], BF16, din["Eall"].ap())
        invc16 = cload("invc16", [128, 4, 16], F32, din["invc16"].ap())
        n1g = cload("n1g", [128, 8], F32, din["n1g"].ap())
        n2g = cload("n2g", [128, 8], F32, din["n2g"].ap())
        pscale = cload("pscale", [128, 4], F32, din["pscale"].ap())
        gq = cload("gq", [128, 1], F32, din["gq"].ap())
        gk = cload("gk", [128, 3], F32, din["gk"].ap())

        ksT = sb("ksT", [128, SEQ], BF16)
        kwT = sb("kwT", [128, SEQ], BF16)
        Tks, Tkw = Tk("ksT"), Tk("kwT")
        vsA = sb("vsA", [128, 32, 2, 65], BF16)
        vwA = sb("vwA", [128, 32, 2, 65], BF16)
        Tvs, Tvw = Tk("vsA"), Tk("vwA")
        kcT = sb("kcT", [128, 256], BF16)
        vcT = sb("vcT", [128, 256], BF16)
        vcA = sb("vcA", [128, 2, 2, 129], BF16)
        Tkc, Tvc, TvcA = Tk("kcT"), Tk("vcT"), Tk("vcA")
        TZ = sb("TZ", [128, 3, 8, 128], BF16)
        TTZ = Tk("TZ")
        skT = sb("skT", [128, 16, 128], BF16)
        Tsk = Tk("skT")
        w1 = {"k": sb("w1k", [128, 16, 128], BF16), "v": sb("w1v", [128, 16, 128], BF16)}
        w2 = {"k": sb("w2k", [128, 2, 128], BF16), "v": sb("w2v", [128, 2, 128], BF16)}
        cb = {"k": sb("cbk", [128, 1], F32), "v": sb("cbv", [128, 1], F32)}
        Tw1 = Tk("w1")
        winB = sb("winB", [128, 8, NB], BF16)
        TwinB = Tk("winB")
        wpool = sb("wpool", [128, 4, 128], BF16)
        Twpool = Tk("wpool")
        adaT = sb("adaT", [128, 96], F32)
        TadaT = Tk("adaT")
        gmod = [sb("gmod1", [128, 8], F32), sb("gmod2", [128, 8], F32)]
        Tgmod = Tk("gmod")
        b31c = sb("b31c", [128, 8], F32)
        b31m = sb("b31m", [128, 8], F32)
        n256 = sb("n256", [128, 1], F32)
        zcol = sb("zcol", [128, 1], F32)
        Tb31 = Tk("b31")
        xt = [sb("xt0", [128, DM], F32), sb("xt1", [128, DM], F32)]
        Txt = [Tk("xt0"), Tk("xt1")]
        xin = [sb("xin0", [128, DM], F32), sb("xin1", [128, DM], F32)]
        Txin = [Tk("xin0"), Tk("xin1")]
        h2T = sb("h2T", [128, 8, TG], BF16)
        Th2 = Tk("h2T")
        ITt = sb("ITt", [128, TG], F32)
        JTt = sb("JTt", [128, TG], F32)
        GTt = sb("GTt", [128, TG], F32)
        Tijg = Tk("ijg")
        NLR = 4
        Lb = [sb("L%d" % i, [128, 128], BF16) for i in range(NLR)]
        Rb = [sb("R%d" % i, [128, 128], BF16) for i in range(NLR)]
        TL = [Tk("L%d" % i) for i in range(NLR)]
        TR = [Tk("R%d" % i) for i in range(NLR)]
        NGA = 3
        gab = [sb("ga%d" % i, [128, TG], BF16) for i in range(NGA)]
        wab = [sb("wa%d" % i, [128, TG], BF16) for i in range(NGA)]
        Tga = [Tk("ga%d" % i) for i in range(NGA)]
        Twa = [Tk("wa%d" % i) for i in range(NGA)]
        NWS = 3
        ws = [sb("ws%d" % i, [128, 8, 256], BF16) for i in range(NWS)]
        Tws = [Tk("ws%d" % i) for i in range(NWS)]
        wsi = [0]
        wbb = [sb("wbb%d" % i, [128, 4, 512], BF16) for i in range(2)]
        Twbb = [Tk("wbb%d" % i) for i in range(2)]
        NUV = 3
        uvu = [sb("uvu%d" % i, [128, 2, 8, 128], BF16) for i in range(NUV)]
        uvv = [sb("uvv%d" % i, [128, 2, DM], BF16) for i in range(NUV)]
        Tuv = [Tk("uv%d" % i) for i in range(NUV)]
        Tus = [Tk("us%d" % i) for i in range(5)]
        TTC = [Tk("TC0"), Tk("TC1")]
        Cstt = [sb("Cst%d" % i, [128, 2, 64], F32) for i in range(2)]
        TCst = [Tk("Cst%d" % i) for i in range(2)]
        arena = sb("arena", [128, 32768], BF16)
        Tout = Tk("out")
        Tscr = {k: Tk(k) for k in ("winA", "winB", "wbp", "wba", "wout", "wq", "uT", "v", "fW", "fC")}

        def adacol(which, k):
            i = (which * 8 + k) * 2
            return adaT[:, i:i + 1]

        def strided(tile, col, step, n):
            base = tile[:, col:col + 1]
            return AP(base.tensor, base.offset, [list(base.ap[0]), [step, n]])

        aoff = [0]

        def carve_reset(base=0):
            aoff[0] = base

        def carve(free_shape, dt):
            n = int(np.prod(free_shape))
            nb = n * (2 if dt == BF16 else 4)
            nb4 = (nb + 3) // 4 * 4
            a = aoff[0]
            aoff[0] += nb4
            assert aoff[0] <= 65536, ("arena overflow", aoff[0])
            v = arena[:, a // 2:(a + nb4) // 2]
            if dt != BF16:
                v = v.bitcast(dt)
            v = v[:, 0:n]
            if len(free_shape) == 2:
                v = v.rearrange("p (a b) -> p a b", a=free_shape[0])
            elif len(free_shape) == 3:
                v = v.rearrange("p (a b c) -> p a b c", a=free_shape[0], b=free_shape[1])
            return v

        try:
          if True:
              carve_reset()
              gbc = [carve([DM], F32), carve([DM], F32)]
              Tgbc = Tk("gbc")
              base1 = aoff[0]
              ones_row = carve([128], F32)
              DMA(ones_row[0:1, :], din["ones_row"].ap(), [], [Tc], nowaw=True)
              cT = carve([8], F32)
              DMA(cT, din["cT"].ap(), [], [Tc], nowaw=True)
              adabT = carve([48], F32)
              DMA(adabT, din["adabT"].ap(), [], [Tc], nowaw=True)
              csil = carve([8, 2], F32)
              Tcs = Tk("csil")
              for dd in range(2):
                  ACT(lambda e: e.activation(out=csil[:, :, dd], in_=cT, func=AF.Silu), [Tc], [Tcs])
              adawt = [carve([8, 512], F32) for i in range(2)]
              Tadaw = [Tk("adaw%d" % i) for i in range(2)]
              adaw_v = din["ada_w"].ap().rearrange("(k p) n -> p k n", p=128)
              PA, TPA = bank()
              for n in range(12):
                  sl = n % 2
                  DMA(adawt[sl], adaw_v[:, :, n * 512:(n + 1) * 512], [], [Tadaw[sl]])
                  for fc in range(4):
                      i = n * 4 + fc
                      for k in range(8):
                          PE(lambda e: e.matmul(PA[:, 2 * i:2 * i + 2], lhsT=adawt[sl][:, k, fc * 128:(fc + 1) * 128],
                                                rhs=csil[:, k, :], start=(k == 0), stop=(k == 7)), [Tcs, Tadaw[sl]], [TPA])
              av = adaT[:, :].rearrange("p (i t) -> p i t", t=2)
              pv_ = PA[:, 0:96].rearrange("p (i t) -> p i t", t=2)
              for dd in range(2):
                  DVE(lambda e: e.tensor_tensor(out=av[:, :, dd], in0=pv_[:, :, dd], in1=adabT, op=ALU.add),
                      [TPA, Tc], [TadaT])
              tmp8 = carve([8], F32)
              Ttmp8 = Tk("tmp8")
              for li, (which, gn) in enumerate(((1, n1g), (4, n2g))):
                  sv = av[:, which * 8:(which + 1) * 8, 0]
                  DVE(lambda e: e.tensor_scalar(out=tmp8, in0=sv, scalar1=1.0, scalar2=None, op0=ALU.add),
                      [TadaT], [Ttmp8])
                  DVE(lambda e: e.tensor_tensor(out=gmod[li][:], in0=tmp8, in1=gn[:], op=ALU.mult),
                      [Ttmp8, Tc], [Tgmod])
              onesq = carve([128], F32)
              Tonesq = Tk("onesq")
              DVE(lambda e: e.memset(onesq, 1.0), [], [Tonesq])
              dg = [carve([128], F32), carve([128], F32)]
              Tdg = [Tk("dg0"), Tk("dg1")]
              for gi, which in enumerate((2, 5)):
                  for k in range(8):
                      d_ = dg[k % 2]
                      DVE(lambda e: e.tensor_scalar(out=d_, in0=ident_f[:], scalar1=adacol(which, k), scalar2=None,
                                                    op0=ALU.mult), [Tc, TadaT], [Tdg[k % 2]])
                      P, TP = bank()
                      PE(lambda e: e.matmul(P[:, 0:128], lhsT=onesq, rhs=d_, start=True, stop=True),
                         [Tonesq, Tdg[k % 2]], [TP])
                      copy_ev(gbc[gi][:, k * 128:(k + 1) * 128], P[:, 0:128], [TP], [Tgbc])
              S.barrier()

              ck("ada")
              carve_reset(base1)
              ones_row = carve([128], F32)
              Tor = Tk("ones_row")
              DMA(ones_row[0:1, :], din["ones_row"].ap(), [], [Tor])
              tab = carve([8], F32)
              Ttab = Tk("tab")
              DVE(lambda e: e.memset(tab[0:64, :], NEG), [], [Ttab])
              DMA(tab[0:32, :], din["rel_bias"].ap(), [], [Ttab])
              r31 = carve([8], F32)
              DMA(r31[0:1, :], din["rel_bias"].ap()[31:32, :], [], [Tor], nowaw=True)
              ohw = carve([RW], F32)
              DMA(ohw[0:33, :], din["ohw"].ap(), [], [Tor], nowaw=True)
              ohc = carve([RC], F32)
              DMA(ohc[0:33, :], din["ohc"].ap(), [], [Tor], nowaw=True)
              fwsb = carve([RW], BF16)
              fcsb = carve([RC], BF16)
              Tfw, Tfc = Tk("fwsb"), Tk("fcsb")
              for (oh, fsb, Tf, R) in ((ohw, fwsb, Tfw, RW), (ohc, fcsb, Tfc, RC)):
                  for c0 in range(0, R, 512):
                      c1 = min(R, c0 + 512)
                      P, TP = bank()
                      PE(lambda e: e.matmul(P[0:8, 0:c1 - c0], lhsT=tab[0:33, 0:8], rhs=oh[0:33, c0:c1],
                                            start=True, stop=True), [Ttab, Tor], [TP])
                      copy_ev(fsb[0:8, c0:c1], P[0:8, 0:c1 - c0], [TP], [Tf])
              DMA(fW_s.ap(), fwsb[0:8, :], [Tfw], [Tscr["fW"]])
              DMA(fC_s.ap(), fcsb[0:8, :], [Tfc], [Tscr["fC"]])
              for i, off in enumerate((1, 129, 513)):
                  src = AP(fW_s, off, [[1, 128], [RW, 8], [1, 128]])
                  DMA(TZ[:, i, :, :], src, [Tscr["fW"]], [TTZ], nowaw=True)
              P, TP = bank()
              PE(lambda e: e.matmul(P[:, 0:8], lhsT=ones_row[0:1, 0:128], rhs=r31[0:1, 0:8], start=True, stop=True),
                 [Tor], [TP])
              ACT(lambda e: e.copy(out=b31c[:], in_=P[:, 0:8]), [TP], [Tb31])
              DVE(lambda e: e.tensor_scalar(out=b31m[:], in0=P[:, 0:8], scalar1=-256.0, scalar2=None, op0=ALU.add),
                  [TP], [Tb31])
              DVE(lambda e: e.memset(n256[:], -256.0), [], [Tb31])
              DVE(lambda e: e.memset(zcol[:], 0.0), [], [Tb31])

              ck("t5")
              DVE(lambda e: e.memset(vsA[:], 1.0), [], [Tvs])
              DVE(lambda e: e.memset(vwA[:], 1.0), [], [Tvw])
              DVE(lambda e: e.memset(kcT[:], 0.0), [], [Tkc])
              DVE(lambda e: e.memset(vcT[:], 0.0), [], [Tvc])
              DVE(lambda e: e.memset(vcA[:], 1.0), [], [TvcA])
              for g in range(2):
                  DMA(vcA[:, :, g, 65:129], din["smapc"].ap(), [], [TvcA])
              S.barrier()

              ck("misc")
              carve_reset(base1)
              stg = carve([16, 128], F32)
              Tstg = Tk("stg")
              pe_c = {"k": carve([16], F32), "v": carve([16], F32)}
              pe_b = {"k": carve([16], BF16), "v": carve([16], BF16)}
              stg2 = carve([64], F32)
              Tstg2 = Tk("stg2")
              for kv in ("k", "v"):
                  DMA(stg, din["w1" + kv].ap().rearrange("(c p) h -> p c h", p=128), [], [Tstg])
                  DVE(lambda e: e.tensor_copy(out=w1[kv][:], in_=stg), [Tstg], [Tw1])
                  DMA(stg2, din["w2" + kv].ap(), [], [Tstg2])
                  DVE(lambda e: e.memset(w2[kv][:], 0.0), [], [Tw1])
                  for g in range(2):
                      DVE(lambda e: e.tensor_copy(out=w2[kv][:, g, 64 * g:64 * g + 64], in_=stg2), [Tstg2], [Tw1])
                  DMA(pe_c[kv], din["pe" + kv].ap(), [], [Tstg2])
                  DVE(lambda e: e.tensor_copy(out=pe_b[kv], in_=pe_c[kv]), [Tstg2], [Tw1])
                  P, TP = bank()
                  for c in range(16):
                      PE(lambda e: e.matmul(P[:, 0:1], lhsT=w1[kv][:, c, :], rhs=pe_b[kv][:, c:c + 1],
                                            start=(c == 0), stop=(c == 15)), [Tw1], [TP])
                  ACT(lambda e: e.copy(out=cb[kv][:], in_=P[:, 0:1]), [TP], [Tw1])
              DMA(stg[:, 0:4, :], din["pool_w"].ap().rearrange("g c d -> c g d"), [], [Tstg])
              DVE(lambda e: e.tensor_copy(out=wpool[:], in_=stg[:, 0:4, :]), [Tstg], [Twpool])
              DMA(stg, din["subk"].ap().rearrange("h p k d -> k (h p) d"), [], [Tstg])
              for cbk in range(4):
                  P, TP = bank()
                  for i in range(4):
                      c = cbk * 4 + i
                      PE(lambda e: e.transpose(out=P[:, i * 128:(i + 1) * 128], in_=stg[:, c, :], identity=ident_f[:]),
                         [Tstg, Tc], [TP])
                  copy_ev(skT[:, cbk * 4:(cbk + 1) * 4, :], P[:, 0:512].rearrange("p (c k) -> p c k", c=4), [TP], [Tsk])
              S.barrier()

              ck("small")
              carve_reset(base1)
              cvf = [carve([8, 512], F32) for i in range(2)]
              cvb = [carve([8, 512], BF16) for i in range(2)]
              Tcvf = [Tk("cvf%d" % i) for i in range(2)]
              Tcvb = [Tk("cvb%d" % i) for i in range(2)]
              cvi = [0]

              def conv(src_v, KC, N, runs, scale_bc=None):
                  for p0 in range(0, N, 512):
                      p1 = min(N, p0 + 512)
                      w = p1 - p0
                      sl = cvi[0] % 2
                      cvi[0] += 1
                      DMA(cvf[sl][:, 0:KC, 0:w], src_v[:, :, p0:p1], [], [Tcvf[sl]])
                      if scale_bc is None:
                          copy_ev(cvb[sl][:, 0:KC, 0:w], cvf[sl][:, 0:KC, 0:w], [Tcvf[sl]], [Tcvb[sl]])
                      else:
                          for k in range(KC):
                              DVE(lambda e: e.tensor_tensor(out=cvb[sl][:, k, 0:w], in0=cvf[sl][:, k, 0:w],
                                                            in1=scale_bc[:, p0:p1], op=ALU.mult),
                                  [Tcvf[sl], Tgbc], [Tcvb[sl]])
                      for (s0, n, dst, d0, Td) in runs:
                          a = max(s0, p0)
                          b = min(s0 + n, p1)
                          if a < b:
                              DMA(dst.ap()[:, :, d0 + a - s0: d0 + b - s0], cvb[sl][:, 0:KC, a - p0:b - p0],
                                  [Tcvb[sl]], [Td], nowaw=True, semtk=Tcvb[sl])

              runs = [(0, 512, winA_s, 0, Tscr["winA"])]
              for h in range(8):
                  pos = 2 * h if h < 4 else 2 * (h - 4) + 1
                  runs.append((512 + 64 * h, 64, winA_s, 512 + 64 * pos, Tscr["winA"]))
              for g in range(2):
                  for dup in range(2):
                      runs.append((1024 + 64 * g, 64, winA_s, 1280 + 128 * g + 64 * dup, Tscr["winA"]))
                      runs.append((1152 + 64 * g, 64, winA_s, 1536 + 128 * g + 64 * dup, Tscr["winA"]))
              runs.append((1280, 128, winA_s, 1024, Tscr["winA"]))
              runs.append((1536, 128, winA_s, 1152, Tscr["winA"]))
              runs.append((1816, 2048, winA_s, 1792, Tscr["winA"]))
              runs.append((1408, 128, winB_s, 0, Tscr["winB"]))
              runs.append((1664, 128, winB_s, 128, Tscr["winB"]))
              runs.append((1792, 24, winB_s, 256, Tscr["winB"]))
              conv(din["w_in"].ap().rearrange("(k p) n -> p k n", p=128), 8, 3864, runs)
              conv(din["wbp"].ap().rearrange("(k p) n -> p k n", p=128), 4, DM, [(0, DM, wbp_s, 0, Tscr["wbp"])])
              conv(din["wba"].ap().rearrange("(k p) n -> p k n", p=128), 4, DM, [(0, DM, wba_s, 0, Tscr["wba"])])
              conv(din["wout"].ap().rearrange("(k p) n -> p k n", p=128), 8, DM, [(0, DM, wout_s, 0, Tscr["wout"])],
                   scale_bc=gbc[0])
              conv(din["wq"].ap().rearrange("(k p) n -> p k n", p=128), 8, 2048, [(0, 2048, wq_s, 0, Tscr["wq"])])
              DMA(winB[:], winB_s.ap(), [Tscr["winB"]], [TwinB])
              S.barrier()

              ck("conv")
              carve_reset(base1)
              if prepass:
                  NPB = 3
                  ublk = [carve([2, DM], F32) for i in range(NPB)]
                  vblk = [carve([2, DM], F32) for i in range(NPB)]
                  uo = [uvu[i][:] for i in range(NPB)]
                  vo = [uvv[i][:] for i in range(NPB)]
                  Tub = [Tk("ublk%d" % i) for i in range(NPB)]
                  Tvb = [Tk("vblk%d" % i) for i in range(NPB)]
                  Tuo = [Tk("uo%d" % i) for i in range(NPB)]
                  Tvo = [Tk("vo%d" % i) for i in range(NPB)]
                  u_v = din["pu"].ap().rearrange("(i j) d -> i j d", j=128)
                  v_v = din["pv"].ap().rearrange("(i j) d -> i j d", j=128)
                  def pre_load(jp):
                      sl = jp % NPB
                      DMA(ublk[sl], u_v[:, 2 * jp:2 * jp + 2, :], [], [Tub[sl]])
                      DMA(vblk[sl], v_v[:, 2 * jp:2 * jp + 2, :], [], [Tvb[sl]])

                  pre_load(0)
                  pre_load(1)
                  for jp in range(64):
                      sl = jp % NPB
                      if jp + 2 < 64:
                          pre_load(jp + 2)
                      for jj in range(2):
                          for hb in range(2):
                              P, TP = bank()
                              for i in range(4):
                                  k = hb * 4 + i
                                  PE(lambda e: e.transpose(out=P[:, i * 128:(i + 1) * 128],
                                                           in_=ublk[sl][:, jj, k * 128:(k + 1) * 128],
                                                           identity=ident_f[:]), [Tub[sl], Tc], [TP])
                              copy_ev(uo[sl][:, jj, hb * 4:(hb + 1) * 4, :],
                                      P[:, 0:512].rearrange("p (k i) -> p k i", k=4), [TP], [Tuo[sl]])
                          POOL(lambda e: e.tensor_tensor(out=vo[sl][:, jj, :], in0=vblk[sl][:, jj, :], in1=gbc[1],
                                                         op=ALU.mult), [Tvb[sl], Tgbc], [Tvo[sl]])
                      DMA(uT_s.ap()[jp], uo[sl], [Tuo[sl]], [Tscr["uT"]], nowaw=True, semtk=Tuo[sl])
                      DMA(v_s.ap()[jp], vo[sl], [Tvo[sl]], [Tscr["v"]], nowaw=True, semtk=Tvo[sl])
              S.barrier()
          ck("pre")
          S.track_all = False
          def dump(name, ap_, tk, shape, dt=F32):
              if not debug:
                  return
              if not isinstance(ap_, AP):
                  ap_ = ap_[:]
              d = nc.dram_tensor("dbg_" + name, list(shape), dt, kind="ExternalOutput")
              dbg[name] = d
              DMA(d.ap(), ap_, [tk], [Tout], nowaw=True, track=True)

          def v8_(t_):
              a_ = t_[:]
              if len(a_.shape) == 4:
                  a_ = a_.rearrange("p a b c -> p (a b c)")
              else:
                  a_ = a_.rearrange("p a b -> p (a b)")
              return a_.rearrange("p (k n) -> p k n", k=8)

          wsl = [ws[i][:] for i in range(NWS)] + [v8_(uvu[i]) for i in range(3)] + [v8_(uvv[i]) for i in range(3)]
          Twsl = list(Tws) + [Tk("wsa%d" % i) for i in range(6)]
          NWSL = len(wsl)

          def ws_next():
              i = wsi[0] % NWSL
              wsi[0] += 1
              return wsl[i], Twsl[i]

          def load_x(G_):
              for tt_ in range(2):
                  DMA(xin[tt_][:], din["x"].ap()[G_ * TG + tt_ * 128:G_ * TG + (tt_ + 1) * 128, :], [], [Txin[tt_]])

          load_x(0)

          def norm_both(li, dstT, Tdst, xn2, Txn2, ss2, Tss2):
              shift_which = 0 if li == 0 else 3
              srcs = [((xin[tt], Txin[tt]) if li == 0 else (xt[tt], Txt[tt])) for tt in range(2)]
              for tt in range(2):
                  xs, Txs = srcs[tt]
                  ACT(lambda e: e.activation(out=xn2[tt][:, :], in_=xs[:], func=AF.Square, accum_out=ss2[tt][:, 0:1]),
                      [Txs], [Txn2[tt], Tss2[tt]])
              for tt in range(2):
                  DVE(lambda e: e.tensor_scalar(out=ss2[tt][:, 1:2], in0=ss2[tt][:, 0:1], scalar1=1.0 / DM, scalar2=EPS,
                                                op0=ALU.mult, op1=ALU.add), [Tss2[tt]], [Tss2[tt]])
              for tt in range(2):
                  ACT(lambda e: e.activation(out=ss2[tt][:, 2:3], in_=ss2[tt][:, 1:2], func=AF.Sqrt), [Tss2[tt]], [Tss2[tt]])
              for tt in range(2):
                  DVE(lambda e: e.reciprocal(out=ss2[tt][:, 3:4], in_=ss2[tt][:, 2:3]), [Tss2[tt]], [Tss2[tt]])
              for tt in range(2):
                  xs, Txs = srcs[tt]
                  DVE(lambda e: e.tensor_scalar(out=xn2[tt][:, :], in0=xs[:], scalar1=ss2[tt][:, 3:4], scalar2=None,
                                                op0=ALU.mult), [Txs, Tss2[tt]], [Txn2[tt]])
              banks_ = [bank() for _ in range(2)]
              for tt in range(2):
                  P, TP = banks_[tt]
                  Pb = bfv(P)
                  for k in range(8):
                      PE(lambda e: e.transpose(out=Pb[:, k * 128:(k + 1) * 128], in_=xn2[tt][:, k * 128:(k + 1) * 128],
                                               identity=ident_b[:]), [Txn2[tt], Tc], [TP])
              for tt in range(2):
                  P, TP = banks_[tt]
                  Pb = bfv(P)
                  for k in range(8):
                      o = dstT[:, k, tt * 128:(tt + 1) * 128]
                      i_ = Pb[:, k * 128:(k + 1) * 128]
                      gm = gmod[li][:, k:k + 1]
                      sh = adacol(shift_which, k)
                      EV(lambda e, a: (e.activation(out=o, in_=i_, func=AF.Identity, scale=gm, bias=sh) if a else
                                       e.tensor_scalar(out=o, in0=i_, scalar1=gm, scalar2=sh, op0=ALU.mult, op1=ALU.add)),
                         [TP, Tgmod, TadaT], [Tdst])

          def load_ws(scr, Tsc, c0):
              wt, Twt = ws_next()
              DMA(wt, scr.ap()[:, :, c0:c0 + 256], [Tsc], [Twt])
              return wt, Twt

          PFW = 3
          for G in range(NG):
              g0 = G * TG
              carve_reset()
              hT = carve([8, TG], BF16); ThT = Tk("hT")
              qT = carve([8, TG], BF16); TqT = Tk("qT")
              zp = carve([4, 272], F32); Tzp = Tk("zp")
              ptmp = [carve([272], F32), carve([272], F32)]; Tptmp = [Tk("pt0"), Tk("pt1")]
              pooled = carve([4, TG], BF16); Tpooled = Tk("pooled")
              ypT = carve([4, TG], BF16); TypT = Tk("ypT")
              yaT = carve([4, TG], BF16); TyaT = Tk("yaT")
              mixT = carve([8, TG], BF16); TmixT = Tk("mixT")
              Xk = [carve([273], BF16), carve([273], BF16)]
              Xv = [carve([273], BF16), carve([273], BF16)]
              TX = Tk("X")
              gtt = [carve([24], F32), carve([24], F32)]; Tgt = [Tk("gt0"), Tk("gt1")]
              OC = carve([8, 129], F32); TOC = Tk("OC")
              Os = carve([8, 65], F32); TOs = Tk("Os")
              Ow = carve([8, 65], F32); TOw = Tk("Ow")
              NPT = 4
              PTb = [carve([512], BF16) for _ in range(NPT)]; TPT = [Tk("PT%d" % i) for i in range(NPT)]
              pti = [0]
              TCt = [[carve([8, 128], BF16) for _ in range(2)] for _ in range(2)]
              xn2 = [carve([DM], BF16), carve([DM], BF16)]; Txn2 = [Tk("xn0"), Tk("xn1")]
              ss2 = [carve([4], F32), carve([4], F32)]; Tss2 = [Tk("ss0"), Tk("ss1")]
              sq = carve([TG], BF16); Tsq = Tk("sq")
              r1 = carve([TG], F32); Tr1 = Tk("r1")
              sqs = [sq, carve([TG], BF16)]; Tsqs = [Tsq, Tk("sq1")]
              r1s = [r1, carve([TG], F32)]; Tr1s = [Tr1, Tk("r11")]
              hid = {"k": [carve([16], BF16), carve([16], BF16)], "v": [carve([16], BF16), carve([16], BF16)]}
              Thid = Tk("hid")
              sc64 = [carve([64], F32), carve([64], F32)]; score2 = [carve([64], F32), carve([64], F32)]
              selb = [carve([128], BF16), carve([128], BF16)]
              m8 = [carve([16], F32), carve([16], F32)]; Tsel = [Tk("sel0"), Tk("sel1")]
              selT = [carve([128], BF16), carve([128], BF16)]; TselT = [Tk("selT0"), Tk("selT1")]
              cf = carve([6, 8], F32); Tcf = Tk("cf")
              yacc = carve([8, 64], F32); ytmp = carve([8, 64], F32); Tyacc = Tk("yacc")
              ya = carve([512], BF16); Tya = Tk("ya")
              mgt = [carve([TG], BF16) for _ in range(2)]; Tmgt = [Tk("mg0"), Tk("mg1")]
              t1 = carve([TG], F32); Tt1 = Tk("t1")
              tmp16 = carve([16], F32)

              if G == 0:
                  pass
              if G == 0:
                  zph = sb("zph", [128, 4, 16], F32); Tzph = Tk("zph")
                  Xh = {("k", 0): sb("Xhk0", [128, 17], BF16), ("k", 1): sb("Xhk1", [128, 17], BF16),
                        ("v", 0): sb("Xhv0", [128, 17], BF16), ("v", 1): sb("Xhv1", [128, 17], BF16)}
                  TXh = Tk("Xh")
                  DVE(lambda e: e.memset(zph[:], 0.0), [], [Tzph])
                  for kk in Xh:
                      DVE(lambda e: e.memset(Xh[kk][:], 0.0), [], [TXh])
              DVE(lambda e: e.tensor_copy(out=zp[:, :, 0:16], in_=zph[:]), [Tzph], [Tzp])
              for kv, XX in (("k", Xk), ("v", Xv)):
                  for g in range(2):
                      POOL(lambda e: e.tensor_copy(out=XX[g][:, 0:17], in_=Xh[(kv, g)][:]), [TXh], [TX])
                      POOL(lambda e: e.memset(XX[g][:, 272:273], 0.0), [], [TX])

              DVE(lambda e: e.memset(qT[64:128, 0:4, :], 0.0), [], [TqT])
              DVE(lambda e: e.memset(qT[0:64, 4:8, :], 0.0), [], [TqT])
              for g in range(2):
                  DVE(lambda e: e.memset(selb[g][:, 64:128], 0.0), [], [Tsel[g]])
              slc = G % 2
              DMA(Cstt[slc][:], din["Cst"].ap()[:, 2 * G:2 * G + 2, :], [], [TCst[slc]])
              for tt_ in range(2):
                  ti_ = 2 * G + tt_
                  for m_ in ([0] if ti_ < 16 else [0, 1]):
                      src = AP(fC_s, 128 * (ti_ - 16 * m_) + 37, [[16, 128], [RC, 8], [1, 128]])
                      DMA(TCt[ti_ % 2][m_], src, [Tscr["fC"]], [TTC[ti_ % 2]], nowaw=(m_ > 0), track=True)
              wq_ = [load_ws(winA_s, Tscr["winA"], p_ * 256) for p_ in range(PFW)]
              DMA(wbb[0][:], wbp_s.ap()[:, :, 0:512], [Tscr["wbp"]], [Twbb[0]])
              DMA(wbb[1][:], wba_s.ap()[:, :, 0:512], [Tscr["wba"]], [Twbb[1]])
              norm_both(0, hT, ThT, xn2, Txn2, ss2, Tss2)
              if G == 0:
                  dump("hT", hT, ThT, [128, 8, TG], BF16)

              ck("A")
              hn_i = [0]

              def headnorm(P, TP, ncols, gcol, mult, epst, dst, Tdst):
                  ii = hn_i[0] % 2
                  hn_i[0] += 1
                  sq_, Tsq_, r1_, Tr1_ = sqs[ii], Tsqs[ii], r1s[ii], Tr1s[ii]
                  ACT(lambda e: e.activation(out=sq_[:, 0:ncols], in_=P[:, 0:ncols], func=AF.Square), [TP], [Tsq_])
                  P2, TP2 = bank()
                  PE(lambda e: e.matmul(P2[:, 0:ncols], lhsT=BD[:], rhs=sq_[:, 0:ncols], start=True, stop=True),
                     [Tsq_, Tc], [TP2])
                  DVE(lambda e: e.tensor_scalar(out=r1_[:, 0:ncols], in0=P2[:, 0:ncols], scalar1=mult, scalar2=epst,
                                                op0=ALU.mult, op1=ALU.add), [TP2], [Tr1_])
                  ACT(lambda e: e.activation(out=r1_[:, 0:ncols], in_=r1_[:, 0:ncols], func=AF.Sqrt), [Tr1_], [Tr1_])
                  DVE(lambda e: e.reciprocal(out=r1_[:, 0:ncols], in_=r1_[:, 0:ncols]), [Tr1_], [Tr1_])
                  if isinstance(dst, tuple):
                      for hh, d_ in enumerate(dst):
                          prr = slice(64 * hh, 64 * hh + 64)
                          DVE(lambda e: e.scalar_tensor_tensor(out=d_, in0=P[prr, 0:ncols], scalar=gcol[prr, :],
                                                               in1=r1_[prr, 0:ncols], op0=ALU.mult, op1=ALU.mult),
                              [TP, Tr1_, Tc], [Tdst])
                  else:
                      DVE(lambda e: e.scalar_tensor_tensor(out=dst, in0=P[:, 0:ncols], scalar=gcol, in1=r1_[:, 0:ncols],
                                                           op0=ALU.mult, op1=ALU.mult), [TP, Tr1_, Tc], [Tdst])

              for pc in range(7):
                  wt, Twt = wq_.pop(0)
                  if pc + PFW < 7:
                      wq_.append(load_ws(winA_s, Tscr["winA"], (pc + PFW) * 256))
                  for i in range(2):
                      ch = pc * 2 + i
                      P, TP = bank()
                      for k in range(8):
                          PE(lambda e: e.matmul(P[:, 0:TG], lhsT=wt[:, k, i * 128:(i + 1) * 128], rhs=hT[:, k, :],
                                                start=(k == 0), stop=(k == 7)), [Twt, ThT], [TP])
                      if ch < 4:
                          ACT(lambda e: e.copy(out=zp[:, ch, 16:272], in_=P[:, 0:TG]), [TP], [Tzp])
                      elif ch < 8:
                          headnorm(P, TP, TG, gq[:, 0:1], 1.0, 64 * EPS, (qT[0:64, ch - 4, :], qT[64:128, ch, :]), TqT)
                      elif ch == 8:
                          headnorm(P, TP, TG, gk[:, 1:2], 1.0 / 64, EPS, ksT[:, g0:g0 + TG], Tks)
                      elif ch == 9:
                          headnorm(P, TP, TG, gk[:, 2:3], 1.0 / 64, EPS, kwT[:, g0:g0 + TG], Tkw)
                      else:
                          XX = Xk if ch < 12 else Xv
                          g = (ch - 10) % 2
                          ACT(lambda e: e.copy(out=XX[g][0:64, 17:273], in_=P[0:64, 0:TG]), [TP], [TX])
                          DVE(lambda e: e.tensor_copy(out=XX[g][64:128, 16:272], in_=P[64:128, 0:TG]), [TP], [TX])
              for tt in range(2):
                  ti = 2 * G + tt
                  P, TP = bank()
                  for k in range(8):
                      PE(lambda e: e.matmul(P[:, 0:NB], lhsT=hT[:, k, tt * 128:(tt + 1) * 128], rhs=winB[:, k, :],
                                            start=(k == 0), stop=(k == 7)), [ThT, TwinB], [TP])
                  ACT(lambda e: e.copy(out=vsA[:, ti, :, 0:64], in_=P[:, 0:128].rearrange("p (g d) -> p g d", g=2)),
                      [TP], [Tvs])
                  DVE(lambda e: e.tensor_copy(out=vwA[:, ti, :, 0:64],
                                              in_=P[:, 128:256].rearrange("p (g d) -> p g d", g=2)), [TP], [Tvw])
                  ACT(lambda e: e.activation(out=gtt[tt][:, :], in_=P[:, 256:280], func=AF.Sigmoid), [TP], [Tgt[tt]])
              if G == 0:
                  dump("qT", qT, TqT, [128, 8, TG], BF16)
                  dump("ksT", ksT[:, 0:TG], Tks, [128, TG], BF16)

              ck("B")
              n_lo = 0 if G == 0 else 16 * G - 1
              n_hi = 16 * G + 15
              nn = n_hi - n_lo
              col0 = 17 + 16 * n_lo - g0
              for kv, XX in (("k", Xk), ("v", Xv)):
                  for g in range(2):
                      P, TP = bank()
                      for c in range(16):
                          PE(lambda e: e.matmul(P[:, 0:nn], lhsT=w1[kv][:, c, :],
                                                rhs=strided(XX[g], col0 + 2 * c, 16, nn),
                                                start=(c == 0), stop=(c == 15)), [Tw1, TX], [TP])
                      ACT(lambda e: e.activation(out=hid[kv][g][:, 0:nn], in_=P[:, 0:nn], func=AF.Gelu_apprx_tanh,
                                                 bias=cb[kv][:, 0:1]), [TP, Tw1], [Thid])
              P, TP = bank()
              for g in range(2):
                  PE(lambda e: e.matmul(P[:, 0:nn], lhsT=w2["k"][:, g, :], rhs=hid["k"][g][:, 0:nn],
                                        start=(g == 0), stop=(g == 1)), [Tw1, Thid], [TP])
              headnorm(P, TP, nn, gk[:, 0:1], 1.0 / 64, EPS, kcT[:, n_lo:n_hi], Tkc)
              P, TP = bank()
              for g in range(2):
                  PE(lambda e: e.matmul(P[:, 0:nn], lhsT=w2["v"][:, g, :], rhs=hid["v"][g][:, 0:nn],
                                        start=(g == 0), stop=(g == 1)), [Tw1, Thid], [TP])
              ACT(lambda e: e.copy(out=vcT[:, n_lo:n_hi], in_=P[:, 0:nn]), [TP], [Tvc])
              P, TP = bank()
              Pb = bfv(P)
              for m in range(2):
                  PE(lambda e: e.transpose(out=Pb[:, m * 128:(m + 1) * 128], in_=vcT[:, m * 128:(m + 1) * 128],
                                           identity=ident_b[:]), [Tvc, Tc], [TP])
              for m in range(2):
                  DVE(lambda e: e.tensor_copy(out=vcA[:, m, :, 0:64],
                                              in_=Pb[:, m * 128:(m + 1) * 128].rearrange("p (g d) -> p g d", g=2)),
                      [TP], [TvcA])
              DVE(lambda e: e.tensor_copy(out=zph[:], in_=zp[:, :, 256:272]), [Tzp], [Tzph])
              for kv, XX in (("k", Xk), ("v", Xv)):
                  for g in range(2):
                      POOL(lambda e: e.tensor_copy(out=Xh[(kv, g)][:], in_=XX[g][:, 256:273]), [TX], [TXh])
              if G == 0:
                  dump("kcT", kcT, Tkc, [128, 256], BF16)
                  dump("vcT", vcT, Tvc, [128, 256], BF16)

              ck("C")
              for tt in range(2):
                  ti = 2 * G + tt
                  qsl = slice(tt * 128, (tt + 1) * 128)
                  ms = [0] if ti < 16 else [0, 1]
                  tcs = ti % 2
                  accs = {}

                  def make_tasks(kind, g, j):
                      h = 4 * g + j
                      if kind == "cmp":
                          return [dict(kind=kind, g=g, j=j, h=h, bt=list(ms), isnear=True, first=True, last=True)]
                      if kind == "sel":
                          kts = list(range(ti + 1))
                          near = [kt for kt in kts if ti - kt <= 1]
                          far = [kt for kt in kts if ti - kt >= 2]
                      else:
                          kts = list(range(max(0, ti - 4), ti + 1))
                          near = [kt for kt in kts if ti - kt in (0, 1, 4)]
                          far = [kt for kt in kts if ti - kt in (2, 3)]
                      batches = [(far[i:i + 4], False) for i in range(0, len(far), 4)] + [(near, True)]
                      out_ = []
                      for bi_, (bt, isnear) in enumerate(batches):
                          out_.append(dict(kind=kind, g=g, j=j, h=h, bt=bt, isnear=isnear, first=(bi_ == 0),
                                           last=(bi_ == len(batches) - 1)))
                      return out_

                  def emit_scores(tk_):
                      kind, g, h, bt, isnear = tk_["kind"], tk_["g"], tk_["h"], tk_["bt"], tk_["isnear"]
                      P, TP = bank("sc")
                      if kind == "cmp":
                          for m in bt:
                              PE(lambda e: e.matmul(P[:, m * 128:(m + 1) * 128], lhsT=kcT[:, m * 128:(m + 1) * 128],
                                                    rhs=qT[:, h, qsl], start=True, stop=False), [Tkc, TqT], [TP])
                              PE(lambda e: e.matmul(P[:, m * 128:(m + 1) * 128], lhsT=Jb[:], rhs=TCt[tcs][m][:, h, :],
                                                    start=False, stop=True), [Tc, TTC[tcs]], [TP])
                          bias = zcol[:, 0:1]
                      else:
                          if kind == "sel":
                              KT, TK_ = ksT, Tks
                              fbias, nbias = b31m[:, h:h + 1], n256[:, 0:1]
                          else:
                              KT, TK_ = kwT, Tkw
                              fbias, nbias = b31c[:, h:h + 1], zcol[:, 0:1]
                          for i, kt in enumerate(bt):
                              rg = slice(i * 128, (i + 1) * 128)
                              ksl = slice(kt * 128, (kt + 1) * 128)
                              only = (kind == "win") and not isnear
                              PE(lambda e: e.matmul(P[:, rg], lhsT=KT[:, ksl], rhs=qT[:, h, qsl], start=True, stop=only),
                                 [TK_, TqT], [TP])
                              if kind == "sel":
                                  PE(lambda e: e.matmul(P[:, rg], lhsT=Eall[:, ksl], rhs=selT[g][:, :],
                                                        start=False, stop=(not isnear)), [Tc, TselT[g]], [TP])
                              if isnear:
                                  zi = {0: 0, 1: 1, 4: 2}[ti - kt]
                                  PE(lambda e: e.matmul(P[:, rg], lhsT=Jb[:], rhs=TZ[:, zi, h, :], start=False, stop=True),
                                     [Tc, TTZ], [TP])
                          bias = nbias if isnear else fbias
                      nb_ = len(bt)
                      pi_ = pti[0] % NPT
                      pti[0] += 1
                      if kind == "cmp" or (kind == "win" and isnear):
                          ACT(lambda e: e.activation(out=PTb[pi_][:, 0:nb_ * 128], in_=P[:, 0:nb_ * 128], func=AF.Exp),
                              [TP], [TPT[pi_]])
                      else:
                          ACT(lambda e: e.activation(out=PTb[pi_][:, 0:nb_ * 128], in_=P[:, 0:nb_ * 128], func=AF.Exp,
                                                     bias=bias), [TP, Tb31], [TPT[pi_]])
                      tk_["pt"] = pi_

                  def emit_pv(tk_):
                      kind, g, h, bt = tk_["kind"], tk_["g"], tk_["h"], tk_["bt"]
                      key = (kind, h)
                      if tk_["first"]:
                          accs[key] = bank("acc")
                      P2, TP2 = accs[key]
                      pi_ = tk_["pt"]
                      for i, kt in enumerate(bt):
                          st_ = tk_["first"] and i == 0
                          sp_ = tk_["last"] and i == len(bt) - 1
                          if kind == "cmp":
                              PE(lambda e: e.matmul(P2[:, 0:129], lhsT=PTb[pi_][:, kt * 128:(kt + 1) * 128],
                                                    rhs=vcA[:, kt, g, :], start=st_, stop=sp_), [TPT[pi_], TvcA], [TP2])
                          else:
                              VA, TV = (vsA, Tvs) if kind == "sel" else (vwA, Tvw)
                              PE(lambda e: e.matmul(P2[:, 0:65], lhsT=PTb[pi_][:, i * 128:(i + 1) * 128],
                                                    rhs=VA[:, kt, g, :], start=st_, stop=sp_), [TPT[pi_], TV], [TP2])
                      if tk_["last"]:
                          if kind == "cmp":
                              ACT(lambda e: e.copy(out=OC[:, h, :], in_=P2[:, 0:129]), [TP2], [TOC])
                          elif kind == "sel":
                              DVE(lambda e: e.tensor_copy(out=Os[:, h, :], in_=P2[:, 0:65]), [TP2], [TOs])
                          else:
                              ACT(lambda e: e.copy(out=Ow[:, h, :], in_=P2[:, 0:65]), [TP2], [TOw])

                  LAGA = 3

                  def run_tasks(tasks):
                      for i_ in range(len(tasks) + LAGA):
                          if i_ < len(tasks):
                              emit_scores(tasks[i_])
                          if i_ >= LAGA:
                              emit_pv(tasks[i_ - LAGA])

                  run_tasks([t_ for g in range(2) for j in range(4) for t_ in make_tasks("cmp", g, j)])
                  ck("D1")
                  DVE(lambda e: e.tensor_scalar(out=cf[:, 0, :], in0=OC[:, :, 64], scalar1=1e-30, scalar2=None,
                                                op0=ALU.max), [TOC], [Tcf])
                  DVE(lambda e: e.reciprocal(out=cf[:, 1, :], in_=cf[:, 0, :]), [Tcf], [Tcf])
                  for g in range(2):
                      sc_, s2_, m8_, sb_ = sc64[g], score2[g], m8[g], selb[g]
                      DVE(lambda e: e.tensor_scalar(out=sc_[:, :], in0=OC[:, 4 * g, 65:129],
                                                    scalar1=cf[:, 1, 4 * g:4 * g + 1], scalar2=None, op0=ALU.mult),
                          [TOC, Tcf], [Tsel[g]])
                      for j in range(1, 4):
                          DVE(lambda e: e.scalar_tensor_tensor(out=sc_[:, :], in0=OC[:, 4 * g + j, 65:129],
                                                               scalar=cf[:, 1, 4 * g + j:4 * g + j + 1], in1=sc_[:, :],
                                                               op0=ALU.mult, op1=ALU.add), [TOC, Tcf, Tsel[g]], [Tsel[g]])
                      DVE(lambda e: e.tensor_tensor(out=sc_[:, :], in0=sc_[:, :], in1=Cstt[slc][:, tt, :], op=ALU.add),
                          [Tsel[g], TCst[slc]], [Tsel[g]])
                      DVE(lambda e: e.max(out=m8_[:, 0:8], in_=sc_[:, :]), [Tsel[g]], [Tsel[g]])
                      DVE(lambda e: e.match_replace(out=s2_[:, :], in_to_replace=m8_[:, 0:8], in_values=sc_[:, :],
                                                    imm_value=-1e30), [Tsel[g]], [Tsel[g]])
                      DVE(lambda e: e.max(out=m8_[:, 8:16], in_=s2_[:, :]), [Tsel[g]], [Tsel[g]])
                      DVE(lambda e: e.tensor_scalar(out=sb_[:, 0:64], in0=sc_[:, :], scalar1=m8_[:, 15:16], scalar2=256.0,
                                                    op0=ALU.is_ge, op1=ALU.mult), [Tsel[g]], [Tsel[g]])
                  if G == 0 and tt == 0:
                      dump("OC", OC, TOC, [128, 8, 129], F32)
                  ck("D2")
                  run_tasks([t_ for g in range(2) for j in range(4) for t_ in make_tasks("win", g, j)])
                  for g in range(2):
                      P, TP = bank("sc")
                      Pb = bfv(P)
                      PE(lambda e: e.transpose(out=Pb[:, 0:128], in_=selb[g][:, :], identity=ident_b[:]),
                         [Tsel[g], Tc], [TP])
                      ACT(lambda e: e.copy(out=selT[g][:, :], in_=Pb[:, 0:128]), [TP], [TselT[g]])
                  run_tasks([t_ for g in range(2) for j in range(4) for t_ in make_tasks("sel", g, j)])
                  ck("D3")
                  gtv = gtt[tt].rearrange("p (h b) -> p h b", b=3)
                  for bi, (O_, TO_) in enumerate(((OC, TOC), (Os, TOs), (Ow, TOw))):
                      DVE(lambda e: e.tensor_scalar(out=cf[:, 2, :], in0=O_[:, :, 64], scalar1=1e-30, scalar2=None,
                                                    op0=ALU.max), [TO_], [Tcf])
                      DVE(lambda e: e.reciprocal(out=cf[:, 2, :], in_=cf[:, 2, :]), [Tcf], [Tcf])
                      DVE(lambda e: e.tensor_tensor(out=cf[:, 3 + bi, :], in0=cf[:, 2, :], in1=gtv[:, :, bi], op=ALU.mult),
                          [Tcf, Tgt[tt]], [Tcf])
                  for bi, (O_, TO_) in enumerate(((OC, TOC), (Os, TOs), (Ow, TOw))):
                      cfb = cf[:, 3 + bi, :].unsqueeze(2).broadcast_to([128, 8, 64])
                      dst = yacc if bi == 0 else ytmp
                      DVE(lambda e: e.tensor_tensor(out=dst, in0=O_[:, :, 0:64], in1=cfb, op=ALU.mult),
                          [TO_, Tcf], [Tyacc])
                      if bi == 1:
                          DVE(lambda e: e.tensor_tensor(out=yacc, in0=yacc, in1=ytmp, op=ALU.add), [Tyacc], [Tyacc])
                      if bi == 2:
                          DVE(lambda e: e.tensor_tensor(out=ya.rearrange("p (h d) -> p h d", h=8), in0=yacc, in1=ytmp,
                                                        op=ALU.add), [Tyacc], [Tya])
                  if G == 0 and tt == 0:
                      dump("ya", ya, Tya, [128, 512], BF16)
                      dump("Os", Os, TOs, [128, 8, 65], F32)
                      dump("Ow", Ow, TOw, [128, 8, 65], F32)
                  P, TP = bank()
                  Pb = bfv(P)
                  for c in range(4):
                      PE(lambda e: e.transpose(out=Pb[:, c * 128:(c + 1) * 128], in_=ya[:, c * 128:(c + 1) * 128],
                                               identity=ident_b[:]), [Tya, Tc], [TP])
                  ACT(lambda e: e.copy(out=yaT[:, :, qsl], in_=Pb[:, 0:512].rearrange("p (c t) -> p c t", c=4)),
                      [TP], [TyaT])

              ck("D")
              for ci in range(4):
                  wdw = 2 ** (ci + 1)
                  a_ap, Ta = zp[:, ci, :], Tzp
                  for stp in range(ci + 1):
                      sh = 2 ** stp
                      lo = 2 ** (stp + 1) - 1
                      d_ap, Td = ptmp[stp % 2], Tptmp[stp % 2]
                      DVE(lambda e: e.tensor_tensor(out=d_ap[:, lo:272], in0=a_ap[:, lo:272], in1=a_ap[:, lo - sh:272 - sh],
                                                    op=ALU.add), [Ta], [Td])
                      a_ap, Ta = d_ap, Td
                  DVE(lambda e: e.scalar_tensor_tensor(out=pooled[:, ci, :], in0=a_ap[:, 16:272], scalar=1.0 / wdw,
                                                       in1=zp[:, ci, 16:272], op0=ALU.mult, op1=ALU.subtract),
                      [Ta, Tzp], [Tpooled])
                  if G == 0:
                      DVE(lambda e: e.tensor_tensor(out=tmp16[:, :], in0=a_ap[:, 16:32], in1=invc16[:, ci, :], op=ALU.mult),
                          [Ta, Tc], [Tt1])
                      DVE(lambda e: e.tensor_tensor(out=pooled[:, ci, 0:16], in0=tmp16[:, :], in1=zp[:, ci, 16:32],
                                                    op=ALU.subtract), [Tt1, Tzp], [Tpooled])
                  P, TP = bank()
                  PE(lambda e: e.matmul(P[:, 0:TG], lhsT=wpool[:, ci, :], rhs=pooled[:, ci, :], start=True, stop=True),
                     [Twpool, Tpooled], [TP])
                  ACT(lambda e: e.activation(out=ypT[:, ci, :], in_=P[:, 0:TG], func=AF.Copy, scale=pscale[:, ci:ci + 1]),
                      [TP, Tc], [TypT])
              if G == 0:
                  dump("ypT", ypT, TypT, [128, 4, TG], BF16)
                  dump("yaT", yaT, TyaT, [128, 4, TG], BF16)
              def load_mw(half_, pr2_):
                  return [load_ws(winA_s, Tscr["winA"], 1792 + gi_ * 1024 + half_ * 512 + pr2_ * 256) for gi_ in range(2)]

              mw_req = [(h_, p_) for h_ in range(2) for p_ in range(2)]
              mw_q = [load_mw(0, 0)]
              wo_all = None
              for half in range(2):
                  if half == 1:
                      DMA(wbb[0][:], wbp_s.ap()[:, :, half * 512:(half + 1) * 512], [Tscr["wbp"]], [Twbb[0]])
                      DMA(wbb[1][:], wba_s.ap()[:, :, half * 512:(half + 1) * 512], [Tscr["wba"]], [Twbb[1]])
                  for pr2 in range(2):
                      mw = mw_q.pop(0)
                      idx_ = half * 2 + pr2
                      if idx_ + 1 < 4:
                          mw_q.append(load_mw(*mw_req[idx_ + 1]))
                      else:
                          wo_all = [[load_ws(wout_s, Tscr["wout"], h_ * 512 + i_ * 256) for i_ in range(2)]
                                    for h_ in range(2)]
                      for i in range(2):
                          mc = half * 4 + pr2 * 2 + i
                          lc = pr2 * 2 + i
                          for gi in range(2):
                              P, TP = bank()
                              for k in range(8):
                                  PE(lambda e: e.matmul(P[:, 0:TG], lhsT=mw[gi][0][:, k, i * 128:(i + 1) * 128],
                                                        rhs=hT[:, k, :], start=(k == 0), stop=(k == 7)),
                                     [mw[gi][1], ThT], [TP])
                              ACT(lambda e: e.activation(out=mgt[gi][:, :], in_=P[:, 0:TG], func=AF.Sigmoid),
                                  [TP], [Tmgt[gi]])
                          Pa, TPa = bank()
                          for c in range(4):
                              PE(lambda e: e.matmul(Pa[:, 0:TG], lhsT=wbb[0][:, c, lc * 128:(lc + 1) * 128],
                                                    rhs=ypT[:, c, :], start=(c == 0), stop=(c == 3)),
                                 [Twbb[0], TypT], [TPa])
                          Pb_, TPb = bank()
                          for c in range(4):
                              PE(lambda e: e.matmul(Pb_[:, 0:TG], lhsT=wbb[1][:, c, lc * 128:(lc + 1) * 128],
                                                    rhs=yaT[:, c, :], start=(c == 0), stop=(c == 3)),
                                 [Twbb[1], TyaT], [TPb])
                          DVE(lambda e: e.tensor_tensor(out=t1[:, :], in0=Pa[:, 0:TG], in1=mgt[0][:, :], op=ALU.mult),
                              [TPa, Tmgt[0]], [Tt1])
                          DVE(lambda e: e.tensor_tensor(out=sq[:, :], in0=Pb_[:, 0:TG], in1=mgt[1][:, :], op=ALU.mult),
                              [TPb, Tmgt[1]], [Tsq])
                          DVE(lambda e: e.tensor_tensor(out=mixT[:, mc, :], in0=t1[:, :], in1=sq[:, :], op=ALU.add),
                              [Tt1, Tsq], [TmixT])
              if G == 0:
                  dump("mixT", mixT, TmixT, [128, 8, TG], BF16)
              ck("E1")
              for half in range(2):
                  wo = wo_all[half]
                  for tt in range(2):
                      P, TP = bank()
                      for i in range(2):
                          for k in range(8):
                              PE(lambda e: e.matmul(P[:, i * 256:(i + 1) * 256], lhsT=mixT[:, k, tt * 128:(tt + 1) * 128],
                                                    rhs=wo[i][0][:, k, :], start=(k == 0), stop=(k == 7)),
                                 [wo[i][1], TmixT], [TP])
                      DVE(lambda e: e.tensor_tensor(out=xt[tt][:, half * 512:(half + 1) * 512], in0=P[:, 0:512],
                                                    in1=xin[tt][:, half * 512:(half + 1) * 512], op=ALU.add),
                          [TP, Txin[tt]], [Txt[tt]])
              if G + 1 < NG:
                  load_x(G + 1)
              if G == 0:
                  dump("x1", xt[0][:], Txt[0], [128, DM], F32)
              ck("E")
              wq_ = [load_ws(wq_s, Tscr["wq"], p_ * 256) for p_ in range(PFW)]
              norm_both(1, h2T, Th2, xn2, Txn2, ss2, Tss2)
              if G == 0:
                  dump("h2T", h2T[:], Th2, [128, 8, TG], BF16)
              S.barrier()

              ck("F")
              carve_reset()
              pqT = carve([16, TG], BF16); Tpq = Tk("pqT")
              scs = carve([16, 128], F32); Tscs = Tk("scs")
              m1 = carve([16, 16], F32); i1 = carve([16, 16], U32); i1f = carve([16, 16], F32); Tm1 = Tk("m1")
              tmpr = carve([16, 128], F32)
              cand = carve([8, 16, 16], F32); Tcand = Tk("cand")
              ctmp = carve([8, 256], F32)
              eq = carve([128, 16], F32); Teq = Tk("eq")
              ts_ = carve([8, 16], F32); tj = carve([8, 16], U32); ta = carve([8, 16], U32); tb_ = carve([8, 16], U32)
              af_ = carve([128], F32); bf_ = carve([128], F32)
              If_ = carve([128], F32); Jf_ = carve([128], F32); ex = carve([8, 16], F32); gf = carve([8, 16], F32)
              s8 = carve([8], F32)
              Tts = Tk("ts")
              for pc in range(8):
                  wt, Twt = wq_.pop(0)
                  if pc + PFW < 8:
                      wq_.append(load_ws(wq_s, Tscr["wq"], (pc + PFW) * 256))
                  for i in range(2):
                      c = 2 * pc + i
                      P, TP = bank()
                      for k in range(8):
                          PE(lambda e: e.matmul(P[:, 0:TG], lhsT=wt[:, k, i * 128:(i + 1) * 128], rhs=h2T[:, k, :],
                                                start=(k == 0), stop=(k == 7)), [Twt, Th2], [TP])
                      ACT(lambda e: e.copy(out=pqT[:, c, :], in_=P[:, 0:TG]), [TP], [Tpq])
              for tt in range(2):
                  tsl = slice(tt * 128, (tt + 1) * 128)
                  for cbk in range(4):
                      P, TP = bank()
                      for i in range(4):
                          c = cbk * 4 + i
                          PE(lambda e: e.matmul(P[:, i * 128:(i + 1) * 128], lhsT=pqT[:, c, tsl], rhs=skT[:, c, :],
                                                start=True, stop=True), [Tpq, Tsk], [TP])
                      ACT(lambda e: e.copy(out=scs[:, cbk * 4:(cbk + 1) * 4, :],
                                           in_=P[:, 0:512].rearrange("p (c k) -> p c k", c=4)), [TP], [Tscs])
                  Ta_ = [Tk("m1a%d" % c) for c in range(16)]
                  Tb_ = [Tk("m1b%d" % c) for c in range(16)]
                  Tr_ = [Tk("tmpr%d" % c) for c in range(16)]
                  for c in range(16):
                      DVE(lambda e: e.max(out=m1[:, c, 0:8], in_=scs[:, c, :]), [Tscs], [Ta_[c]])
                  for c in range(16):
                      DVE(lambda e: e.match_replace(out=tmpr[:, c, :], in_to_replace=m1[:, c, 0:8], in_values=scs[:, c, :],
                                                    imm_value=-1e30), [Tscs, Ta_[c]], [Tr_[c]])
                  for c in range(16):
                      DVE(lambda e: e.max_index(out=i1[:, c, 0:8], in_max=m1[:, c, 0:8], in_values=scs[:, c, :]),
                          [Tscs, Ta_[c]], [Ta_[c]])
                  for c in range(16):
                      DVE(lambda e: e.max(out=m1[:, c, 8:16], in_=tmpr[:, c, :]), [Tr_[c]], [Tb_[c]])
                  for c in range(16):
                      DVE(lambda e: e.max_index(out=i1[:, c, 8:16], in_max=m1[:, c, 8:16], in_values=tmpr[:, c, :]),
                          [Tr_[c], Tb_[c]], [Tb_[c]])
                  TM = Ta_ + Tb_
                  DVE(lambda e: e.tensor_copy(out=i1f, in_=i1), TM, [Tm1])
                  m1v = m1.rearrange("p (h two) a -> p h two a", two=2)
                  i1v = i1f.rearrange("p (h two) a -> p h two a", two=2)
                  DVE(lambda e: e.tensor_tensor(out=cand, in0=m1v[:, :, 0, :].unsqueeze(3).broadcast_to([128, 8, 16, 16]),
                                                in1=m1v[:, :, 1, :].unsqueeze(2).broadcast_to([128, 8, 16, 16]),
                                                op=ALU.add), TM, [Tcand])
                  Tha = [Tk("tsa%d" % h) for h in range(8)]
                  Thb = [Tk("tsb%d" % h) for h in range(8)]
                  Thc = [Tk("ctmp%d" % h) for h in range(8)]
                  cvs = [cand[:, h, :, :].rearrange("p a b -> p (a b)") for h in range(8)]
                  for h in range(8):
                      DVE(lambda e: e.max(out=ts_[:, h, 0:8], in_=cvs[h]), [Tcand], [Tha[h]])
                  for h in range(8):
                      DVE(lambda e: e.match_replace(out=ctmp[:, h, :], in_to_replace=ts_[:, h, 0:8], in_values=cvs[h],
                                                    imm_value=-1e30), [Tcand, Tha[h]], [Thc[h]])
                  for h in range(8):
                      DVE(lambda e: e.max_index(out=tj[:, h, 0:8], in_max=ts_[:, h, 0:8], in_values=cvs[h]),
                          [Tcand, Tha[h]], [Tha[h]])
                  for h in range(8):
                      DVE(lambda e: e.max(out=ts_[:, h, 8:16], in_=ctmp[:, h, :]), [Thc[h]], [Thb[h]])
                  for h in range(8):
                      DVE(lambda e: e.max_index(out=tj[:, h, 8:16], in_max=ts_[:, h, 8:16], in_values=ctmp[:, h, :]),
                          [Thc[h], Thb[h]], [Thb[h]])
                  DVE(lambda e: e.tensor_single_scalar(out=ta, in_=tj, scalar=4, op=ALU.logical_shift_right),
                      Tha + Thb, [Tts])
                  DVE(lambda e: e.tensor_single_scalar(out=tb_, in_=tj, scalar=15, op=ALU.bitwise_and), [Tts], [Tts])
                  DVE(lambda e: e.tensor_copy(out=af_[:, :], in_=ta.rearrange("p h r -> p (h r)")), [Tts], [Tts])
                  DVE(lambda e: e.tensor_copy(out=bf_[:, :], in_=tb_.rearrange("p h r -> p (h r)")), [Tts], [Tts])
                  for (src_f, half_i, dstf) in ((af_, 0, If_), (bf_, 1, Jf_)):
                      DVE(lambda e: e.tensor_tensor(out=eq, in0=src_f[:, :].unsqueeze(2).broadcast_to([128, 128, 16]),
                                                    in1=iota16[:, :].unsqueeze(1).broadcast_to([128, 128, 16]),
                                                    op=ALU.is_equal), [Tts, Tc], [Teq])
                      eq4 = eq.rearrange("p (h r) a -> p h r a", h=8)
                      DVE(lambda e: e.tensor_tensor(out=eq4, in0=eq4,
                                                    in1=i1v[:, :, half_i, :].unsqueeze(2).broadcast_to([128, 8, 16, 16]),
                                                    op=ALU.mult), [Teq, Tm1], [Teq])
                      DVE(lambda e: e.tensor_reduce(out=dstf[:, :], in_=eq, axis=AX.X, op=ALU.add), [Teq], [Tts])
                  DVE(lambda e: e.tensor_tensor(out=ex, in0=ts_, in1=ts_[:, :, 0:1].broadcast_to([128, 8, 16]),
                                                op=ALU.subtract), [Tts], [Tts])
                  ACT(lambda e: e.activation(out=ex, in_=ex, func=AF.Exp), [Tts], [Tts])
                  DVE(lambda e: e.tensor_reduce(out=s8[:, :], in_=ex, axis=AX.X, op=ALU.add), [Tts], [Tts])
                  DVE(lambda e: e.reciprocal(out=s8[:, :], in_=s8[:, :]), [Tts], [Tts])
                  DVE(lambda e: e.tensor_tensor(out=gf, in0=ex, in1=s8[:, :].unsqueeze(2).broadcast_to([128, 8, 16]),
                                                op=ALU.mult), [Tts], [Tts])
                  P, TP = bank()
                  for i, srcT in enumerate((If_[:, :], Jf_[:, :], gf.rearrange("p h r -> p (h r)"))):
                      PE(lambda e: e.transpose(out=P[:, i * 128:(i + 1) * 128], in_=srcT, identity=ident_f[:]),
                         [Tts, Tc], [TP])
                  ACT(lambda e: e.copy(out=ITt[:, tsl], in_=P[:, 0:128]), [TP], [Tijg])
                  ACT(lambda e: e.copy(out=JTt[:, tsl], in_=P[:, 128:256]), [TP], [Tijg])
                  ACT(lambda e: e.copy(out=GTt[:, tsl], in_=P[:, 256:384]), [TP], [Tijg])
                  if G == 0 and tt == 0:
                      dump("If", If_, Tts, [128, 128], F32)
                      dump("skT", skT, Tsk, [128, 16, 128], BF16)
                      dump("pqT", pqT, Tpq, [128, 16, TG], BF16)
                      dump("scs", scs, Tscs, [128, 16, 128], F32)
                      dump("af", af_, Tts, [128, 128], F32)
                      dump("bf", bf_, Tts, [128, 128], F32)
                      dump("tj", tj, Tts, [128, 8, 16], U32)
                      dump("ts", ts_, Tts, [128, 8, 16], F32)
                      dump("m1", m1, Tm1, [128, 16, 16], F32)
                      dump("i1f", i1f, Tm1, [128, 16, 16], F32)
                      dump("Jf", Jf_, Tts, [128, 128], F32)
                      dump("gf", gf, Tts, [128, 8, 16], F32)
              S.barrier()

              ck("P2")
              carve_reset()
              W = carve([128, TG], BF16)
              TW = Tk("W")

              def flat_(t_):
                  a_ = t_[:]
                  return a_.rearrange("p a b -> p (a b)")

              NUS = 5
              uslots = [uvu[0][:], uvu[1][:], uvu[2][:],
                        flat_(ws[0]).rearrange("p (j k i) -> p j k i", j=2, k=8),
                        flat_(ws[2]).rearrange("p (j k i) -> p j k i", j=2, k=8)]
              vslots = [uvv[0][:], uvv[1][:], uvv[2][:],
                        flat_(ws[1]).rearrange("p (j d) -> p j d", j=2),
                        flat_(wbb[0]).rearrange("p (j d) -> p j d", j=2)]
              PFD = 3

              def load_uv(jp):
                  sl = jp % NUS
                  DMA(uslots[sl], uT_s.ap()[jp], [Tscr["uT"]], [Tus[sl]])
                  DMA(vslots[sl], v_s.ap()[jp], [Tscr["v"]], [Tus[sl]], nowaw=True)

              for jp_ in range(PFD):
                  load_uv(jp_)
              for t4 in range(TG // 4):
                  P, TP = bank("hi")
                  for i in range(4):
                      t = 4 * t4 + i
                      sl = t % NLR
                      DVE(lambda e: e.tensor_scalar(out=Lb[sl][:], in0=iotaB[:], scalar1=ITt[:, t:t + 1],
                                                    scalar2=GTt[:, t:t + 1], op0=ALU.is_equal, op1=ALU.mult),
                          [Tc, Tijg], [TL[sl]])
                      DVE(lambda e: e.tensor_scalar(out=Rb[sl][:], in0=iotaB[:], scalar1=JTt[:, t:t + 1], scalar2=None,
                                                    op0=ALU.is_equal), [Tc, Tijg], [TR[sl]])
                      PE(lambda e: e.matmul(P[:, i * 128:(i + 1) * 128], lhsT=Lb[sl][:], rhs=Rb[sl][:], start=True, stop=True),
                         [TL[sl], TR[sl]], [TP])
                  ACT(lambda e: e.copy(out=W[:, :, 4 * t4:4 * t4 + 4], in_=P[:, 0:512].rearrange("p (t j) -> p j t", t=4)),
                      [TP], [TW])


              def stage_a(j):
                  jp, jj = j // 2, j % 2
                  sl = jp % NUS
                  if jj == 0 and jp + PFD < 64:
                      load_uv(jp + PFD)
                  s2 = j % NGA
                  P, TP = bank("hi")
                  for k in range(8):
                      PE(lambda e: e.matmul(P[:, 0:TG], lhsT=uslots[sl][:, jj, k, :], rhs=h2T[:, k, :],
                                            start=(k == 0), stop=(k == 7)), [Tus[sl], Th2], [TP])
                  ACT(lambda e: e.activation(out=gab[s2][:], in_=P[:, 0:TG], func=AF.Gelu_apprx_tanh), [TP], [Tga[s2]])
                  DVE(lambda e: e.tensor_tensor(out=wab[s2][:], in0=gab[s2][:], in1=W[:, j, :], op=ALU.mult),
                      [Tga[s2], TW], [Twa[s2]])

              def stage_b(j):
                  jp, jj = j // 2, j % 2
                  sl = jp % NUS
                  s2 = j % NGA
                  for tt in range(2):
                      for half in range(2):
                          bi = tt * 2 + half
                          PE(lambda e: e.matmul(pb[bi][:, 0:512], lhsT=wab[s2][:, tt * 128:(tt + 1) * 128],
                                                rhs=vslots[sl][:, jj, half * 512:(half + 1) * 512],
                                                start=(j == 0), stop=(j == 127)), [Twa[s2], Tus[sl]], [Tpb[bi]])

              LAG = 2
              for j in range(128 + LAG):
                  if j < 128:
                      stage_a(j)
                  if j >= LAG:
                      stage_b(j - LAG)
              for tt in range(2):
                  for half in range(2):
                      bi = tt * 2 + half
                      DVE(lambda e: e.tensor_tensor(out=xt[tt][:, half * 512:(half + 1) * 512], in0=pb[bi][:, 0:512],
                                                    in1=xt[tt][:, half * 512:(half + 1) * 512], op=ALU.add),
                          [Tpb[bi], Txt[tt]], [Txt[tt]])
                  DMA(out_d.ap()[g0 + tt * 128:g0 + (tt + 1) * 128, :], xt[tt][:], [Txt[tt]], [Tout], nowaw=True, semtk=Txt[tt])
              S.barrier()
        except _Stop:
            pass
        S.wait_all("sp", [Tout])
        S.wait_all("act", [Tout])
        counts = {k: v["n"] for k, v in S.eng.items()}
    return nc, dbg, counts


_CACHE = {}


def kernel(**inputs):
    if "nc" not in _CACHE:
        _CACHE["nc"] = build()[0]
        _CACHE["consts"] = host_consts()
    nc = _CACHE["nc"]
    consts = _CACHE["consts"]
    inputs = {k: np.asarray(v) for k, v in inputs.items()}
    in_maps = [make_in_map(inputs, b, consts) for b in range(8)]
    res = run_bass_kernel_spmd(nc, in_maps, core_ids=list(range(8)))
    out = np.stack([np.asarray(r["out"], np.float32) for r in res.results], axis=0)
    return out
```

```python
import math
from contextlib import ExitStack
import numpy as np
import ml_dtypes
import concourse.bass as bass
import concourse.mybir as mybir
from concourse.bass_utils import run_bass_kernel_spmd
from concourse.bass_types import AP

F32 = mybir.dt.float32
BF16 = mybir.dt.bfloat16
U32 = mybir.dt.uint32
AF = mybir.ActivationFunctionType
ALU = mybir.AluOpType
AX = mybir.AxisListType
NPBF = ml_dtypes.bfloat16

SEQ = 4096
DM = 1024
TG = 256
NGROUPS = SEQ // TG
EPS = 1e-6
NEG = -30000.0
RW = 768
RC = 6200
RC_OFF = 2100
NA = 3840
NB = 280


class Tk:
    __slots__ = ("name", "w", "r", "dsem", "dcnt")

    def __init__(self, name):
        self.name = name
        self.w = {}
        self.r = {}
        self.dsem = None
        self.dcnt = 0


class Sync:
    SEM_MAX = 20000

    def __init__(self, nc, stack):
        self.nc = nc
        self.stack = stack
        self.eng = {}
        self.nsem = 0
        self.pending = {}
        self.track_all = True
        for name, e in [("pe", nc.tensor), ("act", nc.scalar), ("dve", nc.vector),
                        ("pool", nc.gpsimd), ("sp", nc.sync)]:
            self.eng[name] = dict(e=e, sem=self._newsem(name), cnt=0, waited={}, name=name, n=0)

    def _newsem(self, name):
        self.nsem += 1
        return self.stack.enter_context(self.nc.semaphore("s%d_%s" % (self.nsem, name)))

    def _wait(self, E, s, v):
        k = id(s)
        if E["waited"].get(k, 0) >= v:
            return
        E["e"].wait_ge(s, v)
        E["waited"][k] = v

    def _deps(self, E, reads, writes, nowaw=False, skip_own=False):
        deps = {}

        def add(tok):
            if tok is None:
                return
            s, v = tok
            k = id(s)
            if k not in deps or deps[k][1] < v:
                deps[k] = (s, v)
        for t in reads:
            for tok in t.w.values():
                add(tok)
        for t in writes:
            if not nowaw:
                for tok in t.w.values():
                    add(tok)
            for tok in t.r.values():
                add(tok)
        for k, (s, v) in deps.items():
            if skip_own and s is E["sem"]:
                continue
            self._wait(E, s, v)

    def _mark(self, tok, reads, writes, nowaw=False):
        k = id(tok[0])
        for t in reads:
            t.r[k] = tok
        for t in writes:
            if nowaw:
                t.w[k] = tok
            else:
                t.w = {k: tok}
                t.r = {}

    def op(self, en, fn, reads=(), writes=()):
        E = self.eng[en]
        self._deps(E, reads, writes, skip_own=(en == "pe"))
        if E["cnt"] >= self.SEM_MAX:
            E["sem"] = self._newsem(en)
            E["cnt"] = 0
        ins = fn(E["e"])
        E["cnt"] += 1
        E["n"] += 1
        ins.then_inc(E["sem"], 1)
        E["last"] = (E["sem"], E["cnt"])
        self._mark((E["sem"], E["cnt"]), reads, writes)
        return ins

    def dma(self, qn, out, in_, reads=(), writes=(), nowaw=False, track=False, semtk=None, **kw):
        Q = self.eng[qn]
        self._deps(Q, reads, writes, nowaw=nowaw)
        t0 = semtk if semtk is not None else writes[0]
        if t0.dsem is None or t0.dcnt >= self.SEM_MAX:
            t0.dsem = self._newsem("d_" + t0.name)
            t0.dcnt = 0
        ins = Q["e"].dma_start(out=out, in_=in_, **kw)
        t0.dcnt += 16
        Q["n"] += 1
        ins.then_inc(t0.dsem, 16)
        if track or self.track_all:
            self.pending[id(t0.dsem)] = (t0.dsem, t0.dcnt)
        self._mark((t0.dsem, t0.dcnt), reads, writes, nowaw=nowaw)
        return ins

    def wait_all(self, en, tks):
        self._deps(self.eng[en], tks, tks)

    def barrier(self):
        names = ["pe", "act", "dve", "pool"]
        for a in names + ["sp"]:
            for b in names:
                if a != b and self.eng[b].get("last") is not None:
                    self._wait(self.eng[a], *self.eng[b]["last"])
            for (sm, v) in self.pending.values():
                self._wait(self.eng[a], sm, v)
        self.pending = {}


def _bucket(n):
    n = int(n)
    if n < 16:
        return n
    nf = np.float32(n)
    v = np.log(nf / np.float32(16)) / np.float32(math.log(128 / 16)) * np.float32(16)
    return min(31, 16 + int(np.float32(v)))


def host_consts():
    c = {}
    c["ident_f"] = np.eye(128, dtype=np.float32)
    c["ident_b"] = np.eye(128, dtype=np.float32).astype(NPBF)
    c["J_b"] = np.eye(128, dtype=np.float32)[::-1].copy().astype(NPBF)
    bd = np.zeros((128, 128), np.float32)
    bd[:64, :64] = 1
    bd[64:, 64:] = 1
    c["BD_b"] = bd.astype(NPBF)
    c["iotaB"] = np.tile(np.arange(128, dtype=np.float32), (128, 1)).astype(NPBF)
    c["iota16"] = np.tile(np.arange(16, dtype=np.float32), (128, 1))
    c["ones_row"] = np.ones((1, 128), np.float32)
    ohw = np.zeros((33, RW), np.float32)
    for i in range(RW):
        r = i - 128
        if r < 0 or r >= 512:
            ohw[32, i] = 1
        else:
            ohw[_bucket(r), i] = 1
    c["ohw"] = ohw
    ohc = np.zeros((33, RC), np.float32)
    for i in range(RC):
        r = i - RC_OFF
        if r < 0:
            ohc[32, i] = 1
        else:
            ohc[_bucket(r) if r < 128 else 31, i] = 1
    c["ohc"] = ohc
    smap = np.zeros((256, 64), np.float32)
    for n in range(255):
        for p in range(32):
            smap[n, (16 * n + p) // 64] += 1.0 / 32
    c["smapc"] = smap.reshape(2, 128, 64).transpose(1, 0, 2).copy().astype(NPBF)
    E = np.zeros((128, 4096), np.float32)
    E[np.arange(4096) // 64, np.arange(4096)] = 1
    c["Eall"] = E.astype(NPBF)
    t = np.arange(4096)[:, None]
    s = np.arange(64)[None, :]
    cur = t // 64
    forced = (s == 0) | (s == cur) | (s == cur - 1)
    vis = (s * 64) <= t
    C = np.where(vis, np.where(forced, 1000.0, 0.0), -1.0).astype(np.float32)
    c["Cst"] = C.reshape(32, 128, 64).transpose(1, 0, 2).copy()
    inv = np.zeros((128, 4, 16), np.float32)
    for ci, w in enumerate((2, 4, 8, 16)):
        for tt in range(16):
            inv[:, ci, tt] = 1.0 / min(tt + 1, w)
    c["invc16"] = inv
    return c


CONST_SPECS = {
    "ident_f": ([128, 128], F32), "ident_b": ([128, 128], BF16), "J_b": ([128, 128], BF16),
    "BD_b": ([128, 128], BF16), "iotaB": ([128, 128], BF16), "iota16": ([128, 16], F32),
    "ones_row": ([1, 128], F32), "ohw": ([33, RW], F32), "ohc": ([33, RC], F32),
    "smapc": ([128, 2, 64], BF16), "Eall": ([128, 4096], BF16), "Cst": ([128, 32, 64], F32),
    "invc16": ([128, 4, 16], F32),
}

INPUT_SPECS = {
    "x": [SEQ, DM], "cT": [128, 8], "rel_bias": [32, 8], "ada_w": [DM, 6 * DM], "adabT": [128, 48],
    "n1g": [128, 8], "n2g": [128, 8], "w_in": [DM, 3864], "pool_w": [4, 128, 128], "pscale": [128, 4],
    "pek": [128, 16], "w1k": [2048, 128], "w2k": [128, 64], "pev": [128, 16], "w1v": [2048, 128],
    "w2v": [128, 64], "gq": [128, 1], "gk": [128, 3], "wbp": [512, DM], "wba": [512, DM],
    "wout": [DM, DM], "wq": [DM, 2048], "subk": [8, 2, 128, 128], "pu": [16384, DM], "pv": [16384, DM],
}


def colT(v, k):
    return np.ascontiguousarray(np.asarray(v, np.float32).reshape(k, 128).T)


def make_in_map(inputs, b, consts):
    m = {}
    m["x"] = np.ascontiguousarray(inputs["x"][b])
    m["cT"] = colT(inputs["c"][b], 8)
    m["rel_bias"] = np.ascontiguousarray(inputs["rel_bias"])
    m["ada_w"] = np.ascontiguousarray(inputs["ada_w"][0])
    m["adabT"] = colT(inputs["ada_b"][0], 48)
    m["n1g"] = colT(inputs["norm1_g"][0], 8)
    m["n2g"] = colT(inputs["norm2_g"][0], 8)
    m["w_in"] = np.ascontiguousarray(inputs["w_in"][0])
    m["pool_w"] = np.ascontiguousarray(inputs["pool_w"][0])
    m["pscale"] = colT(inputs["pool_scale"][0], 4)
    m["pek"] = colT(inputs["cmp_pe_k"][0].reshape(-1), 16)
    m["w1k"] = np.ascontiguousarray(inputs["cmp_w1_k"][0])
    m["w2k"] = np.ascontiguousarray(inputs["cmp_w2_k"][0])
    m["pev"] = colT(inputs["cmp_pe_v"][0].reshape(-1), 16)
    m["w1v"] = np.ascontiguousarray(inputs["cmp_w1_v"][0])
    m["w2v"] = np.ascontiguousarray(inputs["cmp_w2_v"][0])
    gq = np.asarray(inputs["q_norm_g"][0], np.float32)
    m["gq"] = np.ascontiguousarray(np.concatenate([gq, gq]).reshape(128, 1))
    gk = np.asarray(inputs["k_norm_g"][0], np.float32)
    m["gk"] = np.ascontiguousarray(np.concatenate([gk, gk], axis=1).T)
    m["wbp"] = np.ascontiguousarray(inputs["w_branch_pool"][0])
    m["wba"] = np.ascontiguousarray(inputs["w_branch_attn"][0])
    m["wout"] = np.ascontiguousarray(inputs["w_out"][0])
    m["wq"] = np.ascontiguousarray(inputs["peer_w_q"][0])
    m["subk"] = np.ascontiguousarray(inputs["peer_sub_keys"][0])
    m["pu"] = np.ascontiguousarray(inputs["peer_u"][0])
    m["pv"] = np.ascontiguousarray(inputs["peer_v"][0])
    for k in m:
        m[k] = np.asarray(m[k], np.float32)
    m.update(consts)
    return m


class _Stop(Exception):
    pass


def build(NG=NGROUPS, debug=False, prepass=True, stop=None):
    nc = bass.Bass("TRN2", target_bir_lowering=False)
    din = {}
    for k, shp in INPUT_SPECS.items():
        din[k] = nc.dram_tensor(k, list(shp), F32, kind="ExternalInput")
    for k, (shp, dt) in CONST_SPECS.items():
        din[k] = nc.dram_tensor(k, list(shp), dt, kind="ExternalInput")
    out_d = nc.dram_tensor("out", [SEQ, DM], F32, kind="ExternalOutput")
    winA_s = nc.dram_tensor("winA_s", [128, 8, NA], BF16, kind="Internal")
    winB_s = nc.dram_tensor("winB_s", [128, 8, NB], BF16, kind="Internal")
    wbp_s = nc.dram_tensor("wbp_s", [128, 4, DM], BF16, kind="Internal")
    wba_s = nc.dram_tensor("wba_s", [128, 4, DM], BF16, kind="Internal")
    wout_s = nc.dram_tensor("wout_s", [128, 8, DM], BF16, kind="Internal")
    wq_s = nc.dram_tensor("wq_s", [128, 8, 2048], BF16, kind="Internal")
    uT_s = nc.dram_tensor("uT_s", [64, 128, 2, 8, 128], BF16, kind="Internal")
    v_s = nc.dram_tensor("v_s", [64, 128, 2, DM], BF16, kind="Internal")
    fW_s = nc.dram_tensor("fW_s", [8, RW], BF16, kind="Internal")
    fC_s = nc.dram_tensor("fC_s", [8, RC], BF16, kind="Internal")
    dbg = {}

    with ExitStack() as st:
        S = Sync(nc, st)

        def ck(name):
            if stop == name:
                raise _Stop()

        def sb(name, shape, dt):
            return st.enter_context(nc.sbuf_tensor("s_" + name, list(shape), dt))

        def PE(fn, r, w):
            return S.op("pe", fn, r, w)

        def ACT(fn, r, w):
            return S.op("act", fn, r, w)

        def DVE(fn, r, w):
            return S.op("dve", fn, r, w)

        def POOL(fn, r, w):
            return S.op("pool", fn, r, w)

        def DMA(out, in_, r, w, nowaw=False, q="sp", track=False, semtk=None):
            return S.dma(q, out, in_, r, w, nowaw=nowaw, track=track, semtk=semtk)

        flip = [0]

        def EV(fn, r, w):
            flip[0] ^= 1
            if flip[0]:
                return S.op("act", lambda e: fn(e, True), r, w)
            return S.op("dve", lambda e: fn(e, False), r, w)

        def copy_ev(out, in_, r, w):
            return EV(lambda e, a: (e.copy(out=out, in_=in_) if a else e.tensor_copy(out=out, in_=in_)), r, w)

        pb = [st.enter_context(nc.psum_tensor("pb%d" % i, [128, 512], F32)) for i in range(8)]
        Tpb = [Tk("pb%d" % i) for i in range(8)]
        rot = {"all": [list(range(8)), 0], "acc": [[0, 1, 2], 0], "sc": [[3, 4, 5, 6, 7], 0],
               "hi": [[4, 5, 6, 7], 0]}

        def bank(pool="all"):
            lst, i = rot[pool]
            rot[pool][1] = (i + 1) % len(lst)
            b = lst[i]
            return pb[b], Tpb[b]

        def bfv(P):
            return P[:].bitcast(BF16)

        Tc = Tk("consts")

        def cload(name, shape, dt, src):
            t = sb(name, shape, dt)
            DMA(t[:], src, [], [Tc], nowaw=True)
            return t

        ident_f = cload("ident_f", [128, 128], F32, din["ident_f"].ap())
        ident_b = cload("ident_b", [128, 128], BF16, din["ident_b"].ap())
        Jb = cload("J_b", [128, 128], BF16, din["J_b"].ap())
        BD = cload("BD_b", [128, 128], BF16, din["BD_b"].ap())
        iotaB = cload("iotaB", [128, 128], BF16, din["iotaB"].ap())
        iota16 = cload("iota16", [128, 16], F32, din["iota16"].ap())
        Eall = cload("Eall", [128, 4096], BF16, din["Eall"].ap())
        invc16 = cload("invc16", [128, 4, 16], F32, din["invc16"].ap())
        n1g = cload("n1g", [128, 8], F32, din["n1g"].ap())
        n2g = cload("n2g", [128, 8], F32, din["n2g"].ap())
        pscale = cload("pscale", [128, 4], F32, din["pscale"].ap())
        gq = cload("gq", [128, 1], F32, din["gq"].ap())
        gk = cload("gk", [128, 3], F32, din["gk"].ap())

        ksT = sb("ksT", [128, SEQ], BF16)
        kwT = sb("kwT", [128, SEQ], BF16)
        Tks, Tkw = Tk("ksT"), Tk("kwT")
        vsA = sb("vsA", [128, 32, 2, 65], BF16)
        vwA = sb("vwA", [128, 32, 2, 65], BF16)
        Tvs, Tvw = Tk("vsA"), Tk("vwA")
        kcT = sb("kcT", [128, 256], BF16)
        vcT = sb("vcT", [128, 256], BF16)
        vcA = sb("vcA", [128, 2, 2, 129], BF16)
        Tkc, Tvc, TvcA = Tk("kcT"), Tk("vcT"), Tk("vcA")
        TZ = sb("TZ", [128, 3, 8, 128], BF16)
        TTZ = Tk("TZ")
        skT = sb("skT", [128, 16, 128], BF16)
        Tsk = Tk("skT")
        w1 = {"k": sb("w1k", [128, 16, 128], BF16), "v": sb("w1v", [128, 16, 128], BF16)}
        w2 = {"k": sb("w2k", [128, 2, 128], BF16), "v": sb("w2v", [128, 2, 128], BF16)}
        cb = {"k": sb("cbk", [128, 1], F32), "v": sb("cbv", [128, 1], F32)}
        Tw1 = Tk("w1")
        winB = sb("winB", [128, 8, NB], BF16)
        TwinB = Tk("winB")
        wpool = sb("wpool", [128, 4, 128], BF16)
        Twpool = Tk("wpool")
        adaT = sb("adaT", [128, 96], F32)
        TadaT = Tk("adaT")
        gmod = [sb("gmod1", [128, 8], F32), sb("gmod2", [128, 8], F32)]
        Tgmod = Tk("gmod")
        b31c = sb("b31c", [128, 8], F32)
        b31m = sb("b31m", [128, 8], F32)
        n256 = sb("n256", [128, 1], F32)
        zcol = sb("zcol", [128, 1], F32)
        Tb31 = Tk("b31")
        xt = [sb("xt0", [128, DM], F32), sb("xt1", [128, DM], F32)]
        Txt = [Tk("xt0"), Tk("xt1")]
        xin = [sb("xin0", [128, DM], F32), sb("xin1", [128, DM], F32)]
        Txin = [Tk("xin0"), Tk("xin1")]
        h2T = sb("h2T", [128, 8, TG], BF16)
        Th2 = Tk("h2T")
        ITt = sb("ITt", [128, TG], F32)
        JTt = sb("JTt", [128, TG], F32)
        GTt = sb("GTt", [128, TG], F32)
        Tijg = Tk("ijg")
        NLR = 4
        Lb = [sb("L%d" % i, [128, 128], BF16) for i in range(NLR)]
        Rb = [sb("R%d" % i, [128, 128], BF16) for i in range(NLR)]
        TL = [Tk("L%d" % i) for i in range(NLR)]
        TR = [Tk("R%d" % i) for i in range(NLR)]
        NGA = 3
        gab = [sb("ga%d" % i, [128, TG], BF16) for i in range(NGA)]
        wab = [sb("wa%d" % i, [128, TG], BF16) for i in range(NGA)]
        Tga = [Tk("ga%d" % i) for i in range(NGA)]
        Twa = [Tk("wa%d" % i) for i in range(NGA)]
        NWS = 3
        ws = [sb("ws%d" % i, [128, 8, 256], BF16) for i in range(NWS)]
        Tws = [Tk("ws%d" % i) for i in range(NWS)]
        wsi = [0]
        wbb = [sb("wbb%d" % i, [128, 4, 512], BF16) for i in range(2)]
        Twbb = [Tk("wbb%d" % i) for i in range(2)]
        NUV = 3
        uvu = [sb("uvu%d" % i, [128, 2, 8, 128], BF16) for i in range(NUV)]
        uvv = [sb("uvv%d" % i, [128, 2, DM], BF16) for i in range(NUV)]
        Tuv = [Tk("uv%d" % i) for i in range(NUV)]
        Tus = [Tk("us%d" % i) for i in range(5)]
        TTC = [Tk("TC0"), Tk("TC1")]
        Cstt = [sb("Cst%d" % i, [128, 2, 64], F32) for i in range(2)]
        TCst = [Tk("Cst%d" % i) for i in range(2)]
        arena = sb("arena", [128, 32768], BF16)
        Tout = Tk("out")
        Tscr = {k: Tk(k) for k in ("winA", "winB", "wbp", "wba", "wout", "wq", "uT", "v", "fW", "fC")}

        def adacol(which, k):
            i = (which * 8 + k) * 2
            return adaT[:, i:i + 1]

        def strided(tile, col, step, n):
            base = tile[:, col:col + 1]
            return AP(base.tensor, base.offset, [list(base.ap[0]), [step, n]])

        aoff = [0]

        def carve_reset(base=0):
            aoff[0] = base

        def carve(free_shape, dt):
            n = int(np.prod(free_shape))
            nb = n * (2 if dt == BF16 else 4)
            nb4 = (nb + 3) // 4 * 4
            a = aoff[0]
            aoff[0] += nb4
            assert aoff[0] <= 65536, ("arena overflow", aoff[0])
            v = arena[:, a // 2:(a + nb4) // 2]
            if dt != BF16:
                v = v.bitcast(dt)
            v = v[:, 0:n]
            if len(free_shape) == 2:
                v = v.rearrange("p (a b) -> p a b", a=free_shape[0])
            elif len(free_shape) == 3:
                v = v.rearrange("p (a b c) -> p a b c", a=free_shape[0], b=free_shape[1])
            return v

        try:
          if True:
              carve_reset()
              gbc = [carve([DM], F32), carve([DM], F32)]
              Tgbc = Tk("gbc")
              base1 = aoff[0]
              ones_row = carve([128], F32)
              DMA(ones_row[0:1, :], din["ones_row"].ap(), [], [Tc], nowaw=True)
              cT = carve([8], F32)
              DMA(cT, din["cT"].ap(), [], [Tc], nowaw=True)
              adabT = carve([48], F32)
              DMA(adabT, din["adabT"].ap(), [], [Tc], nowaw=True)
              csil = carve([8, 2], F32)
              Tcs = Tk("csil")
              for dd in range(2):
                  ACT(lambda e: e.activation(out=csil[:, :, dd], in_=cT, func=AF.Silu), [Tc], [Tcs])
              adawt = [carve([8, 512], F32) for i in range(2)]
              Tadaw = [Tk("adaw%d" % i) for i in range(2)]
              adaw_v = din["ada_w"].ap().rearrange("(k p) n -> p k n", p=128)
              PA, TPA = bank()
              for n in range(12):
                  sl = n % 2
                  DMA(adawt[sl], adaw_v[:, :, n * 512:(n + 1) * 512], [], [Tadaw[sl]])
                  for fc in range(4):
                      i = n * 4 + fc
                      for k in range(8):
                          PE(lambda e: e.matmul(PA[:, 2 * i:2 * i + 2], lhsT=adawt[sl][:, k, fc * 128:(fc + 1) * 128],
                                                rhs=csil[:, k, :], start=(k == 0), stop=(k == 7)), [Tcs, Tadaw[sl]], [TPA])
              av = adaT[:, :].rearrange("p (i t) -> p i t", t=2)
              pv_ = PA[:, 0:96].rearrange("p (i t) -> p i t", t=2)
              for dd in range(2):
                  DVE(lambda e: e.tensor_tensor(out=av[:, :, dd], in0=pv_[:, :, dd], in1=adabT, op=ALU.add),
                      [TPA, Tc], [TadaT])
              tmp8 = carve([8], F32)
              Ttmp8 = Tk("tmp8")
              for li, (which, gn) in enumerate(((1, n1g), (4, n2g))):
                  sv = av[:, which * 8:(which + 1) * 8, 0]
                  DVE(lambda e: e.tensor_scalar(out=tmp8, in0=sv, scalar1=1.0, scalar2=None, op0=ALU.add),
                      [TadaT], [Ttmp8])
                  DVE(lambda e: e.tensor_tensor(out=gmod[li][:], in0=tmp8, in1=gn[:], op=ALU.mult),
                      [Ttmp8, Tc], [Tgmod])
              onesq = carve([128], F32)
              Tonesq = Tk("onesq")
              DVE(lambda e: e.memset(onesq, 1.0), [], [Tonesq])
              dg = [carve([128], F32), carve([128], F32)]
              Tdg = [Tk("dg0"), Tk("dg1")]
              for gi, which in enumerate((2, 5)):
                  for k in range(8):
                      d_ = dg[k % 2]
                      DVE(lambda e: e.tensor_scalar(out=d_, in0=ident_f[:], scalar1=adacol(which, k), scalar2=None,
                                                    op0=ALU.mult), [Tc, TadaT], [Tdg[k % 2]])
                      P, TP = bank()
                      PE(lambda e: e.matmul(P[:, 0:128], lhsT=onesq, rhs=d_, start=True, stop=True),
                         [Tonesq, Tdg[k % 2]], [TP])
                      copy_ev(gbc[gi][:, k * 128:(k + 1) * 128], P[:, 0:128], [TP], [Tgbc])
              S.barrier()

              ck("ada")
              carve_reset(base1)
              ones_row = carve([128], F32)
              Tor = Tk("ones_row")
              DMA(ones_row[0:1, :], din["ones_row"].ap(), [], [Tor])
              tab = carve([8], F32)
              Ttab = Tk("tab")
              DVE(lambda e: e.memset(tab[0:64, :], NEG), [], [Ttab])
              DMA(tab[0:32, :], din["rel_bias"].ap(), [], [Ttab])
              r31 = carve([8], F32)
              DMA(r31[0:1, :], din["rel_bias"].ap()[31:32, :], [], [Tor], nowaw=True)
              ohw = carve([RW], F32)
              DMA(ohw[0:33, :], din["ohw"].ap(), [], [Tor], nowaw=True)
              ohc = carve([RC], F32)
              DMA(ohc[0:33, :], din["ohc"].ap(), [], [Tor], nowaw=True)
              fwsb = carve([RW], BF16)
              fcsb = carve([RC], BF16)
              Tfw, Tfc = Tk("fwsb"), Tk("fcsb")
              for (oh, fsb, Tf, R) in ((ohw, fwsb, Tfw, RW), (ohc, fcsb, Tfc, RC)):
                  for c0 in range(0, R, 512):
                      c1 = min(R, c0 + 512)
                      P, TP = bank()
                      PE(lambda e: e.matmul(P[0:8, 0:c1 - c0], lhsT=tab[0:33, 0:8], rhs=oh[0:33, c0:c1],
                                            start=True, stop=True), [Ttab, Tor], [TP])
                      copy_ev(fsb[0:8, c0:c1], P[0:8, 0:c1 - c0], [TP], [Tf])
              DMA(fW_s.ap(), fwsb[0:8, :], [Tfw], [Tscr["fW"]])
              DMA(fC_s.ap(), fcsb[0:8, :], [Tfc], [Tscr["fC"]])
              for i, off in enumerate((1, 129, 513)):
                  src = AP(fW_s, off, [[1, 128], [RW, 8], [1, 128]])
                  DMA(TZ[:, i, :, :], src, [Tscr["fW"]], [TTZ], nowaw=True)
              P, TP = bank()
              PE(lambda e: e.matmul(P[:, 0:8], lhsT=ones_row[0:1, 0:128], rhs=r31[0:1, 0:8], start=True, stop=True),
                 [Tor], [TP])
              ACT(lambda e: e.copy(out=b31c[:], in_=P[:, 0:8]), [TP], [Tb31])
              DVE(lambda e: e.tensor_scalar(out=b31m[:], in0=P[:, 0:8], scalar1=-256.0, scalar2=None, op0=ALU.add),
                  [TP], [Tb31])
              DVE(lambda e: e.memset(n256[:], -256.0), [], [Tb31])
              DVE(lambda e: e.memset(zcol[:], 0.0), [], [Tb31])

              ck("t5")
              DVE(lambda e: e.memset(vsA[:], 1.0), [], [Tvs])
              DVE(lambda e: e.memset(vwA[:], 1.0), [], [Tvw])
              DVE(lambda e: e.memset(kcT[:], 0.0), [], [Tkc])
              DVE(lambda e: e.memset(vcT[:], 0.0), [], [Tvc])
              DVE(lambda e: e.memset(vcA[:], 1.0), [], [TvcA])
              for g in range(2):
                  DMA(vcA[:, :, g, 65:129], din["smapc"].ap(), [], [TvcA])
              S.barrier()

              ck("misc")
              carve_reset(base1)
              stg = carve([16, 128], F32)
              Tstg = Tk("stg")
              pe_c = {"k": carve([16], F32), "v": carve([16], F32)}
              pe_b = {"k": carve([16], BF16), "v": carve([16], BF16)}
              stg2 = carve([64], F32)
              Tstg2 = Tk("stg2")
              for kv in ("k", "v"):
                  DMA(stg, din["w1" + kv].ap().rearrange("(c p) h -> p c h", p=128), [], [Tstg])
                  DVE(lambda e: e.tensor_copy(out=w1[kv][:], in_=stg), [Tstg], [Tw1])
                  DMA(stg2, din["w2" + kv].ap(), [], [Tstg2])
                  DVE(lambda e: e.memset(w2[kv][:], 0.0), [], [Tw1])
                  for g in range(2):
                      DVE(lambda e: e.tensor_copy(out=w2[kv][:, g, 64 * g:64 * g + 64], in_=stg2), [Tstg2], [Tw1])
                  DMA(pe_c[kv], din["pe" + kv].ap(), [], [Tstg2])
                  DVE(lambda e: e.tensor_copy(out=pe_b[kv], in_=pe_c[kv]), [Tstg2], [Tw1])
                  P, TP = bank()
                  for c in range(16):
                      PE(lambda e: e.matmul(P[:, 0:1], lhsT=w1[kv][:, c, :], rhs=pe_b[kv][:, c:c + 1],
                                            start=(c == 0), stop=(c == 15)), [Tw1], [TP])
                  ACT(lambda e: e.copy(out=cb[kv][:], in_=P[:, 0:1]), [TP], [Tw1])
              DMA(stg[:, 0:4, :], din["pool_w"].ap().rearrange("g c d -> c g d"), [], [Tstg])
              DVE(lambda e: e.tensor_copy(out=wpool[:], in_=stg[:, 0:4, :]), [Tstg], [Twpool])
              DMA(stg, din["subk"].ap().rearrange("h p k d -> k (h p) d"), [], [Tstg])
              for cbk in range(4):
                  P, TP = bank()
                  for i in range(4):
                      c = cbk * 4 + i
                      PE(lambda e: e.transpose(out=P[:, i * 128:(i + 1) * 128], in_=stg[:, c, :], identity=ident_f[:]),
                         [Tstg, Tc], [TP])
                  copy_ev(skT[:, cbk * 4:(cbk + 1) * 4, :], P[:, 0:512].rearrange("p (c k) -> p c k", c=4), [TP], [Tsk])
              S.barrier()

              ck("small")
              carve_reset(base1)
              cvf = [carve([8, 512], F32) for i in range(2)]
              cvb = [carve([8, 512], BF16) for i in range(2)]
              Tcvf = [Tk("cvf%d" % i) for i in range(2)]
              Tcvb = [Tk("cvb%d" % i) for i in range(2)]
              cvi = [0]

              def conv(src_v, KC, N, runs, scale_bc=None):
                  for p0 in range(0, N, 512):
                      p1 = min(N, p0 + 512)
                      w = p1 - p0
                      sl = cvi[0] % 2
                      cvi[0] += 1
                      DMA(cvf[sl][:, 0:KC, 0:w], src_v[:, :, p0:p1], [], [Tcvf[sl]])
                      if scale_bc is None:
                          copy_ev(cvb[sl][:, 0:KC, 0:w], cvf[sl][:, 0:KC, 0:w], [Tcvf[sl]], [Tcvb[sl]])
                      else:
                          for k in range(KC):
                              DVE(lambda e: e.tensor_tensor(out=cvb[sl][:, k, 0:w], in0=cvf[sl][:, k, 0:w],
                                                            in1=scale_bc[:, p0:p1], op=ALU.mult),
                                  [Tcvf[sl], Tgbc], [Tcvb[sl]])
                      for (s0, n, dst, d0, Td) in runs:
                          a = max(s0, p0)
                          b = min(s0 + n, p1)
                          if a < b:
                              DMA(dst.ap()[:, :, d0 + a - s0: d0 + b - s0], cvb[sl][:, 0:KC, a - p0:b - p0],
                                  [Tcvb[sl]], [Td], nowaw=True, semtk=Tcvb[sl])

              runs = [(0, 512, winA_s, 0, Tscr["winA"])]
              for h in range(8):
                  pos = 2 * h if h < 4 else 2 * (h - 4) + 1
                  runs.append((512 + 64 * h, 64, winA_s, 512 + 64 * pos, Tscr["winA"]))
              for g in range(2):
                  for dup in range(2):
                      runs.append((1024 + 64 * g, 64, winA_s, 1280 + 128 * g + 64 * dup, Tscr["winA"]))
                      runs.append((1152 + 64 * g, 64, winA_s, 1536 + 128 * g + 64 * dup, Tscr["winA"]))
              runs.append((1280, 128, winA_s, 1024, Tscr["winA"]))
              runs.append((1536, 128, winA_s, 1152, Tscr["winA"]))
              runs.append((1816, 2048, winA_s, 1792, Tscr["winA"]))
              runs.append((1408, 128, winB_s, 0, Tscr["winB"]))
              runs.append((1664, 128, winB_s, 128, Tscr["winB"]))
              runs.append((1792, 24, winB_s, 256, Tscr["winB"]))
              conv(din["w_in"].ap().rearrange("(k p) n -> p k n", p=128), 8, 3864, runs)
              conv(din["wbp"].ap().rearrange("(k p) n -> p k n", p=128), 4, DM, [(0, DM, wbp_s, 0, Tscr["wbp"])])
              conv(din["wba"].ap().rearrange("(k p) n -> p k n", p=128), 4, DM, [(0, DM, wba_s, 0, Tscr["wba"])])
              conv(din["wout"].ap().rearrange("(k p) n -> p k n", p=128), 8, DM, [(0, DM, wout_s, 0, Tscr["wout"])],
                   scale_bc=gbc[0])
              conv(din["wq"].ap().rearrange("(k p) n -> p k n", p=128), 8, 2048, [(0, 2048, wq_s, 0, Tscr["wq"])])
              DMA(winB[:], winB_s.ap(), [Tscr["winB"]], [TwinB])
              S.barrier()

              ck("conv")
              carve_reset(base1)
              if prepass:
                  NPB = 3
                  ublk = [carve([2, DM], F32) for i in range(NPB)]
                  vblk = [carve([2, DM], F32) for i in range(NPB)]
                  uo = [uvu[i][:] for i in range(NPB)]
                  vo = [uvv[i][:] for i in range(NPB)]
                  Tub = [Tk("ublk%d" % i) for i in range(NPB)]
                  Tvb = [Tk("vblk%d" % i) for i in range(NPB)]
                  Tuo = [Tk("uo%d" % i) for i in range(NPB)]
                  Tvo = [Tk("vo%d" % i) for i in range(NPB)]
                  u_v = din["pu"].ap().rearrange("(i j) d -> i j d", j=128)
                  v_v = din["pv"].ap().rearrange("(i j) d -> i j d", j=128)
                  def pre_load(jp):
                      sl = jp % NPB
                      DMA(ublk[sl], u_v[:, 2 * jp:2 * jp + 2, :], [], [Tub[sl]])
                      DMA(vblk[sl], v_v[:, 2 * jp:2 * jp + 2, :], [], [Tvb[sl]])

                  pre_load(0)
                  pre_load(1)
                  for jp in range(64):
                      sl = jp % NPB
                      if jp + 2 < 64:
                          pre_load(jp + 2)
                      for jj in range(2):
                          for hb in range(2):
                              P, TP = bank()
                              for i in range(4):
                                  k = hb * 4 + i
                                  PE(lambda e: e.transpose(out=P[:, i * 128:(i + 1) * 128],
                                                           in_=ublk[sl][:, jj, k * 128:(k + 1) * 128],
                                                           identity=ident_f[:]), [Tub[sl], Tc], [TP])
                              copy_ev(uo[sl][:, jj, hb * 4:(hb + 1) * 4, :],
                                      P[:, 0:512].rearrange("p (k i) -> p k i", k=4), [TP], [Tuo[sl]])
                          POOL(lambda e: e.tensor_tensor(out=vo[sl][:, jj, :], in0=vblk[sl][:, jj, :], in1=gbc[1],
                                                         op=ALU.mult), [Tvb[sl], Tgbc], [Tvo[sl]])
                      DMA(uT_s.ap()[jp], uo[sl], [Tuo[sl]], [Tscr["uT"]], nowaw=True, semtk=Tuo[sl])
                      DMA(v_s.ap()[jp], vo[sl], [Tvo[sl]], [Tscr["v"]], nowaw=True, semtk=Tvo[sl])
              S.barrier()
          ck("pre")
          S.track_all = False
          def dump(name, ap_, tk, shape, dt=F32):
              if not debug:
                  return
              if not isinstance(ap_, AP):
                  ap_ = ap_[:]
              d = nc.dram_tensor("dbg_" + name, list(shape), dt, kind="ExternalOutput")
              dbg[name] = d
              DMA(d.ap(), ap_, [tk], [Tout], nowaw=True, track=True)

          def v8_(t_):
              a_ = t_[:]
              if len(a_.shape) == 4:
                  a_ = a_.rearrange("p a b c -> p (a b c)")
              else:
                  a_ = a_.rearrange("p a b -> p (a b)")
              return a_.rearrange("p (k n) -> p k n", k=8)

          wsl = [ws[i][:] for i in range(NWS)] + [v8_(uvu[i]) for i in range(3)] + [v8_(uvv[i]) for i in range(3)]
          Twsl = list(Tws) + [Tk("wsa%d" % i) for i in range(6)]
          NWSL = len(wsl)

          def ws_next():
              i = wsi[0] % NWSL
              wsi[0] += 1
              return wsl[i], Twsl[i]

          def load_x(G_):
              for tt_ in range(2):
                  DMA(xin[tt_][:], din["x"].ap()[G_ * TG + tt_ * 128:G_ * TG + (tt_ + 1) * 128, :], [], [Txin[tt_]])

          load_x(0)

          def norm_both(li, dstT, Tdst, xn2, Txn2, ss2, Tss2):
              shift_which = 0 if li == 0 else 3
              srcs = [((xin[tt], Txin[tt]) if li == 0 else (xt[tt], Txt[tt])) for tt in range(2)]
              for tt in range(2):
                  xs, Txs = srcs[tt]
                  ACT(lambda e: e.activation(out=xn2[tt][:, :], in_=xs[:], func=AF.Square, accum_out=ss2[tt][:, 0:1]),
                      [Txs], [Txn2[tt], Tss2[tt]])
              for tt in range(2):
                  DVE(lambda e: e.tensor_scalar(out=ss2[tt][:, 1:2], in0=ss2[tt][:, 0:1], scalar1=1.0 / DM, scalar2=EPS,
                                                op0=ALU.mult, op1=ALU.add), [Tss2[tt]], [Tss2[tt]])
              for tt in range(2):
                  ACT(lambda e: e.activation(out=ss2[tt][:, 2:3], in_=ss2[tt][:, 1:2], func=AF.Sqrt), [Tss2[tt]], [Tss2[tt]])
              for tt in range(2):
                  DVE(lambda e: e.reciprocal(out=ss2[tt][:, 3:4], in_=ss2[tt][:, 2:3]), [Tss2[tt]], [Tss2[tt]])
              for tt in range(2):
                  xs, Txs = srcs[tt]
                  if tt == 1:
                      ACT(lambda e: e.activation(out=xn2[tt][:, :], in_=xs[:], func=AF.Copy, scale=ss2[tt][:, 3:4]),
                          [Txs, Tss2[tt]], [Txn2[tt]])
                  else:
                      DVE(lambda e: e.tensor_scalar(out=xn2[tt][:, :], in0=xs[:], scalar1=ss2[tt][:, 3:4], scalar2=None,
                                                    op0=ALU.mult), [Txs, Tss2[tt]], [Txn2[tt]])
              banks_ = [bank() for _ in range(2)]
              for tt in range(2):
                  P, TP = banks_[tt]
                  Pb = bfv(P)
                  for k in range(8):
                      PE(lambda e: e.transpose(out=Pb[:, k * 128:(k + 1) * 128], in_=xn2[tt][:, k * 128:(k + 1) * 128],
                                               identity=ident_b[:]), [Txn2[tt], Tc], [TP])
              for tt in range(2):
                  P, TP = banks_[tt]
                  Pb = bfv(P)
                  for k in range(8):
                      o = dstT[:, k, tt * 128:(tt + 1) * 128]
                      i_ = Pb[:, k * 128:(k + 1) * 128]
                      gm = gmod[li][:, k:k + 1]
                      sh = adacol(shift_which, k)
                      EV(lambda e, a: (e.activation(out=o, in_=i_, func=AF.Identity, scale=gm, bias=sh) if a else
                                       e.tensor_scalar(out=o, in0=i_, scalar1=gm, scalar2=sh, op0=ALU.mult, op1=ALU.add)),
                         [TP, Tgmod, TadaT], [Tdst])

          def load_ws(scr, Tsc, c0):
              wt, Twt = ws_next()
              DMA(wt, scr.ap()[:, :, c0:c0 + 256], [Tsc], [Twt])
              return wt, Twt

          PFW = 3
          for G in range(NG):
              g0 = G * TG
              carve_reset()
              hT = carve([8, TG], BF16); ThT = Tk("hT")
              qT = carve([8, TG], BF16); TqT = Tk("qT")
              zp = carve([4, 272], F32); Tzp = Tk("zp")
              ptmp = [carve([272], F32), carve([272], F32)]; Tptmp = [Tk("pt0"), Tk("pt1")]
              pooled = carve([4, TG], BF16); Tpooled = Tk("pooled")
              ypT = carve([4, TG], BF16); TypT = Tk("ypT")
              yaT = carve([4, TG], BF16); TyaT = Tk("yaT")
              mixT = carve([8, TG], BF16); TmixT = Tk("mixT")
              Xk = [carve([273], BF16), carve([273], BF16)]
              Xv = [carve([273], BF16), carve([273], BF16)]
              TX = Tk("X")
              gtt = [carve([24], F32), carve([24], F32)]; Tgt = [Tk("gt0"), Tk("gt1")]
              OC = carve([8, 129], F32); TOC = Tk("OC")
              Os = carve([8, 65], F32); TOs = Tk("Os")
              Ow = carve([8, 65], F32); TOw = Tk("Ow")
              NPT = 4
              PTb = [carve([512], BF16) for _ in range(NPT)]; TPT = [Tk("PT%d" % i) for i in range(NPT)]
              pti = [0]
              TCt = [[carve([8, 128], BF16) for _ in range(2)] for _ in range(2)]
              xn2 = [carve([DM], BF16), carve([DM], BF16)]; Txn2 = [Tk("xn0"), Tk("xn1")]
              ss2 = [carve([4], F32), carve([4], F32)]; Tss2 = [Tk("ss0"), Tk("ss1")]
              sq = carve([TG], BF16); Tsq = Tk("sq")
              r1 = carve([TG], F32); Tr1 = Tk("r1")
              sqs = [sq, carve([TG], BF16)]; Tsqs = [Tsq, Tk("sq1")]
              r1s = [r1, carve([TG], F32)]; Tr1s = [Tr1, Tk("r11")]
              hid = {"k": [carve([16], BF16), carve([16], BF16)], "v": [carve([16], BF16), carve([16], BF16)]}
              Thid = Tk("hid")
              sc64 = [carve([64], F32), carve([64], F32)]; score2 = [carve([64], F32), carve([64], F32)]
              selb = [carve([128], BF16), carve([128], BF16)]
              m8 = [carve([16], F32), carve([16], F32)]; Tsel = [Tk("sel0"), Tk("sel1")]
              selT = [carve([128], BF16), carve([128], BF16)]; TselT = [Tk("selT0"), Tk("selT1")]
              cf = carve([6, 8], F32); Tcf = Tk("cf")
              yacc = carve([8, 64], F32); ytmp = carve([8, 64], F32); Tyacc = Tk("yacc")
              ya = carve([512], BF16); Tya = Tk("ya")
              mgt = [carve([TG], BF16) for _ in range(2)]; Tmgt = [Tk("mg0"), Tk("mg1")]
              t1 = carve([TG], F32); Tt1 = Tk("t1")
              tmp16 = carve([16], F32)

              if G == 0:
                  pass
              if G == 0:
                  zph = sb("zph", [128, 4, 16], F32); Tzph = Tk("zph")
                  Xh = {("k", 0): sb("Xhk0", [128, 17], BF16), ("k", 1): sb("Xhk1", [128, 17], BF16),
                        ("v", 0): sb("Xhv0", [128, 17], BF16), ("v", 1): sb("Xhv1", [128, 17], BF16)}
                  TXh = Tk("Xh")
                  DVE(lambda e: e.memset(zph[:], 0.0), [], [Tzph])
                  for kk in Xh:
                      DVE(lambda e: e.memset(Xh[kk][:], 0.0), [], [TXh])
              DVE(lambda e: e.tensor_copy(out=zp[:, :, 0:16], in_=zph[:]), [Tzph], [Tzp])
              for kv, XX in (("k", Xk), ("v", Xv)):
                  for g in range(2):
                      POOL(lambda e: e.tensor_copy(out=XX[g][:, 0:17], in_=Xh[(kv, g)][:]), [TXh], [TX])
                      POOL(lambda e: e.memset(XX[g][:, 272:273], 0.0), [], [TX])

              POOL(lambda e: e.memset(qT[64:128, 0:4, :], 0.0), [], [TqT])
              POOL(lambda e: e.memset(qT[0:64, 4:8, :], 0.0), [], [TqT])
              for g in range(2):
                  DVE(lambda e: e.memset(selb[g][:, 64:128], 0.0), [], [Tsel[g]])
              slc = G % 2
              DMA(Cstt[slc][:], din["Cst"].ap()[:, 2 * G:2 * G + 2, :], [], [TCst[slc]])
              for tt_ in range(2):
                  ti_ = 2 * G + tt_
                  for m_ in ([0] if ti_ < 16 else [0, 1]):
                      src = AP(fC_s, 128 * (ti_ - 16 * m_) + 37, [[16, 128], [RC, 8], [1, 128]])
                      DMA(TCt[ti_ % 2][m_], src, [Tscr["fC"]], [TTC[ti_ % 2]], nowaw=(m_ > 0), track=True)
              wq_ = [load_ws(winA_s, Tscr["winA"], p_ * 256) for p_ in range(PFW)]
              DMA(wbb[0][:], wbp_s.ap()[:, :, 0:512], [Tscr["wbp"]], [Twbb[0]])
              DMA(wbb[1][:], wba_s.ap()[:, :, 0:512], [Tscr["wba"]], [Twbb[1]])
              norm_both(0, hT, ThT, xn2, Txn2, ss2, Tss2)
              if G == 0:
                  dump("hT", hT, ThT, [128, 8, TG], BF16)

              ck("A")
              hn_i = [0]

              def headnorm(P, TP, ncols, gcol, mult, epst, dst, Tdst):
                  ii = hn_i[0] % 2
                  hn_i[0] += 1
                  sq_, Tsq_, r1_, Tr1_ = sqs[ii], Tsqs[ii], r1s[ii], Tr1s[ii]
                  ACT(lambda e: e.activation(out=sq_[:, 0:ncols], in_=P[:, 0:ncols], func=AF.Square), [TP], [Tsq_])
                  P2, TP2 = bank()
                  PE(lambda e: e.matmul(P2[:, 0:ncols], lhsT=BD[:], rhs=sq_[:, 0:ncols], start=True, stop=True),
                     [Tsq_, Tc], [TP2])
                  DVE(lambda e: e.tensor_scalar(out=r1_[:, 0:ncols], in0=P2[:, 0:ncols], scalar1=mult, scalar2=epst,
                                                op0=ALU.mult, op1=ALU.add), [TP2], [Tr1_])
                  ACT(lambda e: e.activation(out=r1_[:, 0:ncols], in_=r1_[:, 0:ncols], func=AF.Sqrt), [Tr1_], [Tr1_])
                  DVE(lambda e: e.reciprocal(out=r1_[:, 0:ncols], in_=r1_[:, 0:ncols]), [Tr1_], [Tr1_])
                  if isinstance(dst, tuple):
                      for hh, d_ in enumerate(dst):
                          prr = slice(64 * hh, 64 * hh + 64)
                          DVE(lambda e: e.scalar_tensor_tensor(out=d_, in0=P[prr, 0:ncols], scalar=gcol[prr, :],
                                                               in1=r1_[prr, 0:ncols], op0=ALU.mult, op1=ALU.mult),
                              [TP, Tr1_, Tc], [Tdst])
                  else:
                      DVE(lambda e: e.scalar_tensor_tensor(out=dst, in0=P[:, 0:ncols], scalar=gcol, in1=r1_[:, 0:ncols],
                                                           op0=ALU.mult, op1=ALU.mult), [TP, Tr1_, Tc], [Tdst])

              for pc in range(7):
                  wt, Twt = wq_.pop(0)
                  if pc + PFW < 7:
                      wq_.append(load_ws(winA_s, Tscr["winA"], (pc + PFW) * 256))
                  for i in range(2):
                      ch = pc * 2 + i
                      P, TP = bank()
                      for k in range(8):
                          PE(lambda e: e.matmul(P[:, 0:TG], lhsT=wt[:, k, i * 128:(i + 1) * 128], rhs=hT[:, k, :],
                                                start=(k == 0), stop=(k == 7)), [Twt, ThT], [TP])
                      if ch < 4:
                          ACT(lambda e: e.copy(out=zp[:, ch, 16:272], in_=P[:, 0:TG]), [TP], [Tzp])
                      elif ch < 8:
                          headnorm(P, TP, TG, gq[:, 0:1], 1.0, 64 * EPS, (qT[0:64, ch - 4, :], qT[64:128, ch, :]), TqT)
                      elif ch == 8:
                          headnorm(P, TP, TG, gk[:, 1:2], 1.0 / 64, EPS, ksT[:, g0:g0 + TG], Tks)
                      elif ch == 9:
                          headnorm(P, TP, TG, gk[:, 2:3], 1.0 / 64, EPS, kwT[:, g0:g0 + TG], Tkw)
                      else:
                          XX = Xk if ch < 12 else Xv
                          g = (ch - 10) % 2
                          ACT(lambda e: e.copy(out=XX[g][0:64, 17:273], in_=P[0:64, 0:TG]), [TP], [TX])
                          DVE(lambda e: e.tensor_copy(out=XX[g][64:128, 16:272], in_=P[64:128, 0:TG]), [TP], [TX])
              for tt in range(2):
                  ti = 2 * G + tt
                  P, TP = bank()
                  for k in range(8):
                      PE(lambda e: e.matmul(P[:, 0:NB], lhsT=hT[:, k, tt * 128:(tt + 1) * 128], rhs=winB[:, k, :],
                                            start=(k == 0), stop=(k == 7)), [ThT, TwinB], [TP])
                  ACT(lambda e: e.copy(out=vsA[:, ti, :, 0:64], in_=P[:, 0:128].rearrange("p (g d) -> p g d", g=2)),
                      [TP], [Tvs])
                  DVE(lambda e: e.tensor_copy(out=vwA[:, ti, :, 0:64],
                                              in_=P[:, 128:256].rearrange("p (g d) -> p g d", g=2)), [TP], [Tvw])
                  ACT(lambda e: e.activation(out=gtt[tt][:, :], in_=P[:, 256:280], func=AF.Sigmoid), [TP], [Tgt[tt]])
              if G == 0:
                  dump("qT", qT, TqT, [128, 8, TG], BF16)
                  dump("ksT", ksT[:, 0:TG], Tks, [128, TG], BF16)

              ck("B")
              n_lo = 0 if G == 0 else 16 * G - 1
              n_hi = 16 * G + 15
              nn = n_hi - n_lo
              col0 = 17 + 16 * n_lo - g0
              for kv, XX in (("k", Xk), ("v", Xv)):
                  for g in range(2):
                      P, TP = bank()
                      for c in range(16):
                          PE(lambda e: e.matmul(P[:, 0:nn], lhsT=w1[kv][:, c, :],
                                                rhs=strided(XX[g], col0 + 2 * c, 16, nn),
                                                start=(c == 0), stop=(c == 15)), [Tw1, TX], [TP])
                      ACT(lambda e: e.activation(out=hid[kv][g][:, 0:nn], in_=P[:, 0:nn], func=AF.Gelu_apprx_tanh,
                                                 bias=cb[kv][:, 0:1]), [TP, Tw1], [Thid])
              P, TP = bank()
              for g in range(2):
                  PE(lambda e: e.matmul(P[:, 0:nn], lhsT=w2["k"][:, g, :], rhs=hid["k"][g][:, 0:nn],
                                        start=(g == 0), stop=(g == 1)), [Tw1, Thid], [TP])
              headnorm(P, TP, nn, gk[:, 0:1], 1.0 / 64, EPS, kcT[:, n_lo:n_hi], Tkc)
              P, TP = bank()
              for g in range(2):
                  PE(lambda e: e.matmul(P[:, 0:nn], lhsT=w2["v"][:, g, :], rhs=hid["v"][g][:, 0:nn],
                                        start=(g == 0), stop=(g == 1)), [Tw1, Thid], [TP])
              ACT(lambda e: e.copy(out=vcT[:, n_lo:n_hi], in_=P[:, 0:nn]), [TP], [Tvc])
              P, TP = bank()
              Pb = bfv(P)
              for m in range(2):
                  PE(lambda e: e.transpose(out=Pb[:, m * 128:(m + 1) * 128], in_=vcT[:, m * 128:(m + 1) * 128],
                                           identity=ident_b[:]), [Tvc, Tc], [TP])
              for m in range(2):
                  DVE(lambda e: e.tensor_copy(out=vcA[:, m, :, 0:64],
                                              in_=Pb[:, m * 128:(m + 1) * 128].rearrange("p (g d) -> p g d", g=2)),
                      [TP], [TvcA])
              DVE(lambda e: e.tensor_copy(out=zph[:], in_=zp[:, :, 256:272]), [Tzp], [Tzph])
              for kv, XX in (("k", Xk), ("v", Xv)):
                  for g in range(2):
                      POOL(lambda e: e.tensor_copy(out=Xh[(kv, g)][:], in_=XX[g][:, 256:273]), [TX], [TXh])
              if G == 0:
                  dump("kcT", kcT, Tkc, [128, 256], BF16)
                  dump("vcT", vcT, Tvc, [128, 256], BF16)

              ck("C")
              for tt in range(2):
                  ti = 2 * G + tt
                  qsl = slice(tt * 128, (tt + 1) * 128)
                  ms = [0] if ti < 16 else [0, 1]
                  tcs = ti % 2
                  accs = {}

                  def make_tasks(kind, g, j):
                      h = 4 * g + j
                      if kind == "cmp":
                          return [dict(kind=kind, g=g, j=j, h=h, bt=list(ms), isnear=True, first=True, last=True)]
                      if kind == "sel":
                          kts = list(range(ti + 1))
                          near = [kt for kt in kts if ti - kt <= 1]
                          far = [kt for kt in kts if ti - kt >= 2]
                      else:
                          kts = list(range(max(0, ti - 4), ti + 1))
                          near = [kt for kt in kts if ti - kt in (0, 1, 4)]
                          far = [kt for kt in kts if ti - kt in (2, 3)]
                      batches = [(far[i:i + 4], False) for i in range(0, len(far), 4)] + [(near, True)]
                      out_ = []
                      for bi_, (bt, isnear) in enumerate(batches):
                          out_.append(dict(kind=kind, g=g, j=j, h=h, bt=bt, isnear=isnear, first=(bi_ == 0),
                                           last=(bi_ == len(batches) - 1)))
                      return out_

                  def emit_scores(tk_):
                      kind, g, h, bt, isnear = tk_["kind"], tk_["g"], tk_["h"], tk_["bt"], tk_["isnear"]
                      P, TP = bank("sc")
                      if kind == "cmp":
                          for m in bt:
                              PE(lambda e: e.matmul(P[:, m * 128:(m + 1) * 128], lhsT=kcT[:, m * 128:(m + 1) * 128],
                                                    rhs=qT[:, h, qsl], start=True, stop=False), [Tkc, TqT], [TP])
                              PE(lambda e: e.matmul(P[:, m * 128:(m + 1) * 128], lhsT=Jb[:], rhs=TCt[tcs][m][:, h, :],
                                                    start=False, stop=True), [Tc, TTC[tcs]], [TP])
                          bias = zcol[:, 0:1]
                      else:
                          if kind == "sel":
                              KT, TK_ = ksT, Tks
                              fbias, nbias = b31m[:, h:h + 1], n256[:, 0:1]
                          else:
                              KT, TK_ = kwT, Tkw
                              fbias, nbias = b31c[:, h:h + 1], zcol[:, 0:1]
                          for i, kt in enumerate(bt):
                              rg = slice(i * 128, (i + 1) * 128)
                              ksl = slice(kt * 128, (kt + 1) * 128)
                              only = (kind == "win") and not isnear
                              PE(lambda e: e.matmul(P[:, rg], lhsT=KT[:, ksl], rhs=qT[:, h, qsl], start=True, stop=only),
                                 [TK_, TqT], [TP])
                              if kind == "sel":
                                  PE(lambda e: e.matmul(P[:, rg], lhsT=Eall[:, ksl], rhs=selT[g][:, :],
                                                        start=False, stop=(not isnear)), [Tc, TselT[g]], [TP])
                              if isnear:
                                  zi = {0: 0, 1: 1, 4: 2}[ti - kt]
                                  PE(lambda e: e.matmul(P[:, rg], lhsT=Jb[:], rhs=TZ[:, zi, h, :], start=False, stop=True),
                                     [Tc, TTZ], [TP])
                          bias = nbias if isnear else fbias
                      nb_ = len(bt)
                      pi_ = pti[0] % NPT
                      pti[0] += 1
                      if kind == "cmp" or (kind == "win" and isnear):
                          ACT(lambda e: e.activation(out=PTb[pi_][:, 0:nb_ * 128], in_=P[:, 0:nb_ * 128], func=AF.Exp),
                              [TP], [TPT[pi_]])
                      else:
                          ACT(lambda e: e.activation(out=PTb[pi_][:, 0:nb_ * 128], in_=P[:, 0:nb_ * 128], func=AF.Exp,
                                                     bias=bias), [TP, Tb31], [TPT[pi_]])
                      tk_["pt"] = pi_

                  def emit_pv(tk_):
                      kind, g, h, bt = tk_["kind"], tk_["g"], tk_["h"], tk_["bt"]
                      key = (kind, h)
                      if tk_["first"]:
                          accs[key] = bank("acc")
                      P2, TP2 = accs[key]
                      pi_ = tk_["pt"]
                      for i, kt in enumerate(bt):
                          st_ = tk_["first"] and i == 0
                          sp_ = tk_["last"] and i == len(bt) - 1
                          if kind == "cmp":
                              PE(lambda e: e.matmul(P2[:, 0:129], lhsT=PTb[pi_][:, kt * 128:(kt + 1) * 128],
                                                    rhs=vcA[:, kt, g, :], start=st_, stop=sp_), [TPT[pi_], TvcA], [TP2])
                          else:
                              VA, TV = (vsA, Tvs) if kind == "sel" else (vwA, Tvw)
                              PE(lambda e: e.matmul(P2[:, 0:65], lhsT=PTb[pi_][:, i * 128:(i + 1) * 128],
                                                    rhs=VA[:, kt, g, :], start=st_, stop=sp_), [TPT[pi_], TV], [TP2])
                      if tk_["last"]:
                          if kind == "cmp":
                              ACT(lambda e: e.copy(out=OC[:, h, :], in_=P2[:, 0:129]), [TP2], [TOC])
                          elif kind == "sel":
                              ACT(lambda e: e.copy(out=Os[:, h, :], in_=P2[:, 0:65]), [TP2], [TOs])
                          else:
                              ACT(lambda e: e.copy(out=Ow[:, h, :], in_=P2[:, 0:65]), [TP2], [TOw])

                  LAGA = 3

                  def run_tasks(tasks):
                      for i_ in range(len(tasks) + LAGA):
                          if i_ < len(tasks):
                              emit_scores(tasks[i_])
                          if i_ >= LAGA:
                              emit_pv(tasks[i_ - LAGA])

                  run_tasks([t_ for g in range(2) for j in range(4) for t_ in make_tasks("cmp", g, j)])
                  ck("D1")
                  DVE(lambda e: e.tensor_scalar(out=cf[:, 0, :], in0=OC[:, :, 64], scalar1=1e-30, scalar2=None,
                                                op0=ALU.max), [TOC], [Tcf])
                  DVE(lambda e: e.reciprocal(out=cf[:, 1, :], in_=cf[:, 0, :]), [Tcf], [Tcf])
                  for g in range(2):
                      sc_, s2_, m8_, sb_ = sc64[g], score2[g], m8[g], selb[g]
                      DVE(lambda e: e.tensor_scalar(out=sc_[:, :], in0=OC[:, 4 * g, 65:129],
                                                    scalar1=cf[:, 1, 4 * g:4 * g + 1], scalar2=None, op0=ALU.mult),
                          [TOC, Tcf], [Tsel[g]])
                      for j in range(1, 4):
                          DVE(lambda e: e.scalar_tensor_tensor(out=sc_[:, :], in0=OC[:, 4 * g + j, 65:129],
                                                               scalar=cf[:, 1, 4 * g + j:4 * g + j + 1], in1=sc_[:, :],
                                                               op0=ALU.mult, op1=ALU.add), [TOC, Tcf, Tsel[g]], [Tsel[g]])
                      DVE(lambda e: e.tensor_tensor(out=sc_[:, :], in0=sc_[:, :], in1=Cstt[slc][:, tt, :], op=ALU.add),
                          [Tsel[g], TCst[slc]], [Tsel[g]])
                      DVE(lambda e: e.max(out=m8_[:, 0:8], in_=sc_[:, :]), [Tsel[g]], [Tsel[g]])
                      DVE(lambda e: e.match_replace(out=s2_[:, :], in_to_replace=m8_[:, 0:8], in_values=sc_[:, :],
                                                    imm_value=-1e30), [Tsel[g]], [Tsel[g]])
                      DVE(lambda e: e.max(out=m8_[:, 8:16], in_=s2_[:, :]), [Tsel[g]], [Tsel[g]])
                      DVE(lambda e: e.tensor_scalar(out=sb_[:, 0:64], in0=sc_[:, :], scalar1=m8_[:, 15:16], scalar2=256.0,
                                                    op0=ALU.is_ge, op1=ALU.mult), [Tsel[g]], [Tsel[g]])
                  if G == 0 and tt == 0:
                      dump("OC", OC, TOC, [128, 8, 129], F32)
                  ck("D2")
                  run_tasks([t_ for g in range(2) for j in range(4) for t_ in make_tasks("win", g, j)])
                  for g in range(2):
                      P, TP = bank("sc")
                      Pb = bfv(P)
                      PE(lambda e: e.transpose(out=Pb[:, 0:128], in_=selb[g][:, :], identity=ident_b[:]),
                         [Tsel[g], Tc], [TP])
                      ACT(lambda e: e.copy(out=selT[g][:, :], in_=Pb[:, 0:128]), [TP], [TselT[g]])
                  run_tasks([t_ for g in range(2) for j in range(4) for t_ in make_tasks("sel", g, j)])
                  ck("D3")
                  gtv = gtt[tt].rearrange("p (h b) -> p h b", b=3)
                  for bi, (O_, TO_) in enumerate(((OC, TOC), (Os, TOs), (Ow, TOw))):
                      DVE(lambda e: e.tensor_scalar(out=cf[:, 2, :], in0=O_[:, :, 64], scalar1=1e-30, scalar2=None,
                                                    op0=ALU.max), [TO_], [Tcf])
                      DVE(lambda e: e.reciprocal(out=cf[:, 2, :], in_=cf[:, 2, :]), [Tcf], [Tcf])
                      DVE(lambda e: e.tensor_tensor(out=cf[:, 3 + bi, :], in0=cf[:, 2, :], in1=gtv[:, :, bi], op=ALU.mult),
                          [Tcf, Tgt[tt]], [Tcf])
                  for bi, (O_, TO_) in enumerate(((OC, TOC), (Os, TOs), (Ow, TOw))):
                      cfb = cf[:, 3 + bi, :].unsqueeze(2).broadcast_to([128, 8, 64])
                      dst = yacc if bi == 0 else ytmp
                      DVE(lambda e: e.tensor_tensor(out=dst, in0=O_[:, :, 0:64], in1=cfb, op=ALU.mult),
                          [TO_, Tcf], [Tyacc])
                      if bi == 1:
                          DVE(lambda e: e.tensor_tensor(out=yacc, in0=yacc, in1=ytmp, op=ALU.add), [Tyacc], [Tyacc])
                      if bi == 2:
                          DVE(lambda e: e.tensor_tensor(out=ya.rearrange("p (h d) -> p h d", h=8), in0=yacc, in1=ytmp,
                                                        op=ALU.add), [Tyacc], [Tya])
                  if G == 0 and tt == 0:
                      dump("ya", ya, Tya, [128, 512], BF16)
                      dump("Os", Os, TOs, [128, 8, 65], F32)
                      dump("Ow", Ow, TOw, [128, 8, 65], F32)
                  P, TP = bank()
                  Pb = bfv(P)
                  for c in range(4):
                      PE(lambda e: e.transpose(out=Pb[:, c * 128:(c + 1) * 128], in_=ya[:, c * 128:(c + 1) * 128],
                                               identity=ident_b[:]), [Tya, Tc], [TP])
                  ACT(lambda e: e.copy(out=yaT[:, :, qsl], in_=Pb[:, 0:512].rearrange("p (c t) -> p c t", c=4)),
                      [TP], [TyaT])

              ck("D")
              for ci in range(4):
                  wdw = 2 ** (ci + 1)
                  a_ap, Ta = zp[:, ci, :], Tzp
                  for stp in range(ci + 1):
                      sh = 2 ** stp
                      lo = 2 ** (stp + 1) - 1
                      d_ap, Td = ptmp[stp % 2], Tptmp[stp % 2]
                      DVE(lambda e: e.tensor_tensor(out=d_ap[:, lo:272], in0=a_ap[:, lo:272], in1=a_ap[:, lo - sh:272 - sh],
                                                    op=ALU.add), [Ta], [Td])
                      a_ap, Ta = d_ap, Td
                  DVE(lambda e: e.scalar_tensor_tensor(out=pooled[:, ci, :], in0=a_ap[:, 16:272], scalar=1.0 / wdw,
                                                       in1=zp[:, ci, 16:272], op0=ALU.mult, op1=ALU.subtract),
                      [Ta, Tzp], [Tpooled])
                  if G == 0:
                      DVE(lambda e: e.tensor_tensor(out=tmp16[:, :], in0=a_ap[:, 16:32], in1=invc16[:, ci, :], op=ALU.mult),
                          [Ta, Tc], [Tt1])
                      DVE(lambda e: e.tensor_tensor(out=pooled[:, ci, 0:16], in0=tmp16[:, :], in1=zp[:, ci, 16:32],
                                                    op=ALU.subtract), [Tt1, Tzp], [Tpooled])
                  P, TP = bank()
                  PE(lambda e: e.matmul(P[:, 0:TG], lhsT=wpool[:, ci, :], rhs=pooled[:, ci, :], start=True, stop=True),
                     [Twpool, Tpooled], [TP])
                  ACT(lambda e: e.activation(out=ypT[:, ci, :], in_=P[:, 0:TG], func=AF.Copy, scale=pscale[:, ci:ci + 1]),
                      [TP, Tc], [TypT])
              if G == 0:
                  dump("ypT", ypT, TypT, [128, 4, TG], BF16)
                  dump("yaT", yaT, TyaT, [128, 4, TG], BF16)
              def load_mw(half_, pr2_):
                  return [load_ws(winA_s, Tscr["winA"], 1792 + gi_ * 1024 + half_ * 512 + pr2_ * 256) for gi_ in range(2)]

              mw_req = [(h_, p_) for h_ in range(2) for p_ in range(2)]
              mw_q = [load_mw(0, 0)]
              wo_all = None
              for half in range(2):
                  if half == 1:
                      DMA(wbb[0][:], wbp_s.ap()[:, :, half * 512:(half + 1) * 512], [Tscr["wbp"]], [Twbb[0]])
                      DMA(wbb[1][:], wba_s.ap()[:, :, half * 512:(half + 1) * 512], [Tscr["wba"]], [Twbb[1]])
                  for pr2 in range(2):
                      mw = mw_q.pop(0)
                      idx_ = half * 2 + pr2
                      if idx_ + 1 < 4:
                          mw_q.append(load_mw(*mw_req[idx_ + 1]))
                      else:
                          wo_all = [[load_ws(wout_s, Tscr["wout"], h_ * 512 + i_ * 256) for i_ in range(2)]
                                    for h_ in range(2)]
                      for i in range(2):
                          mc = half * 4 + pr2 * 2 + i
                          lc = pr2 * 2 + i
                          for gi in range(2):
                              P, TP = bank()
                              for k in range(8):
                                  PE(lambda e: e.matmul(P[:, 0:TG], lhsT=mw[gi][0][:, k, i * 128:(i + 1) * 128],
                                                        rhs=hT[:, k, :], start=(k == 0), stop=(k == 7)),
                                     [mw[gi][1], ThT], [TP])
                              ACT(lambda e: e.activation(out=mgt[gi][:, :], in_=P[:, 0:TG], func=AF.Sigmoid),
                                  [TP], [Tmgt[gi]])
                          Pa, TPa = bank()
                          for c in range(4):
                              PE(lambda e: e.matmul(Pa[:, 0:TG], lhsT=wbb[0][:, c, lc * 128:(lc + 1) * 128],
                                                    rhs=ypT[:, c, :], start=(c == 0), stop=(c == 3)),
                                 [Twbb[0], TypT], [TPa])
                          Pb_, TPb = bank()
                          for c in range(4):
                              PE(lambda e: e.matmul(Pb_[:, 0:TG], lhsT=wbb[1][:, c, lc * 128:(lc + 1) * 128],
                                                    rhs=yaT[:, c, :], start=(c == 0), stop=(c == 3)),
                                 [Twbb[1], TyaT], [TPb])
                          DVE(lambda e: e.tensor_tensor(out=t1[:, :], in0=Pa[:, 0:TG], in1=mgt[0][:, :], op=ALU.mult),
                              [TPa, Tmgt[0]], [Tt1])
                          DVE(lambda e: e.tensor_tensor(out=sq[:, :], in0=Pb_[:, 0:TG], in1=mgt[1][:, :], op=ALU.mult),
                              [TPb, Tmgt[1]], [Tsq])
                          DVE(lambda e: e.tensor_tensor(out=mixT[:, mc, :], in0=t1[:, :], in1=sq[:, :], op=ALU.add),
                              [Tt1, Tsq], [TmixT])
              if G == 0:
                  dump("mixT", mixT, TmixT, [128, 8, TG], BF16)
              ck("E1")
              for half in range(2):
                  wo = wo_all[half]
                  for tt in range(2):
                      P, TP = bank()
                      for i in range(2):
                          for k in range(8):
                              PE(lambda e: e.matmul(P[:, i * 256:(i + 1) * 256], lhsT=mixT[:, k, tt * 128:(tt + 1) * 128],
                                                    rhs=wo[i][0][:, k, :], start=(k == 0), stop=(k == 7)),
                                 [wo[i][1], TmixT], [TP])
                      DVE(lambda e: e.tensor_tensor(out=xt[tt][:, half * 512:(half + 1) * 512], in0=P[:, 0:512],
                                                    in1=xin[tt][:, half * 512:(half + 1) * 512], op=ALU.add),
                          [TP, Txin[tt]], [Txt[tt]])
              if G + 1 < NG:
                  load_x(G + 1)
              if G == 0:
                  dump("x1", xt[0][:], Txt[0], [128, DM], F32)
              ck("E")
              wq_ = [load_ws(wq_s, Tscr["wq"], p_ * 256) for p_ in range(PFW)]
              norm_both(1, h2T, Th2, xn2, Txn2, ss2, Tss2)
              if G == 0:
                  dump("h2T", h2T[:], Th2, [128, 8, TG], BF16)
              S.barrier()

              ck("F")
              carve_reset()
              pqT = carve([16, TG], BF16); Tpq = Tk("pqT")
              scs = carve([16, 128], F32); Tscs = Tk("scs")
              m1 = carve([16, 16], F32); i1 = carve([16, 16], U32); i1f = carve([16, 16], F32); Tm1 = Tk("m1")
              tmpr = carve([16, 128], F32)
              cand = carve([8, 16, 16], F32); Tcand = Tk("cand")
              ctmp = carve([8, 256], F32)
              eq = carve([128, 16], F32); Teq = Tk("eq")
              ts_ = carve([8, 16], F32); tj = carve([8, 16], U32); ta = carve([8, 16], U32); tb_ = carve([8, 16], U32)
              af_ = carve([128], F32); bf_ = carve([128], F32)
              If_ = carve([128], F32); Jf_ = carve([128], F32); ex = carve([8, 16], F32); gf = carve([8, 16], F32)
              s8 = carve([8], F32)
              Tts = Tk("ts")
              for pc in range(8):
                  wt, Twt = wq_.pop(0)
                  if pc + PFW < 8:
                      wq_.append(load_ws(wq_s, Tscr["wq"], (pc + PFW) * 256))
                  for i in range(2):
                      c = 2 * pc + i
                      P, TP = bank()
                      for k in range(8):
                          PE(lambda e: e.matmul(P[:, 0:TG], lhsT=wt[:, k, i * 128:(i + 1) * 128], rhs=h2T[:, k, :],
                                                start=(k == 0), stop=(k == 7)), [Twt, Th2], [TP])
                      ACT(lambda e: e.copy(out=pqT[:, c, :], in_=P[:, 0:TG]), [TP], [Tpq])
              for tt in range(2):
                  tsl = slice(tt * 128, (tt + 1) * 128)
                  for cbk in range(4):
                      P, TP = bank()
                      for i in range(4):
                          c = cbk * 4 + i
                          PE(lambda e: e.matmul(P[:, i * 128:(i + 1) * 128], lhsT=pqT[:, c, tsl], rhs=skT[:, c, :],
                                                start=True, stop=True), [Tpq, Tsk], [TP])
                      ACT(lambda e: e.copy(out=scs[:, cbk * 4:(cbk + 1) * 4, :],
                                           in_=P[:, 0:512].rearrange("p (c k) -> p c k", c=4)), [TP], [Tscs])
                  Ta_ = [Tk("m1a%d" % c) for c in range(16)]
                  Tb_ = [Tk("m1b%d" % c) for c in range(16)]
                  Tr_ = [Tk("tmpr%d" % c) for c in range(16)]
                  for c in range(16):
                      DVE(lambda e: e.max(out=m1[:, c, 0:8], in_=scs[:, c, :]), [Tscs], [Ta_[c]])
                  for c in range(16):
                      DVE(lambda e: e.match_replace(out=tmpr[:, c, :], in_to_replace=m1[:, c, 0:8], in_values=scs[:, c, :],
                                                    imm_value=-1e30), [Tscs, Ta_[c]], [Tr_[c]])
                  for c in range(16):
                      DVE(lambda e: e.max_index(out=i1[:, c, 0:8], in_max=m1[:, c, 0:8], in_values=scs[:, c, :]),
                          [Tscs, Ta_[c]], [Ta_[c]])
                  for c in range(16):
                      DVE(lambda e: e.max(out=m1[:, c, 8:16], in_=tmpr[:, c, :]), [Tr_[c]], [Tb_[c]])
                  for c in range(16):
                      DVE(lambda e: e.max_index(out=i1[:, c, 8:16], in_max=m1[:, c, 8:16], in_values=tmpr[:, c, :]),
                          [Tr_[c], Tb_[c]], [Tb_[c]])
                  TM = Ta_ + Tb_
                  DVE(lambda e: e.tensor_copy(out=i1f, in_=i1), TM, [Tm1])
                  m1v = m1.rearrange("p (h two) a -> p h two a", two=2)
                  i1v = i1f.rearrange("p (h two) a -> p h two a", two=2)
                  DVE(lambda e: e.tensor_tensor(out=cand, in0=m1v[:, :, 0, :].unsqueeze(3).broadcast_to([128, 8, 16, 16]),
                                                in1=m1v[:, :, 1, :].unsqueeze(2).broadcast_to([128, 8, 16, 16]),
                                                op=ALU.add), TM, [Tcand])
                  Tha = [Tk("tsa%d" % h) for h in range(8)]
                  Thb = [Tk("tsb%d" % h) for h in range(8)]
                  Thc = [Tk("ctmp%d" % h) for h in range(8)]
                  cvs = [cand[:, h, :, :].rearrange("p a b -> p (a b)") for h in range(8)]
                  for h in range(8):
                      DVE(lambda e: e.max(out=ts_[:, h, 0:8], in_=cvs[h]), [Tcand], [Tha[h]])
                  for h in range(8):
                      DVE(lambda e: e.match_replace(out=ctmp[:, h, :], in_to_replace=ts_[:, h, 0:8], in_values=cvs[h],
                                                    imm_value=-1e30), [Tcand, Tha[h]], [Thc[h]])
                  for h in range(8):
                      DVE(lambda e: e.max_index(out=tj[:, h, 0:8], in_max=ts_[:, h, 0:8], in_values=cvs[h]),
                          [Tcand, Tha[h]], [Tha[h]])
                  for h in range(8):
                      DVE(lambda e: e.max(out=ts_[:, h, 8:16], in_=ctmp[:, h, :]), [Thc[h]], [Thb[h]])
                  for h in range(8):
                      DVE(lambda e: e.max_index(out=tj[:, h, 8:16], in_max=ts_[:, h, 8:16], in_values=ctmp[:, h, :]),
                          [Thc[h], Thb[h]], [Thb[h]])
                  DVE(lambda e: e.tensor_single_scalar(out=ta, in_=tj, scalar=4, op=ALU.logical_shift_right),
                      Tha + Thb, [Tts])
                  DVE(lambda e: e.tensor_single_scalar(out=tb_, in_=tj, scalar=15, op=ALU.bitwise_and), [Tts], [Tts])
                  DVE(lambda e: e.tensor_copy(out=af_[:, :], in_=ta.rearrange("p h r -> p (h r)")), [Tts], [Tts])
                  DVE(lambda e: e.tensor_copy(out=bf_[:, :], in_=tb_.rearrange("p h r -> p (h r)")), [Tts], [Tts])
                  for (src_f, half_i, dstf) in ((af_, 0, If_), (bf_, 1, Jf_)):
                      DVE(lambda e: e.tensor_tensor(out=eq, in0=src_f[:, :].unsqueeze(2).broadcast_to([128, 128, 16]),
                                                    in1=iota16[:, :].unsqueeze(1).broadcast_to([128, 128, 16]),
                                                    op=ALU.is_equal), [Tts, Tc], [Teq])
                      eq4 = eq.rearrange("p (h r) a -> p h r a", h=8)
                      DVE(lambda e: e.tensor_tensor(out=eq4, in0=eq4,
                                                    in1=i1v[:, :, half_i, :].unsqueeze(2).broadcast_to([128, 8, 16, 16]),
                                                    op=ALU.mult), [Teq, Tm1], [Teq])
                      DVE(lambda e: e.tensor_reduce(out=dstf[:, :], in_=eq, axis=AX.X, op=ALU.add), [Teq], [Tts])
                  DVE(lambda e: e.tensor_tensor(out=ex, in0=ts_, in1=ts_[:, :, 0:1].broadcast_to([128, 8, 16]),
                                                op=ALU.subtract), [Tts], [Tts])
                  ACT(lambda e: e.activation(out=ex, in_=ex, func=AF.Exp), [Tts], [Tts])
                  DVE(lambda e: e.tensor_reduce(out=s8[:, :], in_=ex, axis=AX.X, op=ALU.add), [Tts], [Tts])
                  DVE(lambda e: e.reciprocal(out=s8[:, :], in_=s8[:, :]), [Tts], [Tts])
                  DVE(lambda e: e.tensor_tensor(out=gf, in0=ex, in1=s8[:, :].unsqueeze(2).broadcast_to([128, 8, 16]),
                                                op=ALU.mult), [Tts], [Tts])
                  P, TP = bank()
                  for i, srcT in enumerate((If_[:, :], Jf_[:, :], gf.rearrange("p h r -> p (h r)"))):
                      PE(lambda e: e.transpose(out=P[:, i * 128:(i + 1) * 128], in_=srcT, identity=ident_f[:]),
                         [Tts, Tc], [TP])
                  ACT(lambda e: e.copy(out=ITt[:, tsl], in_=P[:, 0:128]), [TP], [Tijg])
                  ACT(lambda e: e.copy(out=JTt[:, tsl], in_=P[:, 128:256]), [TP], [Tijg])
                  ACT(lambda e: e.copy(out=GTt[:, tsl], in_=P[:, 256:384]), [TP], [Tijg])
                  if G == 0 and tt == 0:
                      dump("If", If_, Tts, [128, 128], F32)
                      dump("skT", skT, Tsk, [128, 16, 128], BF16)
                      dump("pqT", pqT, Tpq, [128, 16, TG], BF16)
                      dump("scs", scs, Tscs, [128, 16, 128], F32)
                      dump("af", af_, Tts, [128, 128], F32)
                      dump("bf", bf_, Tts, [128, 128], F32)
                      dump("tj", tj, Tts, [128, 8, 16], U32)
                      dump("ts", ts_, Tts, [128, 8, 16], F32)
                      dump("m1", m1, Tm1, [128, 16, 16], F32)
                      dump("i1f", i1f, Tm1, [128, 16, 16], F32)
                      dump("Jf", Jf_, Tts, [128, 128], F32)
                      dump("gf", gf, Tts, [128, 8, 16], F32)
              S.barrier()

              ck("P2")
              carve_reset()
              W = carve([128, TG], BF16)
              TW = Tk("W")

              def flat_(t_):
                  a_ = t_[:]
                  return a_.rearrange("p a b -> p (a b)")

              NUS = 5
              uslots = [uvu[0][:], uvu[1][:], uvu[2][:],
                        flat_(ws[0]).rearrange("p (j k i) -> p j k i", j=2, k=8),
                        flat_(ws[2]).rearrange("p (j k i) -> p j k i", j=2, k=8)]
              vslots = [uvv[0][:], uvv[1][:], uvv[2][:],
                        flat_(ws[1]).rearrange("p (j d) -> p j d", j=2),
                        flat_(wbb[0]).rearrange("p (j d) -> p j d", j=2)]
              PFD = 3

              def load_uv(jp):
                  sl = jp % NUS
                  DMA(uslots[sl], uT_s.ap()[jp], [Tscr["uT"]], [Tus[sl]])
                  DMA(vslots[sl], v_s.ap()[jp], [Tscr["v"]], [Tus[sl]], nowaw=True)

              for jp_ in range(PFD):
                  load_uv(jp_)
              for t4 in range(TG // 4):
                  P, TP = bank("hi")
                  for i in range(4):
                      t = 4 * t4 + i
                      sl = t % NLR
                      DVE(lambda e: e.tensor_scalar(out=Lb[sl][:], in0=iotaB[:], scalar1=ITt[:, t:t + 1],
                                                    scalar2=GTt[:, t:t + 1], op0=ALU.is_equal, op1=ALU.mult),
                          [Tc, Tijg], [TL[sl]])
                      DVE(lambda e: e.tensor_scalar(out=Rb[sl][:], in0=iotaB[:], scalar1=JTt[:, t:t + 1], scalar2=None,
                                                    op0=ALU.is_equal), [Tc, Tijg], [TR[sl]])
                      PE(lambda e: e.matmul(P[:, i * 128:(i + 1) * 128], lhsT=Lb[sl][:], rhs=Rb[sl][:], start=True, stop=True),
                         [TL[sl], TR[sl]], [TP])
                  ACT(lambda e: e.copy(out=W[:, :, 4 * t4:4 * t4 + 4], in_=P[:, 0:512].rearrange("p (t j) -> p j t", t=4)),
                      [TP], [TW])


              def stage_a(j):
                  jp, jj = j // 2, j % 2
                  sl = jp % NUS
                  if jj == 0 and jp + PFD < 64:
                      load_uv(jp + PFD)
                  s2 = j % NGA
                  P, TP = bank("hi")
                  for k in range(8):
                      PE(lambda e: e.matmul(P[:, 0:TG], lhsT=uslots[sl][:, jj, k, :], rhs=h2T[:, k, :],
                                            start=(k == 0), stop=(k == 7)), [Tus[sl], Th2], [TP])
                  ACT(lambda e: e.activation(out=gab[s2][:], in_=P[:, 0:TG], func=AF.Gelu_apprx_tanh), [TP], [Tga[s2]])
                  DVE(lambda e: e.tensor_tensor(out=wab[s2][:], in0=gab[s2][:], in1=W[:, j, :], op=ALU.mult),
                      [Tga[s2], TW], [Twa[s2]])

              def stage_b(j):
                  jp, jj = j // 2, j % 2
                  sl = jp % NUS
                  s2 = j % NGA
                  for tt in range(2):
                      for half in range(2):
                          bi = tt * 2 + half
                          PE(lambda e: e.matmul(pb[bi][:, 0:512], lhsT=wab[s2][:, tt * 128:(tt + 1) * 128],
                                                rhs=vslots[sl][:, jj, half * 512:(half + 1) * 512],
                                                start=(j == 0), stop=(j == 127)), [Twa[s2], Tus[sl]], [Tpb[bi]])

              LAG = 2
              for j in range(128 + LAG):
                  if j < 128:
                      stage_a(j)
                  if j >= LAG:
                      stage_b(j - LAG)
              for tt in range(2):
                  for half in range(2):
                      bi = tt * 2 + half
                      DVE(lambda e: e.tensor_tensor(out=xt[tt][:, half * 512:(half + 1) * 512], in0=pb[bi][:, 0:512],
                                                    in1=xt[tt][:, half * 512:(half + 1) * 512], op=ALU.add),
                          [Tpb[bi], Txt[tt]], [Txt[tt]])
                  DMA(out_d.ap()[g0 + tt * 128:g0 + (tt + 1) * 128, :], xt[tt][:], [Txt[tt]], [Tout], nowaw=True, semtk=Txt[tt])
              S.barrier()
        except _Stop:
            pass
        S.wait_all("sp", [Tout])
        S.wait_all("act", [Tout])
        counts = {k: v["n"] for k, v in S.eng.items()}
    return nc, dbg, counts


_CACHE = {}


def kernel(**inputs):
    if "nc" not in _CACHE:
        _CACHE["nc"] = build()[0]
        _CACHE["consts"] = host_consts()
    nc = _CACHE["nc"]
    consts = _CACHE["consts"]
    inputs = {k: np.asarray(v) for k, v in inputs.items()}
    in_maps = [make_in_map(inputs, b, consts) for b in range(8)]
    res = run_bass_kernel_spmd(nc, in_maps, core_ids=list(range(8)))
    out = np.stack([np.asarray(r["out"], np.float32) for r in res.results], axis=0)
    return out
```
